# Optimizing a Trainium2 kernel written in Bass

```python
import jax, jax.numpy as jnp
from jax import lax
import numpy as np

D_MODEL = 2048
BATCH = 2
SEQ = 4096
DEPTH = 1

CHUNK = 64
N_MEM = 256
EPS = 1e-6
GLA_HEADS = 4
GLA_DK = D_MODEL // 16
GLA_DV = D_MODEL // 8
GLA_LOWRANK = 16
GLA_GATE_NORMALIZER = 16.0
GLA_WIDTH = GLA_HEADS * GLA_DV
HGRN_HEADS = 8
HGRN_DH = D_MODEL // 16
HGRN_WIDTH = HGRN_HEADS * HGRN_DH
MIX_WIDTH = GLA_WIDTH + HGRN_WIDTH
IN_SPLITS = (GLA_HEADS * GLA_DK, GLA_HEADS * GLA_DK, GLA_WIDTH, GLA_WIDTH, GLA_LOWRANK,
             HGRN_WIDTH, HGRN_WIDTH, HGRN_WIDTH, HGRN_WIDTH)
IN_WIDTH = 512 + 512 + 1024 + 1024 + 16 + 4 * 1024
XA_HEADS = 4
XA_DH = D_MODEL // XA_HEADS
N_GROUPS = 4
EXPERTS_PER_GROUP = 8
N_EXPERTS = N_GROUPS * EXPERTS_PER_GROUP
TOP_K = 2
D_EXPERT = D_MODEL // 2
EXPERT_BLOCK = 128

kernel_name = "hybrid_gla_hgrn2_memxattn_hmoe"


def rmsnorm(x, g):
    xf = x.astype(jnp.float32)
    y = xf * lax.rsqrt(jnp.mean(xf * xf, axis=-1, keepdims=True) + EPS) * g.astype(jnp.float32)
    return y.astype(x.dtype)


def to_chunks(t, n_heads):
    B, T, _ = t.shape
    return t.reshape(B, T // CHUNK, CHUNK, n_heads, -1).transpose(1, 0, 3, 2, 4).astype(jnp.float32)


def from_chunks(t):
    N, B, H, C, d = t.shape
    return t.transpose(1, 0, 3, 2, 4).reshape(B, N * C, H, d)


def chunk_gated_linear_attention(q, k, v, log_a, n_heads, scale):
    qc = to_chunks(q, n_heads) * scale
    kc = to_chunks(k, n_heads)
    vc = to_chunks(v, n_heads)
    bc = jnp.cumsum(to_chunks(log_a, n_heads), axis=3)
    causal = jnp.tril(jnp.ones((CHUNK, CHUNK), dtype=bool))

    def step(S, inp):
        q_, k_, v_, b_ = inp
        rel = b_[:, :, :, None, :] - b_[:, :, None, :, :]
        decay = jnp.exp(jnp.where(causal[:, :, None], rel, -jnp.inf))
        A = jnp.einsum('bhid,bhjd,bhijd->bhij', q_, k_, decay)
        b_last = b_[:, :, -1:, :]
        o = (jnp.einsum('bhij,bhjv->bhiv', A, v_)
             + jnp.einsum('bhid,bhdv->bhiv', q_ * jnp.exp(b_), S))
        S = (jnp.exp(b_last[:, :, 0, :])[..., None] * S
             + jnp.einsum('bhjd,bhjv->bhdv', k_ * jnp.exp(b_last - b_), v_))
        return S, o

    B = q.shape[0]
    S0 = jnp.zeros((B, n_heads, qc.shape[-1], vc.shape[-1]), jnp.float32)
    _, o = lax.scan(step, S0, (qc, kc, vc, bc))
    return from_chunks(o)


def hybrid_mixer(h, w_in, w_alpha_up, b_alpha, gla_norm_g, lb, hgrn_norm_g, w_out):
    B, T, _ = h.shape
    f32 = jnp.float32
    proj = h @ w_in
    offsets = [int(o) for o in np.cumsum(IN_SPLITS)[:-1]]
    gq, gk, gv, gg, g_lr, hq, hf, hi, hg = jnp.split(proj, offsets, axis=-1)

    log_alpha = jax.nn.log_sigmoid((g_lr @ w_alpha_up + b_alpha).astype(f32)) / GLA_GATE_NORMALIZER
    o_gla = chunk_gated_linear_attention(gq, gk, gv, log_alpha, GLA_HEADS, GLA_DK ** -0.5)
    o_gla = rmsnorm(o_gla, gla_norm_g) * jax.nn.silu(gg.reshape(B, T, GLA_HEADS, GLA_DV).astype(f32))

    z = hf.astype(f32)
    log_f = jnp.logaddexp(jnp.log(lb), jnp.log1p(-lb) + jax.nn.log_sigmoid(z))
    k_h = (1.0 - lb) * jax.nn.sigmoid(-z)
    o_h = chunk_gated_linear_attention(jax.nn.silu(hq.astype(f32)), k_h, hi, log_f, HGRN_HEADS, 1.0)
    o_h = rmsnorm(o_h, hgrn_norm_g) * jax.nn.silu(hg.reshape(B, T, HGRN_HEADS, HGRN_DH).astype(f32))

    o = jnp.concatenate([o_gla.reshape(B, T, GLA_WIDTH), o_h.reshape(B, T, HGRN_WIDTH)], axis=-1)
    return o.astype(h.dtype) @ w_out


def memory_cross_attention(h, mem_n, w_q, w_kv, w_o):
    B, T, D = h.shape
    M = mem_n.shape[1]
    q = (h @ w_q).reshape(B, T, XA_HEADS, XA_DH)
    k, v = jnp.split(mem_n @ w_kv, 2, axis=-1)
    k = k.reshape(B, M, XA_HEADS, XA_DH)
    v = v.reshape(B, M, XA_HEADS, XA_DH)
    s = jnp.einsum('bqhd,bkhd->bhqk', q, k).astype(jnp.float32) * (XA_DH ** -0.5)
    p = jax.nn.softmax(s, axis=-1).astype(v.dtype)
    o = jnp.einsum('bhqk,bkhd->bqhd', p, v).reshape(B, T, D)
    return o @ w_o


def grouped_expert_mlp(xt, expert_idx, w_gate, w_up, w_down):
    M, K = expert_idx.shape
    D = xt.shape[-1]
    A = M * K
    flat_e = expert_idx.reshape(A)
    order = jnp.argsort(flat_e)
    sorted_e = flat_e[order]
    counts = jnp.bincount(flat_e, length=N_EXPERTS)
    padded = ((counts + EXPERT_BLOCK - 1) // EXPERT_BLOCK) * EXPERT_BLOCK
    pad_end = jnp.cumsum(padded)
    pad_start = pad_end - padded
    start = jnp.cumsum(counts) - counts
    dest = pad_start[sorted_e] + (jnp.arange(A) - start[sorted_e])
    n_rows = A + N_EXPERTS * EXPERT_BLOCK
    n_blocks = n_rows // EXPERT_BLOCK
    buf = jnp.zeros((n_rows, D), xt.dtype).at[dest].set(xt[order // K])
    block_e = jnp.minimum(jnp.searchsorted(pad_end, jnp.arange(n_blocks) * EXPERT_BLOCK, side='right'),
                          N_EXPERTS - 1)

    def block_fn(args):
        xb, e = args
        hb = jax.nn.silu(xb @ w_gate[e]) * (xb @ w_up[e])
        return hb @ w_down[e]

    yb = lax.map(block_fn, (buf.reshape(n_blocks, EXPERT_BLOCK, D), block_e)).reshape(n_rows, D)
    y = jnp.zeros((A, D), yb.dtype).at[order].set(yb[dest])
    return y.reshape(M, K, D)


def hierarchical_moe(h, w_rg, b_rg, w_re, b_re, w_gate, w_up, w_down):
    B, T, D = h.shape
    xt = h.reshape(B * T, D)
    g_prob = jax.nn.softmax((xt @ w_rg).astype(jnp.float32) + b_rg.astype(jnp.float32), axis=-1)
    p_group, group = lax.top_k(g_prob, 1)
    e_logits = ((xt @ w_re).astype(jnp.float32) + b_re.astype(jnp.float32)).reshape(-1, N_GROUPS, EXPERTS_PER_GROUP)
    e_logits = jnp.take_along_axis(e_logits, group[:, :, None], axis=1)[:, 0]
    top_val, top_idx = lax.top_k(e_logits, TOP_K)
    gate = jax.nn.softmax(top_val, axis=-1) * p_group
    expert_idx = group * EXPERTS_PER_GROUP + top_idx
    y = grouped_expert_mlp(xt, expert_idx, w_gate, w_up, w_down)
    out = jnp.einsum('mk,mkd->md', gate.astype(y.dtype), y)
    return out.reshape(B, T, D)


def setup_inputs(seed: int = 0) -> dict:
    key = jax.random.key(seed)
    ks = jax.random.split(key, 32)
    f32 = jnp.float32
    D, L = D_MODEL, DEPTH

    def nrm(k, shape, scale):
        return jax.random.normal(k, shape, f32) * scale

    def gain(k, shape):
        return 1.0 + 0.02 * jax.random.normal(k, shape, f32)

    return {
        "x": nrm(ks[0], (BATCH, SEQ, D), 1.0),
        "mem": nrm(ks[1], (BATCH, N_MEM, D), 1.0),
        "norm_mix_g": gain(ks[2], (L, D)),
        "w_in": nrm(ks[3], (L, D, IN_WIDTH), D ** -0.5),
        "w_gla_alpha_up": nrm(ks[4], (L, GLA_LOWRANK, GLA_HEADS * GLA_DK), GLA_LOWRANK ** -0.5),
        "b_gla_alpha": nrm(ks[5], (L, GLA_HEADS * GLA_DK), 0.1),
        "gla_out_norm_g": gain(ks[6], (L, GLA_DV)),
        "hgrn_lb_logits": nrm(ks[7], (L + 1, HGRN_WIDTH), 0.5),
        "hgrn_out_norm_g": gain(ks[8], (L, HGRN_DH)),
        "w_mix_out": nrm(ks[9], (L, MIX_WIDTH, D), MIX_WIDTH ** -0.5),
        "norm_xattn_g": gain(ks[10], (L, D)),
        "norm_mem_g": gain(ks[11], (L, D)),
        "w_xattn_q": nrm(ks[12], (L, D, D), D ** -0.5),
        "w_xattn_kv": nrm(ks[13], (L, D, 2 * D), D ** -0.5),
        "w_xattn_out": nrm(ks[14], (L, D, D), D ** -0.5),
        "norm_ffn_g": gain(ks[15], (L, D)),
        "w_router_group": nrm(ks[16], (L, D, N_GROUPS), D ** -0.5),
        "b_router_group": nrm(ks[17], (L, N_GROUPS), 0.01),
        "w_router_expert": nrm(ks[18], (L, D, N_EXPERTS), D ** -0.5),
        "b_router_expert": nrm(ks[19], (L, N_EXPERTS), 0.01),
        "w_expert_gate": nrm(ks[20], (L, N_EXPERTS, D, D_EXPERT), D ** -0.5),
        "w_expert_up": nrm(ks[21], (L, N_EXPERTS, D, D_EXPERT), D ** -0.5),
        "w_expert_down": nrm(ks[22], (L, N_EXPERTS, D_EXPERT, D), D_EXPERT ** -0.5),
        "norm_final_g": gain(ks[23], (D,)),
    }


def reference(x, mem, norm_mix_g, w_in, w_gla_alpha_up, b_gla_alpha, gla_out_norm_g, hgrn_lb_logits,
              hgrn_out_norm_g, w_mix_out, norm_xattn_g, norm_mem_g, w_xattn_q, w_xattn_kv, w_xattn_out,
              norm_ffn_g, w_router_group, b_router_group, w_router_expert, b_router_expert,
              w_expert_gate, w_expert_up, w_expert_down, norm_final_g):
    lb_all = jnp.cumsum(jax.nn.softmax(hgrn_lb_logits.astype(jnp.float32), axis=0), axis=0)
    h = x
    for l in range(DEPTH):
        a = rmsnorm(h, norm_mix_g[l])
        h = h + hybrid_mixer(a, w_in[l], w_gla_alpha_up[l], b_gla_alpha[l], gla_out_norm_g[l],
                             lb_all[l], hgrn_out_norm_g[l], w_mix_out[l])
        a = rmsnorm(h, norm_xattn_g[l])
        h = h + memory_cross_attention(a, rmsnorm(mem, norm_mem_g[l]), w_xattn_q[l], w_xattn_kv[l], w_xattn_out[l])
        a = rmsnorm(h, norm_ffn_g[l])
        h = h + hierarchical_moe(a, w_router_group[l], b_router_group[l], w_router_expert[l], b_router_expert[l],
                                 w_expert_gate[l], w_expert_up[l], w_expert_down[l])
    return rmsnorm(h, norm_final_g)
```

```python
import contextlib
import numpy as np
import concourse.bass as bass
import concourse.mybir as mybir
from concourse.bass_utils import run_bass_kernel_spmd

F32 = mybir.dt.float32
BF16 = mybir.dt.bfloat16
I32 = mybir.dt.int32
ALU = mybir.AluOpType
AF = mybir.ActivationFunctionType
AX = mybir.AxisListType

SAME_ENGINE_SYNC = True
D = 2048
T = 1024
NT = 8
KT = 16
EPS = 1e-6
NPREV = 3
N_EXP = 32
CAP = 128


class Tok:
    __slots__ = ("name", "last_write", "readers", "excl")

    def __init__(self, name, excl=False):
        self.name = name
        self.last_write = None
        self.readers = []
        self.excl = excl


class Op:
    __slots__ = ("eng", "fn", "reads", "writes", "dma_key", "idx", "signal", "seq",
                 "waits", "dma_cum", "deps", "barrier")

    def __init__(self, eng, fn, reads, writes, dma_key):
        self.eng = eng
        self.fn = fn
        self.reads = reads
        self.writes = writes
        self.dma_key = dma_key
        self.signal = False
        self.seq = 0
        self.waits = []
        self.dma_cum = 0
        self.deps = []
        self.barrier = False


class Prog:
    ENGS = ("pe", "act", "dve", "pool", "sp")

    def __init__(self, nc):
        self.nc = nc
        self.ops = []
        self.final_dma = []

    def tok(self, name="t"):
        return Tok(name)

    def toks(self, name, n, excl=False):
        return [Tok("%s%d" % (name, i), excl) for i in range(n)]

    def add(self, eng, fn, reads=(), writes=(), dma_key=None):
        op = Op(eng, fn, [t for t in reads if t is not None],
                [t for t in writes if t is not None], dma_key)
        op.idx = len(self.ops)
        self.ops.append(op)
        return op

    def pe(self, fn, reads=(), writes=()):
        return self.add("pe", fn, reads, writes)

    def act(self, fn, reads=(), writes=()):
        return self.add("act", fn, reads, writes)

    def dve(self, fn, reads=(), writes=()):
        return self.add("dve", fn, reads, writes)

    def pool(self, fn, reads=(), writes=()):
        return self.add("pool", fn, reads, writes)

    def dma(self, q, fn, key, reads=(), writes=(), final=False):
        op = self.add(q, fn, reads, writes, dma_key=key)
        if final:
            self.final_dma.append(op)
        return op

    def barrier(self):
        op = Op("sp", lambda e: e.nop(), [], [], None)
        op.barrier = True
        op.idx = len(self.ops)
        self.ops.append(op)
        return op

    def build(self):
        nc = self.nc
        ops = self.ops
        last_eng = {}
        last_dma = {}
        cur_barrier = None
        seen_after = set()
        for op in ops:
            if op.barrier:
                op.deps = [d for d in last_eng.values() if d.eng != "sp"] + \
                          [d for k_, d in last_dma.items() if not str(k_).startswith("cv")]
                for d in op.deps:
                    if d.dma_key is None:
                        d.signal = True
                cur_barrier = op
                seen_after = set()
                continue
            if op.dma_key is not None:
                last_dma[op.dma_key] = op
            else:
                last_eng[op.eng] = op
            if cur_barrier is not None and op.eng not in seen_after:
                seen_after.add(op.eng)
                if op.eng != "sp":
                    op.deps.append(cur_barrier)
                    cur_barrier.signal = True
            deps = {}
            for t in op.reads:
                if t.last_write is not None:
                    deps[t.last_write.idx] = ("raw", t.last_write)
                if t.excl:
                    for r in t.readers:
                        if r.idx not in deps and r.eng != op.eng:
                            deps[r.idx] = ("war", r)
            for t in op.writes:
                if t.last_write is not None:
                    deps.setdefault(t.last_write.idx, ("waw", t.last_write))
                for r in t.readers:
                    if r.idx not in deps:
                        deps[r.idx] = ("war", r)
            for t in op.reads:
                t.readers.append(op)
            for t in op.writes:
                t.last_write = op
                t.readers = []
            best = {}
            for kind, d in deps.values():
                if d is op:
                    continue
                if d.dma_key is None and d.eng == op.eng:
                    if op.eng == "pe" or op.eng == "sp":
                        continue
                    if not SAME_ENGINE_SYNC:
                        continue
                if d.dma_key is not None:
                    op.deps.append(d)
                else:
                    b = best.get(d.eng)
                    if b is None or d.idx > b.idx:
                        best[d.eng] = d
            for d in best.values():
                op.deps.append(d)
                d.signal = True
        cnt = {e: 0 for e in self.ENGS}
        dma_cnt = {}
        for op in ops:
            if op.dma_key is not None:
                dma_cnt[op.dma_key] = dma_cnt.get(op.dma_key, 0) + 16
                op.dma_cum = dma_cnt[op.dma_key]
            elif op.signal:
                cnt[op.eng] += 1
                op.seq = cnt[op.eng]
        waited = {e: {} for e in self.ENGS}
        dma_cnt2 = {}
        for op in ops:
            if op.dma_key is not None:
                dma_cnt2[op.dma_key] = dma_cnt2.get(op.dma_key, 0) + 16
            need = {}
            for d in op.deps:
                if d.dma_key is not None:
                    k = ("dma", d.dma_key)
                    v = dma_cnt2[d.dma_key] if d.dma_key != op.dma_key else d.dma_cum
                else:
                    k = ("eng", d.eng)
                    v = d.seq
                if v > need.get(k, 0):
                    need[k] = v
            w = waited[op.eng]
            for k, v in need.items():
                if w.get(k, 0) >= v:
                    continue
                w[k] = v
                op.waits.append((k, v))
        dma_keys = sorted(dma_cnt.keys(), key=str)
        self.n_sems = len(dma_keys) + len(self.ENGS)
        self.counts = dict(cnt)
        sems = {}
        with contextlib.ExitStack() as st:
            for e in self.ENGS:
                sems[("eng", e)] = st.enter_context(nc.semaphore("s_" + e))
            for i, k in enumerate(dma_keys):
                sems[("dma", k)] = st.enter_context(nc.semaphore("d_%d" % i))
            block = st.enter_context(nc.Block())
            per = {e: [o for o in ops if o.eng == e] for e in self.ENGS}
            final = [(("dma", o.dma_key), dma_cnt[o.dma_key]) for o in self.final_dma]

            def emit(engine, name):
                for op in per[name]:
                    for k, v in op.waits:
                        engine.wait_ge(sems[k], v)
                    ins = op.fn(engine)
                    if op.dma_key is not None:
                        ins.then_inc(sems[("dma", op.dma_key)], 16)
                    elif op.signal:
                        ins.then_inc(sems[("eng", name)], 1)
                if name == "sp":
                    done = set()
                    for k, v in final:
                        if k in done:
                            continue
                        done.add(k)
                        engine.wait_ge(sems[k], v)

            @block.tensor
            def _(e):
                emit(e, "pe")

            @block.scalar
            def _(e):
                emit(e, "act")

            @block.vector
            def _(e):
                emit(e, "dve")

            @block.gpsimd
            def _(e):
                emit(e, "pool")

            @block.sync
            def _(e):
                emit(e, "sp")
        return nc


ARENA_BYTES = 207 * 1024
NCONV = 24
CONV_SET = set(e for e in range(32) if e % 4 != 0)
DBG = {"units": 12, "seg0": 0, "proj": True, "full": True, "skip": "", "cut": 99}


class Arena:
    def __init__(self, ap):
        self.ap = ap
        self.top = 0
        self.high = 0

    def alloc(self, shape, dt=F32):
        assert shape[0] <= 128
        n = 1
        for s_ in shape[1:]:
            n *= s_
        esz = 2 if dt == BF16 else 4
        nbytes = (n * esz + 63) // 64 * 64
        off = self.top
        self.top += nbytes
        self.high = max(self.high, self.top)
        assert self.top <= ARENA_BYTES, ("arena overflow", self.top)
        v = self.ap[:, off // 2:(off + n * esz) // 2]
        if dt != BF16:
            v = v.bitcast(dt)
        if shape[0] < 128:
            v = v[0:shape[0]]
        if len(shape) == 3:
            v = v.rearrange("p (a b) -> p a b", b=shape[2])
        elif len(shape) == 4:
            v = v.rearrange("p (a b c) -> p a b c", b=shape[2], c=shape[3])
        return v


def build_nc(stage=3, final_norm=True):
    nc = bass.Bass("TRN2", target_bir_lowering=False)
    P = Prog(nc)
    st = contextlib.ExitStack()
    names = []

    def din(name, shape, dt=F32):
        names.append(name)
        return nc.dram_tensor(name, list(shape), dt, kind="ExternalInput").ap()

    x_own = din("x_own", [T, D])
    x_prev = din("x_prev", [NPREV * T, D])
    g_mix = din("norm_mix_g", [D])
    w_in = din("w_in", [D, 7184])
    w_up = din("w_gla_alpha_up", [16, 512])
    b_alpha = din("b_gla_alpha", [512, 1])
    gla_g = din("gla_out_norm_g", [256])
    lb_logits = din("hgrn_lb_logits", [2, 1024])
    hgrn_g = din("hgrn_out_norm_g", [128])
    w_mo = din("w_mix_out", [D, D])
    g_fin = din("norm_final_g", [D])
    if stage >= 2:
        mem = din("mem", [256, D])
        g_xa = din("norm_xattn_g", [D])
        g_mem = din("norm_mem_g", [D])
        w_xq = din("w_xattn_q", [D, D])
        w_xkv = din("w_xattn_kv", [D, 2 * D])
        w_xo = din("w_xattn_out", [D, D])
    if stage >= 3:
        g_ffn = din("norm_ffn_g", [D])
        w_rg = din("w_router_group", [D, 4])
        b_rg = din("b_router_group", [4])
        w_re = din("w_router_expert", [D, 32])
        b_re = din("b_router_expert", [32])
        w_eg = din("w_expert_gate", [N_EXP * D, 1024])
        w_eu = din("w_expert_up", [N_EXP * D, 1024])
        w_ed = din("w_expert_down", [N_EXP * 1024, D])
    out = nc.dram_tensor("out", [T, D], F32, kind="ExternalOutput").ap()
    conv_list = []
    if stage >= 3 and NCONV > 0:
        wbf_g = nc.dram_tensor("wbf_g", [N_EXP * D, 1024], BF16).ap()
        wbf_u = nc.dram_tensor("wbf_u", [N_EXP * D, 1024], BF16).ap()
        wbf_d = nc.dram_tensor("wbf_d", [N_EXP * 1024, D], BF16).ap()
        for ex in range(N_EXP):
            if ex in CONV_SET:
                conv_list.append((wbf_g[ex * D:(ex + 1) * D, :], w_eg[ex * D:(ex + 1) * D, :]))
                conv_list.append((wbf_u[ex * D:(ex + 1) * D, :], w_eu[ex * D:(ex + 1) * D, :]))
                conv_list.append((wbf_d[ex * 1024:(ex + 1) * 1024, :], w_ed[ex * 1024:(ex + 1) * 1024, :]))

    with st:
        arena_t = st.enter_context(nc.sbuf_tensor("arena", [128, ARENA_BYTES // 2], BF16))
        A = Arena(arena_t[:])
        psf = [st.enter_context(nc.psum_tensor("psf%d" % i, [128, 512], F32)) for i in range(8)]
        t_psf = P.toks("psf", 8, excl=True)

        def psb(i):
            return psf[i][:].bitcast(BF16)

        ones_f = A.alloc([128, 128])
        ident_bf = A.alloc([128, 128], BF16)
        ident_f = A.alloc([128, 128])
        maskT = A.alloc([128, 128])
        ltri = A.alloc([128, 128], BF16)
        ones_bf = A.alloc([128, 128], BF16)
        iota_row = A.alloc([128, 128])
        iota_p = A.alloc([128, 1])
        scanmask = A.alloc([128, T])
        gj = A.alloc([128, 4 * D], BF16)
        gbc = gj[:, 0:2 * D].bitcast(F32)
        ja = gj[:, 2 * D:4 * D]
        junk = ja[:, 0:D]
        a_tok = ja[:, D:2 * D]
        stat = A.alloc([128, 4, NT])
        gla_gbc = A.alloc([128, 256])
        hgrn_gbc = A.alloc([128, 128])
        wup_sb = A.alloc([16, 512])
        balpha_sb = A.alloc([128, 4])
        nbalpha = A.alloc([128, 4])
        lbl_sb = A.alloc([128, 2, 8])
        lb_sb = A.alloc([128, 8])
        oml_sb = A.alloc([128, 8])
        aT = A.alloc([128, KT, T], BF16)
        t_const, t_gbc, t_junk, t_atok, t_stat, t_par = P.toks("pp", 6)
        t_aT = P.toks("aT", NT)
        M_PERSIST = A.top

        P.pool(lambda e: e.memset(ones_f, 1.0), writes=[t_const])
        P.pool(lambda e: e.memset(ones_bf, 1.0), writes=[t_const])
        for dst in (ident_bf, ident_f) if "asel" not in DBG["skip"] else ():
            P.pool(lambda e, dst=dst: e.affine_select(out=dst, in_=ones_f, pattern=[[-1, 128]],
                                                      compare_op=ALU.is_equal, fill=0.0, base=0, channel_multiplier=1),
                   reads=[t_const], writes=[t_const])
        if "asel" not in DBG["skip"]:
            P.pool(lambda e: e.affine_select(out=maskT, in_=ones_f, pattern=[[1, 128]],
                                             compare_op=ALU.is_ge, fill=0.0, base=0, channel_multiplier=-1),
                   reads=[t_const], writes=[t_const])
            P.pool(lambda e: e.affine_select(out=ltri, in_=ones_f, pattern=[[1, 128]],
                                             compare_op=ALU.is_gt, fill=0.0, base=0, channel_multiplier=-1),
                   reads=[t_const], writes=[t_const])
        if "iota" not in DBG["skip"]:
            P.pool(lambda e: e.iota(iota_row, pattern=[[1, 128]], base=0, channel_multiplier=0,
                                    allow_small_or_imprecise_dtypes=True), writes=[t_const])
            P.pool(lambda e: e.iota(iota_p, pattern=[[0, 1]], base=0, channel_multiplier=1,
                                    allow_small_or_imprecise_dtypes=True), writes=[t_const])
        P.pool(lambda e: e.memset(scanmask, 1.0), writes=[t_const])
        P.pool(lambda e: e.memset(scanmask.rearrange("p (c j) -> p c j", j=128)[:, :, 0:1], 0.0),
               reads=[t_const], writes=[t_const])

        def load_gbc(src):
            P.dma("sp", lambda e: e.dma_start(out=gbc, in_=src.partition_broadcast(128)), "gbc", writes=[t_gbc])

        P.dma("sp", lambda e: e.dma_start(out=gla_gbc, in_=gla_g.partition_broadcast(128)), "par", writes=[t_par])
        P.dma("sp", lambda e: e.dma_start(out=hgrn_gbc, in_=hgrn_g.partition_broadcast(128)), "par", writes=[t_par])
        P.dma("sp", lambda e: e.dma_start(out=wup_sb, in_=w_up), "par", writes=[t_par])
        for hh in range(4):
            P.dma("sp", lambda e, hh=hh: e.dma_start(out=balpha_sb[:, hh:hh + 1], in_=b_alpha[hh * 128:(hh + 1) * 128, :]),
                  "par", writes=[t_par])
        for s_ in range(2) if "lbl" not in DBG["skip"] else ():
            for hh in range(8):
                P.dma("sp", lambda e, s_=s_, hh=hh: e.dma_start(
                    out=lbl_sb[:, s_, hh:hh + 1],
                    in_=lb_logits[s_:s_ + 1, hh * 128:(hh + 1) * 128].rearrange("o p -> p o")),
                    "par", writes=[t_par])
        P.dve(lambda e: e.tensor_sub(out=lb_sb, in0=lbl_sb[:, 0, :], in1=lbl_sb[:, 1, :]), reads=[t_par], writes=[t_par])
        P.act(lambda e: e.activation(out=lb_sb, in_=lb_sb, func=AF.Exp, scale=-1.0), reads=[t_par], writes=[t_par])
        P.dve(lambda e: e.tensor_scalar(out=lb_sb, in0=lb_sb, scalar1=1.0, scalar2=None, op0=ALU.add), reads=[t_par], writes=[t_par])
        P.dve(lambda e: e.reciprocal(out=lb_sb, in_=lb_sb), reads=[t_par], writes=[t_par])
        P.dve(lambda e: e.tensor_scalar(out=oml_sb, in0=lb_sb, scalar1=-1.0, scalar2=1.0,
                                        op0=ALU.mult, op1=ALU.add), reads=[t_par], writes=[t_par])
        P.dve(lambda e: e.tensor_scalar(out=nbalpha, in0=balpha_sb, scalar1=-1.0, scalar2=None,
                                        op0=ALU.mult), reads=[t_par], writes=[t_par])

        rr = {"ev": 0, "pa": 0, "pb": 0, "pool": [0, 1]}

        def evac(fn_act, fn_dve, reads, writes):
            rr["ev"] ^= 1
            if rr["ev"]:
                P.act(fn_act, reads, writes)
            else:
                P.dve(fn_dve, reads, writes)

        def copy_evac(out_ap, in_ap, reads, writes):
            evac(lambda e: e.activation(out=out_ap, in_=in_ap, func=AF.Copy),
                 lambda e: e.tensor_copy(out=out_ap, in_=in_ap), reads, writes)

        def sigmoid_to(out_ap, in_ap, reads, tok):
            P.act(lambda e: e.activation(out=out_ap, in_=in_ap, func=AF.Sigmoid), reads=reads, writes=[tok])

        def silu_to(out_ap, in_ap, reads, tok):
            P.act(lambda e: e.activation(out=out_ap, in_=in_ap, func=AF.Silu), reads=reads, writes=[tok])

        def sumsq(src_ap, src_tok, dst_ap, n):
            P.dve(lambda e: e.scalar_tensor_tensor(out=junk[:, 0:n], in0=src_ap, scalar=1.0, in1=src_ap,
                                                   op0=ALU.mult, op1=ALU.mult, accum_out=dst_ap),
                  reads=[src_tok], writes=[t_junk, t_stat])

        def rstd_of(dst_ap, ss_ap, tmp_ap, dim, tok):
            P.act(lambda e: e.activation(out=tmp_ap, in_=ss_ap, func=AF.Ln, scale=1.0 / dim, bias=EPS), reads=[tok], writes=[tok])
            P.act(lambda e: e.activation(out=dst_ap, in_=tmp_ap, func=AF.Exp, scale=-0.5), reads=[tok], writes=[tok])

        def next_pa():
            rr["pa"] = (rr["pa"] + 1) % len(rr["pool"])
            return rr["pool"][rr["pa"]]

        def next_pb():
            rr["pb"] ^= 1
            return 6 + rr["pb"]

        def transpose_tile(src, src_tok, dstT, dst_tok, cols):
            for half in range(2):
                pb = next_pb()
                for j in range(8):
                    kt = half * 8 + j
                    P.pe(lambda e, pb=pb, j=j, kt=kt: e.transpose(out=psb(pb)[:, j * 128:(j + 1) * 128],
                                                                   in_=src[:, kt * 128:(kt + 1) * 128],
                                                                   identity=ident_bf),
                         reads=[src_tok, t_const], writes=[t_psf[pb]])
                copy_evac(dstT[:, half * 8:(half + 1) * 8, cols],
                          psb(pb).rearrange("p (j c) -> p j c", c=128),
                          [t_psf[pb]], [dst_tok])

        def rms_stats(src_fn, src_toks, ntile, dim=D):
            for i in range(ntile):
                sumsq(src_fn(i), src_toks[i], stat[:, 0, i:i + 1], dim)
            rstd_of(stat[:, 3, 0:ntile], stat[:, 0, 0:ntile], stat[:, 1, 0:ntile], dim, t_stat)

        def norm_to_T(src_fn, src_toks, ntile, dstT, dst_toks):
            rms_stats(src_fn, src_toks, ntile)
            for i in range(ntile):
                P.dve(lambda e, i=i: e.scalar_tensor_tensor(out=a_tok, in0=src_fn(i), scalar=stat[:, 3, i:i + 1],
                                                            in1=gbc, op0=ALU.mult, op1=ALU.mult),
                      reads=[src_toks[i], t_stat, t_gbc], writes=[t_atok])
                transpose_tile(a_tok, t_atok, dstT, dst_toks[i], slice(i * 128, (i + 1) * 128))

        def wload(dst_ap, src_ap, key, tok):
            P.dma("pool", lambda e: e.dma_start(out=dst_ap, in_=src_ap), key, writes=[tok])

        t_cv = P.tok("cv")
        conv_state = {"i": 0}

        def emit_conv(n):
            for _ in range(n):
                if conv_state["i"] >= len(conv_list):
                    return
                dst, src = conv_list[conv_state["i"]]
                conv_state["i"] += 1
                tk = P.tok("cvi")
                conv_state["last_tok"] = tk
                P.dma("pool", lambda e, dst=dst, src=src: e.dma_start(out=dst, in_=src), "cv", writes=[tk])

        def wview(w, c0, n, r0=0, rows=D):
            return w[r0:r0 + rows, c0:c0 + n].rearrange("(kt p) n -> p kt n", p=128)

        def proj_fm(wap, wtok, src_T, src_toks, half, pa, M=128):
            for kt in range(KT):
                P.pe(lambda e, kt=kt: e.matmul(psf[pa][0:M, :], lhsT=wap[:, kt, :],
                                               rhs=src_T[:, kt, half * 512:(half + 1) * 512],
                                               start=(kt == 0), stop=(kt == KT - 1)),
                     reads=[wtok] + src_toks[half * 4:(half + 1) * 4], writes=[t_psf[pa]])

        o_tok = A.alloc([128, NT, D], BF16)
        t_otok = P.toks("otok", NT)
        M_AFTER_OTOK = A.top
        wq_b = [A.alloc([128, KT, 128], BF16) for _ in range(2)]
        wk_b = [A.alloc([128, KT, 128], BF16) for _ in range(2)]
        wvg_b = [A.alloc([128, KT, 512], BF16) for _ in range(1)]
        wlr = A.alloc([128, KT, 16], BF16)
        t_wq = P.toks("wq", 2)
        t_wk = P.toks("wk", 2)
        t_wvgh = P.toks("wvgh", 2)
        t_wlr = P.tok("wlr")
        qf = A.alloc([128, T])
        kf = A.alloc([128, T])
        t1 = A.alloc([128, T])
        t2 = A.alloc([128, T])
        t3 = A.alloc([128, T])
        qt = A.alloc([128, T], BF16)
        ktl = A.alloc([128, T], BF16)
        qs = A.alloc([128, T], BF16)
        khT = A.alloc([128, T], BF16)
        t_qf, t_kf, t_t1, t_t2, t_t3, t_qt, t_ktl, t_qs, t_khT = P.toks("mx", 9)
        sm = A.alloc([128, 4, NT])
        sm2 = A.alloc([128, 2, NT])
        t_sm = P.tok("sm")
        v_tok = A.alloc([128, NT, 256], BF16)
        sg_tok = A.alloc([128, NT, 256], BF16)
        sg_tmp = A.alloc([128, 256])
        khat = A.alloc([128, NT, 128], BF16)
        AT_sb = A.alloc([128, NT, 128], BF16)
        S_all = A.alloc([128, 2048])
        S_bf = A.alloc([128, NT + 1, 256], BF16)
        glrT = A.alloc([16, T])
        ost = A.alloc([128, 4, NT])
        o_all = A.alloc([128, NT, 256])
        S_tmp = A.alloc([128, 256])
        t_osb = P.toks("osb", NT)
        t_Stmp = P.tok("Stmp")
        xs = [A.alloc([128, D]) for _ in range(2)]
        t_v, t_sg, t_sgtmp, t_khat, t_AT, t_Sbf, t_glr, t_ost = P.toks("mb", 8)
        t_S = P.toks("S", 12)
        t_xs = P.toks("xs", 2)

        P.pool(lambda e: e.memset(S_all, 0.0), writes=t_S)
        P.pool(lambda e: e.memset(AT_sb, 0.0), writes=[t_AT])

        def capture(fn):
            start = len(P.ops)
            fn()
            ops_ = P.ops[start:]
            del P.ops[start:]
            return ops_

        def interleave(a, b):
            na, nb = len(a), len(b)
            ia = ib = 0
            while ia < na or ib < nb:
                if ib >= nb or (ia < na and ia * nb <= ib * na):
                    op = a[ia]
                    ia += 1
                else:
                    op = b[ib]
                    ib += 1
                op.idx = len(P.ops)
                P.ops.append(op)

        def full_cols(u):
            if u < 4:
                return u * 128, 512 + u * 128, 1024 + u * 256, 2048 + u * 256, 256
            hu = u - 4
            return 3088 + hu * 128, 4112 + hu * 128, 5136 + hu * 128, 6160 + hu * 128, 128

        def full_loads_qk(u):
            cq, ck, cv, cg, dv = full_cols(u)
            wb = u % 2
            wload(wq_b[wb], wview(w_in, cq, 128), "wq%d" % wb, t_wq[wb])
            wload(wk_b[wb], wview(w_in, ck, 128), "wk%d" % wb, t_wk[wb])

        def full_loads_vg(u):
            cq, ck, cv, cg, dv = full_cols(u)
            wvg = wvg_b[0]
            wload(wvg[:, :, 0:dv], wview(w_in, cv, dv), "wvg0", t_wvgh[0])
            wload(wvg[:, :, dv:2 * dv], wview(w_in, cg, dv), "wvg1", t_wvgh[1])

        def mixer_unit(u, full):
            gla = u < 4
            wb = u % 2
            dv = 256 if gla else 128
            nv = 2 * dv if full else dv
            sc = (-1.0 / 16.0) if gla else 1.0
            if gla:
                cq, ck, cv, cg = u * 128, 512 + u * 128, 1024 + u * 256, 2048 + u * 256
                scol = u * 256
            else:
                hu = u - 4
                cq, ck, cv, cg = 3088 + hu * 128, 4112 + hu * 128, 5136 + hu * 128, 6160 + hu * 128
                scol = 1024 + hu * 128
            tS = t_S[u]
            if full:
                wvg = wvg_b[0]
                twvg_r = list(t_wvgh)
            else:
                hsel = u % 2
                wvg = wvg_b[0][:, :, hsel * 256:(hsel + 1) * 256]
                twvg_r = [t_wvgh[hsel]]
            if full:
                if u == 0:
                    full_loads_qk(0)
                    full_loads_vg(0)
                if u + 1 < DBG["units"]:
                    full_loads_qk(u + 1)
            else:
                wload(wk_b[wb], wview(w_in, ck, 128), "wk%d" % wb, t_wk[wb])
                wload(wvg[:, :, 0:dv], wview(w_in, cv, dv), "wvg%d" % hsel, t_wvgh[hsel])
            if full:
                for half in range(2):
                    pa = next_pa()
                    proj_fm(wq_b[wb], t_wq[wb], aT, t_aT, half, pa)
                    sl = slice(half * 512, (half + 1) * 512)
                    if gla:
                        P.act(lambda e, pa=pa, sl=sl: e.activation(out=qf[:, sl], in_=psf[pa][:], func=AF.Copy,
                                                                  scale=128.0 ** -0.5),
                              reads=[t_psf[pa]], writes=[t_qf])
                    else:
                        silu_to(qf[:, sl], psf[pa][:], [t_psf[pa]], t_qf)
            for half in range(2):
                pa = next_pa()
                sl = slice(half * 512, (half + 1) * 512)
                proj_fm(wk_b[wb], t_wk[wb], aT, t_aT, half, pa)
                if gla:
                    copy_evac(kf[:, sl], psf[pa][:], [t_psf[pa]], [t_kf])
                    pz = next_pa()
                    P.pe(lambda e, pz=pz, sl=sl: e.matmul(psf[pz][:], lhsT=wup_sb[:, u * 128:(u + 1) * 128],
                                                         rhs=glrT[:, sl], start=True, stop=True),
                         reads=[t_par, t_glr], writes=[t_psf[pz]])
                    P.act(lambda e, pz=pz, sl=sl: e.activation(out=t1[:, sl], in_=psf[pz][:], func=AF.Exp,
                                                              scale=-1.0, bias=nbalpha[:, u:u + 1]),
                          reads=[t_psf[pz], t_par], writes=[t_t1])
                else:
                    sigmoid_to(t1[:, sl], psf[pa][:], [t_psf[pa]], t_t1)
            def chain_part():
                if gla:
                    for half in range(2):
                        P.act(lambda e, half=half: e.activation(out=t1[:, half * 512:(half + 1) * 512], in_=t1[:, half * 512:(half + 1) * 512],
                                                                func=AF.Ln, bias=1.0), reads=[t_t1], writes=[t_t1])
                else:
                    P.dve(lambda e: e.tensor_scalar(out=t2, in0=t1, scalar1=oml_sb[:, hu:hu + 1],
                                                    scalar2=lb_sb[:, hu:hu + 1], op0=ALU.mult, op1=ALU.add),
                          reads=[t_t1, t_par], writes=[t_t2])
                    P.dve(lambda e: e.tensor_scalar(out=kf, in0=t2, scalar1=-1.0, scalar2=1.0,
                                                    op0=ALU.mult, op1=ALU.add), reads=[t_t2], writes=[t_kf])
                    P.act(lambda e: e.activation(out=t1, in_=t2, func=AF.Ln), reads=[t_t2], writes=[t_t1])
                P.dve(lambda e: e.tensor_tensor_scan(out=t2, data0=scanmask, data1=t1, initial=0.0,
                                                     op0=ALU.mult, op1=ALU.add),
                      reads=[t_t1, t_const], writes=[t_t2])
                c3 = t2.rearrange("p (n j) -> p n j", j=128)
                cref = c3[:, :, 63:64]
                clast = c3[:, :, 127:128]
                cref2 = cref.rearrange("p n o -> p (n o)")
                clast2 = clast.rearrange("p n o -> p (n o)")
                P.dve(lambda e: e.tensor_tensor(out=t1.rearrange("p (n j) -> p n j", j=128), in0=c3,
                                                in1=cref.to_broadcast([128, NT, 128]), op=ALU.subtract),
                      reads=[t_t2], writes=[t_t1])
                if full:
                    P.act(lambda e: e.activation(out=sm[:, 0, :], in_=cref2, func=AF.Exp, scale=sc),
                          reads=[t_t2], writes=[t_sm])
                P.dve(lambda e: e.tensor_tensor(out=sm[:, 3, :], in0=clast2, in1=cref2, op=ALU.subtract),
                      reads=[t_t2], writes=[t_sm])
                P.act(lambda e: e.activation(out=sm[:, 1, :], in_=sm[:, 3, :], func=AF.Exp, scale=sc),
                      reads=[t_sm], writes=[t_sm])
                P.act(lambda e: e.activation(out=sm[:, 2, :], in_=clast2, func=AF.Exp, scale=sc),
                      reads=[t_t2], writes=[t_sm])
                P.act(lambda e: e.activation(out=t3, in_=t1, func=AF.Exp, scale=-sc), reads=[t_t1], writes=[t_t3])
                P.dve(lambda e: e.tensor_tensor(out=ktl, in0=kf, in1=t3, op=ALU.mult),
                      reads=[t_kf, t_t3], writes=[t_ktl])
                P.pool(lambda e: e.tensor_tensor(out=khT.rearrange("p (n j) -> p n j", j=128),
                                                 in0=ktl.rearrange("p (n j) -> p n j", j=128),
                                                 in1=sm[:, 1, :].unsqueeze(2).to_broadcast([128, NT, 128]), op=ALU.mult),
                       reads=[t_ktl, t_sm], writes=[t_khT])
                if full:
                    P.act(lambda e: e.activation(out=t3, in_=t1, func=AF.Exp, scale=sc), reads=[t_t1], writes=[t_t3])
                    P.dve(lambda e: e.tensor_tensor(out=qt, in0=qf, in1=t3, op=ALU.mult),
                          reads=[t_qf, t_t3], writes=[t_qt])
                    P.pool(lambda e: e.tensor_tensor(out=qs.rearrange("p (n j) -> p n j", j=128),
                                                     in0=qt.rearrange("p (n j) -> p n j", j=128),
                                                     in1=sm[:, 0, :].unsqueeze(2).to_broadcast([128, NT, 128]), op=ALU.mult),
                           reads=[t_qt, t_sm], writes=[t_qs])

            def vg_part():
                gb = gla_gbc if gla else hgrn_gbc
                for i in range(NT):
                    pa = next_pa()
                    for kt in range(KT):
                        P.pe(lambda e, kt=kt, i=i, pa=pa: e.matmul(psf[pa][:, 0:nv], lhsT=aT[:, kt, i * 128:(i + 1) * 128],
                                                                   rhs=wvg[:, kt, 0:nv],
                                                                   start=(kt == 0), stop=(kt == KT - 1)),
                             reads=twvg_r + [t_aT[i]], writes=[t_psf[pa]])
                    P.act(lambda e, i=i, pa=pa: e.activation(out=v_tok[:, i, 0:dv], in_=psf[pa][:, 0:dv], func=AF.Copy),
                          reads=[t_psf[pa]], writes=[t_v])
                    if full:
                        silu_to(sg_tmp[:, 0:dv], psf[pa][:, dv:2 * dv], [t_psf[pa]], t_sgtmp)
                        P.pool(lambda e, i=i: e.tensor_tensor(out=sg_tok[:, i, 0:dv], in0=sg_tmp[:, 0:dv],
                                                              in1=gb[:, 0:dv], op=ALU.mult),
                               reads=[t_sgtmp, t_par], writes=[t_sg])

            if full:
                ops_a = capture(chain_part)
                ops_b = capture(vg_part)
                interleave(ops_a, ops_b)
            else:
                chain_part()
                vg_part()
            if full and u + 1 < DBG["units"]:
                full_loads_vg(u + 1)
            if full:
                emit_conv(2)
            pb = next_pb()
            for n in range(NT):
                P.pe(lambda e, n=n: e.transpose(out=psb(pb)[:, n * 128:(n + 1) * 128],
                                               in_=khT[:, n * 128:(n + 1) * 128], identity=ident_bf),
                     reads=[t_khT, t_const], writes=[t_psf[pb]])
            copy_evac(khat, psb(pb).rearrange("p (n c) -> p n c", c=128), [t_psf[pb]], [t_khat])
            if full:
                for g2 in range(2):
                    for n4 in range(4):
                        n = g2 * 4 + n4
                        P.pe(lambda e, n=n, n4=n4, g2=g2: e.matmul(psf[2 + g2][:, n4 * 128 + 64:(n4 + 1) * 128],
                                                                  lhsT=ktl[:, n * 128:(n + 1) * 128],
                                                                  rhs=qt[:, n * 128 + 64:(n + 1) * 128], start=True, stop=True),
                             reads=[t_ktl, t_qt], writes=[t_psf[2 + g2]])
                        P.pe(lambda e, n=n, n4=n4, g2=g2: e.matmul(psf[2 + g2][0:64, n4 * 128:n4 * 128 + 64],
                                                                  lhsT=ktl[:, n * 128:n * 128 + 64],
                                                                  rhs=qt[:, n * 128:n * 128 + 64], start=True, stop=True),
                             reads=[t_ktl, t_qt], writes=[t_psf[2 + g2]])
                    pv = psf[2 + g2][:].rearrange("p (n c) -> p n c", c=128)
                    P.dve(lambda e, g2=g2, pv=pv: e.tensor_tensor(out=AT_sb[:, g2 * 4:(g2 + 1) * 4, 64:128], in0=pv[:, :, 64:128],
                                                                 in1=maskT[:, 64:128].unsqueeze(1).to_broadcast([128, 4, 64]),
                                                                 op=ALU.mult),
                          reads=[t_psf[2 + g2], t_const], writes=[t_AT])
                    P.dve(lambda e, g2=g2, pv=pv: e.tensor_tensor(out=AT_sb[0:64, g2 * 4:(g2 + 1) * 4, 0:64], in0=pv[0:64, :, 0:64],
                                                                 in1=maskT[0:64, 0:64].unsqueeze(1).to_broadcast([64, 4, 64]),
                                                                 op=ALU.mult),
                          reads=[t_psf[2 + g2], t_const], writes=[t_AT])
            Su = S_all[:, scol:scol + dv]
            St = S_tmp[:, 0:dv]
            if full:
                P.act(lambda e: e.activation(out=S_bf[:, 0, 0:dv], in_=Su, func=AF.Copy), reads=[tS], writes=[t_Sbf])
            per_bank = 512 // dv
            kvp = [4, 5, 2, 3] if full else [3, 4, 5]
            for n in range(NT):
                bank = kvp[(n // per_bank) % len(kvp)]
                off = (n % per_bank) * dv
                P.pe(lambda e, n=n, bank=bank, off=off: e.matmul(psf[bank][:, off:off + dv], lhsT=khat[:, n, :],
                                                                rhs=v_tok[:, n, 0:dv], start=True, stop=True),
                     reads=[t_khat, t_v], writes=[t_psf[bank]])
            for n in range(NT):
                bank = kvp[(n // per_bank) % len(kvp)]
                off = (n % per_bank) * dv
                if full:
                    src, dst = (Su, St) if n % 2 == 0 else (St, Su)
                    tsrc, tdst = (tS, t_Stmp) if n % 2 == 0 else (t_Stmp, tS)
                else:
                    src, dst, tsrc, tdst = Su, Su, tS, tS
                P.dve(lambda e, n=n, bank=bank, off=off, src=src, dst=dst: e.scalar_tensor_tensor(
                    out=dst, in0=src, scalar=sm[:, 2, n:n + 1], in1=psf[bank][:, off:off + dv],
                    op0=ALU.mult, op1=ALU.add), reads=[tsrc, t_sm, t_psf[bank]], writes=[tdst])
                if full and n < NT - 1:
                    P.act(lambda e, n=n, dst=dst: e.activation(out=S_bf[:, n + 1, 0:dv], in_=dst, func=AF.Copy),
                          reads=[tdst], writes=[t_Sbf])
            if full:
                for n in range(NT):
                    pa = next_pa()
                    P.pe(lambda e, n=n, pa=pa: e.matmul(psf[pa][:, 0:dv], lhsT=AT_sb[:, n, :], rhs=v_tok[:, n, 0:dv],
                                                        start=True, stop=False),
                         reads=[t_AT, t_v], writes=[t_psf[pa]])
                    P.pe(lambda e, n=n, pa=pa: e.matmul(psf[pa][:, 0:dv], lhsT=qs[:, n * 128:(n + 1) * 128],
                                                        rhs=S_bf[:, n, 0:dv], start=False, stop=True),
                         reads=[t_qs, t_Sbf], writes=[t_psf[pa]])
                    P.act(lambda e, n=n, pa=pa: e.activation(out=o_all[:, n, 0:dv], in_=psf[pa][:, 0:dv], func=AF.Copy),
                          reads=[t_psf[pa]], writes=[t_osb[n]])
                    P.dve(lambda e, n=n: e.scalar_tensor_tensor(out=junk[:, 0:dv], in0=o_all[:, n, 0:dv], scalar=1.0, in1=o_all[:, n, 0:dv],
                                                                op0=ALU.mult, op1=ALU.mult, accum_out=ost[:, 0, n:n + 1]),
                          reads=[t_osb[n]], writes=[t_junk, t_ost])
                rstd_of(ost[:, 3, :], ost[:, 0, :], ost[:, 1, :], dv, t_ost)
                for n in range(NT):
                    P.dve(lambda e, n=n: e.scalar_tensor_tensor(
                        out=o_tok[:, n, scol:scol + dv], in0=o_all[:, n, 0:dv], scalar=ost[:, 3, n:n + 1],
                        in1=sg_tok[:, n, 0:dv], op0=ALU.mult, op1=ALU.mult),
                        reads=[t_osb[n], t_ost, t_sg], writes=[t_otok[n]])

        o_flat = o_tok.rearrange("p a b -> p (a b)")
        KF = [kf, o_flat[:, 0:2 * T].bitcast(F32)]
        T1 = [t1, o_flat[:, 2 * T:4 * T].bitcast(F32)]
        VT = [v_tok, o_flat[:, 4 * T:4 * T + NT * 256].rearrange("p (a b) -> p a b", b=256)]
        t_KF = [t_kf, P.tok("kf2")]
        t_T1 = [t_t1, P.tok("t1b")]
        t_VT = [t_v, P.tok("v2")]

        def unit_cfg(u):
            gla = u < 4
            dv = 256 if gla else 128
            sc = (-1.0 / 16.0) if gla else 1.0
            if gla:
                ck, cv, scol = 512 + u * 128, 1024 + u * 256, u * 256
            else:
                hu = u - 4
                ck, cv, scol = 4112 + hu * 128, 5136 + hu * 128, 1024 + hu * 128
            return gla, dv, sc, ck, cv, scol

        def state_A_load(u):
            gla, dv, sc, ck, cv, scol = unit_cfg(u)
            s_ = u % 2
            wvg = wvg_b[0][:, :, s_ * 256:(s_ + 1) * 256]
            wload(wk_b[s_], wview(w_in, ck, 128), "wk%d" % s_, t_wk[s_])
            wload(wvg[:, :, 0:dv], wview(w_in, cv, dv), "wvg%d" % s_, t_wvgh[s_])

        def state_A_pe(u):
            gla, dv, sc, ck, cv, scol = unit_cfg(u)
            s_ = u % 2
            wvg = wvg_b[0][:, :, s_ * 256:(s_ + 1) * 256]
            for half in range(2):
                proj_fm(wk_b[s_], t_wk[s_], aT, t_aT, half, half)
            per_bank = 512 // dv
            for i in range(NT):
                bank = 2 + i // per_bank
                off = (i % per_bank) * dv
                for kt in range(KT):
                    P.pe(lambda e, kt=kt, i=i, bank=bank, off=off: e.matmul(psf[bank][:, off:off + dv], lhsT=aT[:, kt, i * 128:(i + 1) * 128],
                                                                            rhs=wvg[:, kt, 0:dv], start=(kt == 0), stop=(kt == KT - 1)),
                         reads=[t_wvgh[s_], t_aT[i]], writes=[t_psf[bank]])

        def state_A_kevac(u):
            gla, dv, sc, ck, cv, scol = unit_cfg(u)
            s_ = u % 2
            for half in range(2):
                pa = half
                sl = slice(half * 512, (half + 1) * 512)
                if gla:
                    copy_evac(KF[s_][:, sl], psf[pa][:], [t_psf[pa]], [t_KF[s_]])
                    P.pe(lambda e, pa=pa, sl=sl: e.matmul(psf[pa][:], lhsT=wup_sb[:, u * 128:(u + 1) * 128],
                                                         rhs=glrT[:, sl], start=True, stop=True),
                         reads=[t_par, t_glr], writes=[t_psf[pa]])
                    P.act(lambda e, pa=pa, sl=sl: e.activation(out=T1[s_][:, sl], in_=psf[pa][:], func=AF.Exp,
                                                              scale=-1.0, bias=nbalpha[:, u:u + 1]),
                          reads=[t_psf[pa], t_par], writes=[t_T1[s_]])
                else:
                    sigmoid_to(T1[s_][:, sl], psf[pa][:], [t_psf[pa]], t_T1[s_])

        def state_A_evac(u):
            gla, dv, sc, ck, cv, scol = unit_cfg(u)
            s_ = u % 2
            per_bank = 512 // dv
            for b in range(NT // per_bank):
                bank = 2 + b
                copy_evac(VT[s_][:, b * per_bank:(b + 1) * per_bank, 0:dv],
                          psf[bank][:].rearrange("p (a b) -> p a b", b=dv), [t_psf[bank]], [t_VT[s_]])

        def state_B_chain(u):
            gla, dv, sc, ck, cv, scol = unit_cfg(u)
            s_ = u % 2
            kf_, t1_, tkf_, tt1_ = KF[s_], T1[s_], t_KF[s_], t_T1[s_]
            if gla:
                for half in range(2):
                    P.act(lambda e, half=half: e.activation(out=t1_[:, half * 512:(half + 1) * 512], in_=t1_[:, half * 512:(half + 1) * 512],
                                                            func=AF.Ln, bias=1.0), reads=[tt1_], writes=[tt1_])
            else:
                hu = u - 4
                P.dve(lambda e: e.tensor_scalar(out=t2, in0=t1_, scalar1=oml_sb[:, hu:hu + 1],
                                                scalar2=lb_sb[:, hu:hu + 1], op0=ALU.mult, op1=ALU.add),
                      reads=[tt1_, t_par], writes=[t_t2])
                P.dve(lambda e: e.tensor_scalar(out=kf_, in0=t2, scalar1=-1.0, scalar2=1.0,
                                                op0=ALU.mult, op1=ALU.add), reads=[t_t2], writes=[tkf_])
                P.act(lambda e: e.activation(out=t1_, in_=t2, func=AF.Ln), reads=[t_t2], writes=[tt1_])
            P.dve(lambda e: e.tensor_tensor_scan(out=t2, data0=scanmask, data1=t1_, initial=0.0,
                                                 op0=ALU.mult, op1=ALU.add),
                  reads=[tt1_, t_const], writes=[t_t2])
            c3 = t2.rearrange("p (n j) -> p n j", j=128)
            cref = c3[:, :, 63:64]
            clast = c3[:, :, 127:128]
            cref2 = cref.rearrange("p n o -> p (n o)")
            clast2 = clast.rearrange("p n o -> p (n o)")
            P.dve(lambda e: e.tensor_tensor(out=t1_.rearrange("p (n j) -> p n j", j=128), in0=c3,
                                            in1=cref.to_broadcast([128, NT, 128]), op=ALU.subtract),
                  reads=[t_t2], writes=[tt1_])
            P.dve(lambda e: e.tensor_tensor(out=sm[:, 3, :], in0=clast2, in1=cref2, op=ALU.subtract),
                  reads=[t_t2], writes=[t_sm])
            P.dve(lambda e: e.tensor_tensor_scan(out=sm2[:, 0, :], data0=scanmask[:, 1:1 + NT], data1=clast2, initial=0.0,
                                                 op0=ALU.mult, op1=ALU.add), reads=[t_t2, t_const], writes=[t_sm])
            P.dve(lambda e: e.tensor_scalar(out=sm2[:, 1, :], in0=sm2[:, 0, :], scalar1=-1.0, scalar2=sm2[:, 0, NT - 1:NT],
                                            op0=ALU.mult, op1=ALU.add), reads=[t_sm], writes=[t_sm])
            P.dve(lambda e: e.tensor_tensor(out=sm[:, 3, :], in0=sm[:, 3, :], in1=sm2[:, 1, :], op=ALU.add),
                  reads=[t_sm], writes=[t_sm])
            P.act(lambda e: e.activation(out=sm[:, 1, :], in_=sm[:, 3, :], func=AF.Exp, scale=sc),
                  reads=[t_sm], writes=[t_sm])
            P.act(lambda e: e.activation(out=sm[:, 2, 0:1], in_=sm2[:, 0, NT - 1:NT], func=AF.Exp, scale=sc),
                  reads=[t_sm], writes=[t_sm])
            P.act(lambda e: e.activation(out=t3, in_=t1_, func=AF.Exp, scale=-sc), reads=[tt1_], writes=[t_t3])
            P.dve(lambda e: e.tensor_tensor(out=ktl, in0=kf_, in1=t3, op=ALU.mult),
                  reads=[tkf_, t_t3], writes=[t_ktl])
            P.pool(lambda e: e.tensor_tensor(out=khT.rearrange("p (n j) -> p n j", j=128),
                                             in0=ktl.rearrange("p (n j) -> p n j", j=128),
                                             in1=sm[:, 1, :].unsqueeze(2).to_broadcast([128, NT, 128]), op=ALU.mult),
                   reads=[t_ktl, t_sm], writes=[t_khT])

        def state_B_pe(u):
            gla, dv, sc, ck, cv, scol = unit_cfg(u)
            s_ = u % 2
            tS = t_S[u]
            pb = 7
            for n in range(NT):
                P.pe(lambda e, n=n: e.transpose(out=psb(pb)[:, n * 128:(n + 1) * 128],
                                               in_=khT[:, n * 128:(n + 1) * 128], identity=ident_bf),
                     reads=[t_khT, t_const], writes=[t_psf[pb]])
            copy_evac(khat, psb(pb).rearrange("p (n c) -> p n c", c=128), [t_psf[pb]], [t_khat])
            Su = S_all[:, scol:scol + dv]
            for n in range(NT):
                P.pe(lambda e, n=n: e.matmul(psf[6][:, 0:dv], lhsT=khat[:, n, :], rhs=VT[s_][:, n, 0:dv],
                                            start=(n == 0), stop=(n == NT - 1)),
                     reads=[t_khat, t_VT[s_]], writes=[t_psf[6]])
            P.dve(lambda e: e.scalar_tensor_tensor(out=Su, in0=Su, scalar=sm[:, 2, 0:1], in1=psf[6][:, 0:dv],
                                                   op0=ALU.mult, op1=ALU.add), reads=[tS, t_sm, t_psf[6]], writes=[tS])

        def mixer_segment_state():
            wload(wlr, wview(w_in, 3072, 16), "wlr", t_wlr)
            for half in range(2):
                pa = half
                proj_fm(wlr, t_wlr, aT, t_aT, half, pa, M=16)
                copy_evac(glrT[:, half * 512:(half + 1) * 512], psf[pa][0:16, :], [t_psf[pa]], [t_glr])
            nu = DBG["units"]
            if nu == 0:
                return
            state_A_load(0)
            if nu > 1:
                state_A_load(1)
            state_A_pe(0)
            state_A_kevac(0)
            state_A_evac(0)
            for u in range(nu):
                if u + 2 < nu:
                    state_A_load(u + 2)
                if u + 1 < nu:
                    state_A_pe(u + 1)
                state_B_chain(u)
                emit_conv(1)
                if u + 1 < nu:
                    state_A_kevac(u + 1)
                    state_A_evac(u + 1)
                state_B_pe(u)

        def mixer_segment(full):
            if not full and "nopipe" not in DBG["skip"]:
                mixer_segment_state()
                return
            rr["pool"] = [0, 1] if full else [0, 1, 2]
            rr["pa"] = 0
            wload(wlr, wview(w_in, 3072, 16), "wlr", t_wlr)
            for half in range(2):
                pa = next_pa()
                proj_fm(wlr, t_wlr, aT, t_aT, half, pa, M=16)
                copy_evac(glrT[:, half * 512:(half + 1) * 512], psf[pa][0:16, :], [t_psf[pa]], [t_glr])
            for u in range(DBG["units"]):
                mixer_unit(u, full and DBG["full"])

        load_gbc(g_mix)
        for seg in range(DBG["seg0"], NPREV + 1):
            src = x_prev if seg < NPREV else x_own
            r0 = seg * T if seg < NPREV else 0
            for i in range(NT):
                P.dma("sp", lambda e, i=i, src=src, r0=r0: e.dma_start(out=xs[i % 2], in_=src[r0 + i * 128: r0 + (i + 1) * 128, :]),
                      "xs%d" % (i % 2), writes=[t_xs[i % 2]])
                sumsq(xs[i % 2], t_xs[i % 2], stat[:, 0, i:i + 1], D)
                rstd_of(stat[:, 3, i:i + 1], stat[:, 0, i:i + 1], stat[:, 1, i:i + 1], D, t_stat)
                P.dve(lambda e, i=i: e.scalar_tensor_tensor(out=a_tok, in0=xs[i % 2], scalar=stat[:, 3, i:i + 1],
                                                            in1=gbc, op0=ALU.mult, op1=ALU.mult),
                      reads=[t_xs[i % 2], t_stat, t_gbc], writes=[t_atok])
                transpose_tile(a_tok, t_atok, aT, t_aT[i], slice(i * 128, (i + 1) * 128))
            if seg == NPREV and seg > DBG["seg0"]:
                P.barrier()
            mixer_segment(full=(seg == NPREV))

        if "barrier" not in DBG["skip"]:
            P.barrier()
        A.top = M_AFTER_OTOK
        h = A.alloc([128, NT, D])
        t_h = P.toks("h", NT)
        wbig = [A.alloc([128, KT, 512], BF16) for _ in range(2)]
        t_wbig = P.toks("wbig", 2)
        M_AFTER_WBIG = A.top
        for i in range(NT):
            P.dma("sp", lambda e, i=i: e.dma_start(out=h[:, i, :], in_=x_own[i * 128:(i + 1) * 128, :]), "h%d" % i,
                  writes=[t_h[i]])

        def proj_residual(w, srcT, src_toks):
            for c in range(4):
                wb = c % 2
                wload(wbig[wb], wview(w, c * 512, 512), "wbig%d" % wb, t_wbig[wb])
                emit_conv(1)
                for i in range(NT):
                    pa = next_pa()
                    for kt in range(KT):
                        P.pe(lambda e, kt=kt, i=i, pa=pa, wb=wb: e.matmul(psf[pa][:], lhsT=srcT[:, kt, i * 128:(i + 1) * 128],
                                                                          rhs=wbig[wb][:, kt, :],
                                                                          start=(kt == 0), stop=(kt == KT - 1)),
                             reads=[t_wbig[wb], src_toks[i]], writes=[t_psf[pa]])
                    P.dve(lambda e, i=i, pa=pa, c=c: e.tensor_tensor(out=h[:, i, c * 512:(c + 1) * 512],
                                                                    in0=psf[pa][:], in1=h[:, i, c * 512:(c + 1) * 512],
                                                                    op=ALU.add),
                          reads=[t_psf[pa], t_h[i]], writes=[t_h[i]])

        if DBG["proj"]:
            for i in range(NT):
                transpose_tile(o_tok[:, i, :], t_otok[i], aT, t_aT[i], slice(i * 128, (i + 1) * 128))
            proj_residual(w_mo, aT, t_aT)

        if stage >= 2:
            P.barrier()
            xa_base = M_PERSIST
            A.top = xa_base
            mem_f = A.alloc([128, 2, D])
            memT = A.alloc([128, KT, 256], BF16)
            assert A.top <= M_AFTER_OTOK
            A.top = M_AFTER_WBIG
            kT = A.alloc([128, KT, 256], BF16)
            v_mem = A.alloc([128, 2, D], BF16)
            p_sb = A.alloc([128, 4, 256], BF16)
            pT_sb = A.alloc([128, 8, 128], BF16)
            xst = A.alloc([128, 16])
            t_kT, t_vmem, t_p, t_pT, t_xst = P.toks("xa", 5)
            t_memf = P.toks("memf", 2)
            t_memT = P.toks("memT", 2)
            for i in range(2):
                P.dma("sp", lambda e, i=i: e.dma_start(out=mem_f[:, i, :], in_=mem[i * 128:(i + 1) * 128, :]), "memf%d" % i,
                      writes=[t_memf[i]])
            load_gbc(g_mem)
            norm_to_T(lambda i: mem_f[:, i, :], t_memf, 2, memT, t_memT)
            for c in range(8):
                wb = c % 2
                wload(wbig[wb], wview(w_xkv, c * 512, 512), "wbig%d" % wb, t_wbig[wb])
                if c < 4:
                    for j in range(4):
                        pa = next_pa()
                        for kt in range(KT):
                            P.pe(lambda e, kt=kt, j=j, pa=pa, wb=wb: e.matmul(psf[pa][:, 0:256], lhsT=wbig[wb][:, kt, j * 128:(j + 1) * 128],
                                                                              rhs=memT[:, kt, :], start=(kt == 0), stop=(kt == KT - 1)),
                                 reads=[t_wbig[wb]] + t_memT, writes=[t_psf[pa]])
                        copy_evac(kT[:, c * 4 + j, :], psf[pa][:, 0:256], [t_psf[pa]], [t_kT])
                else:
                    for mt in range(2):
                        pa = next_pa()
                        for kt in range(KT):
                            P.pe(lambda e, kt=kt, mt=mt, pa=pa, wb=wb: e.matmul(psf[pa][:], lhsT=memT[:, kt, mt * 128:(mt + 1) * 128],
                                                                                rhs=wbig[wb][:, kt, :], start=(kt == 0), stop=(kt == KT - 1)),
                                 reads=[t_wbig[wb], t_memT[mt]], writes=[t_psf[pa]])
                        copy_evac(v_mem[:, mt, (c - 4) * 512:(c - 3) * 512], psf[pa][:], [t_psf[pa]], [t_vmem])
            P.barrier()
            A.top = xa_base
            qT = A.alloc([128, KT, T], BF16)
            t_qT = P.toks("qT", 2)
            assert A.top <= M_AFTER_OTOK
            load_gbc(g_xa)
            norm_to_T(lambda i: h[:, i, :], t_h, NT, aT, t_aT)
            for c in range(4):
                wb = c % 2
                wload(wbig[wb], wview(w_xq, c * 512, 512), "wbig%d" % wb, t_wbig[wb])
                emit_conv(1)
                for j in range(4):
                    for half in range(2):
                        pa = next_pa()
                        proj_fm(wbig[wb][:, :, j * 128:(j + 1) * 128], t_wbig[wb], aT, t_aT, half, pa)
                        evac(lambda e, pa=pa, c=c, j=j, half=half: e.activation(out=qT[:, c * 4 + j, half * 512:(half + 1) * 512],
                                                                                in_=psf[pa][:], func=AF.Copy, scale=512.0 ** -0.5),
                             lambda e, pa=pa, c=c, j=j, half=half: e.tensor_scalar(out=qT[:, c * 4 + j, half * 512:(half + 1) * 512],
                                                                                   in0=psf[pa][:], scalar1=512.0 ** -0.5, scalar2=None,
                                                                                   op0=ALU.mult),
                             [t_psf[pa]], [t_qT[half]])
            for i in range(NT):
                tsl = slice(i * 128, (i + 1) * 128)
                for pr in range(2):
                    bank = 2 + pr
                    for hh in range(2):
                        hd = pr * 2 + hh
                        for j in range(4):
                            P.pe(lambda e, bank=bank, hh=hh, hd=hd, j=j, tsl=tsl: e.matmul(psf[bank][:, hh * 256:(hh + 1) * 256],
                                                                                  lhsT=qT[:, hd * 4 + j, tsl], rhs=kT[:, hd * 4 + j, :],
                                                                                  start=(j == 0), stop=(j == 3)),
                                 reads=[t_qT[i // 4], t_kT], writes=[t_psf[bank]])
                    P.dve(lambda e, bank=bank, pr=pr: e.tensor_reduce(out=xst[:, pr * 2:pr * 2 + 2],
                                                                     in_=psf[bank][:].rearrange("p (a b) -> p a b", b=256),
                                                                     axis=AX.X, op=ALU.max),
                          reads=[t_psf[bank]], writes=[t_xst])
                    P.dve(lambda e, pr=pr: e.tensor_scalar(out=xst[:, 4 + pr * 2:6 + pr * 2], in0=xst[:, pr * 2:pr * 2 + 2],
                                                           scalar1=-1.0, scalar2=None, op0=ALU.mult),
                          reads=[t_xst], writes=[t_xst])
                    for hh in range(2):
                        hd = pr * 2 + hh
                        P.act(lambda e, bank=bank, hh=hh, hd=hd: e.activation(out=p_sb[:, hd, :], in_=psf[bank][:, hh * 256:(hh + 1) * 256],
                                                                             func=AF.Exp, bias=xst[:, 4 + hd:5 + hd],
                                                                             accum_out=xst[:, 8 + hd:9 + hd]),
                              reads=[t_psf[bank], t_xst], writes=[t_p, t_xst])
                P.dve(lambda e: e.reciprocal(out=xst[:, 12:16], in_=xst[:, 8:12]), reads=[t_xst], writes=[t_xst])
                pb = next_pb()
                for hd in range(4):
                    for mt in range(2):
                        P.pe(lambda e, hd=hd, mt=mt, pb=pb: e.transpose(out=psb(pb)[:, (hd * 2 + mt) * 128:(hd * 2 + mt + 1) * 128],
                                                                in_=p_sb[:, hd, mt * 128:(mt + 1) * 128], identity=ident_bf),
                             reads=[t_p, t_const], writes=[t_psf[pb]])
                copy_evac(pT_sb, psb(pb).rearrange("p (n c) -> p n c", c=128), [t_psf[pb]], [t_pT])
                for hd in range(4):
                    pa = next_pa()
                    for mt in range(2):
                        P.pe(lambda e, hd=hd, mt=mt, pa=pa: e.matmul(psf[pa][:], lhsT=pT_sb[:, hd * 2 + mt, :],
                                                                     rhs=v_mem[:, mt, hd * 512:(hd + 1) * 512],
                                                                     start=(mt == 0), stop=(mt == 1)),
                             reads=[t_pT, t_vmem], writes=[t_psf[pa]])
                    evac(lambda e, hd=hd, pa=pa: e.activation(out=a_tok[:, hd * 512:(hd + 1) * 512], in_=psf[pa][:], func=AF.Copy,
                                                              scale=xst[:, 12 + hd:13 + hd]),
                         lambda e, hd=hd, pa=pa: e.tensor_scalar(out=a_tok[:, hd * 512:(hd + 1) * 512], in0=psf[pa][:],
                                                                 scalar1=xst[:, 12 + hd:13 + hd], scalar2=None, op0=ALU.mult),
                         [t_psf[pa], t_xst], [t_atok])
                transpose_tile(a_tok, t_atok, aT, t_aT[i], tsl)
            proj_residual(w_xo, aT, t_aT)

        if stage >= 3:
            P.barrier()
            A.top = M_PERSIST - KT * T * 2
            moe_lo = A.top
            a3_tok = A.alloc([128, NT, D], BF16)
            t_a3 = P.toks("a3", NT)
            G_all = A.alloc([128, NT, 32])
            ind_all = A.alloc([128, NT, 32])
            ind_bf = A.alloc([128, NT, 32], BF16)
            rank_all = A.alloc([128, NT, 32])
            GT = A.alloc([32, T])
            rankT = A.alloc([32, T])
            moe_ov = A.top
            wr_sb = A.alloc([128, KT, 36])
            br_bc = A.alloc([128, 36])
            lg = A.alloc([128, 36])
            rt = A.alloc([128, 64])
            ml = A.alloc([128, 32])
            top8 = A.alloc([128, 8])
            a3fT = A.alloc([128, 4, 128])
            a3f = ja.bitcast(F32)
            A.top = moe_ov
            esel = A.alloc([32, 128])
            Sel = A.alloc([128, NT, 128], BF16)
            gbc_sb = A.alloc([128, T])
            GselT = A.alloc([128, T], BF16)
            xeT = A.alloc([128, KT, 128], BF16)
            hb_tmp = A.alloc([128, 512])
            hb = A.alloc([128, 1024], BF16)
            hbT = A.alloc([128, 8, 128], BF16)
            Y_sb = A.alloc([128, 512], BF16)
            assert A.top <= M_AFTER_OTOK, ("moe work region overflow", A.top, M_AFTER_OTOK)
            A.top = M_AFTER_OTOK + NT * D * 4
            NSLOT = (ARENA_BYTES - A.top) // (KT * 512 * 2)
            slots = [A.alloc([128, KT, 512], BF16) for _ in range(NSLOT)]
            t_slot = P.toks("slot", NSLOT)
            (t_G, t_ind, t_rank, t_GT, t_rankT, t_esel, t_wr, t_lg, t_rt, t_ml, t_top8, t_a3f_, t_a3fT, t_Sel,
             t_gbcsb, t_GselT, t_xeT, t_hbtmp, t_hb, t_hbT, t_Y) = P.toks("moe", 21)
            t_a3f = t_junk

            emit_conv(len(conv_list))
            P.dma("sp", lambda e: e.dma_start(out=wr_sb[:, :, 0:4], in_=w_rg.rearrange("(kt p) n -> p kt n", p=128)), "wr", writes=[t_wr])
            P.dma("sp", lambda e: e.dma_start(out=wr_sb[:, :, 4:36], in_=w_re.rearrange("(kt p) n -> p kt n", p=128)), "wr", writes=[t_wr])
            P.dma("sp", lambda e: e.dma_start(out=br_bc[:, 0:4], in_=b_rg.partition_broadcast(128)), "wr", writes=[t_wr])
            P.dma("sp", lambda e: e.dma_start(out=br_bc[:, 4:36], in_=b_re.partition_broadcast(128)), "wr", writes=[t_wr])
            load_gbc(g_ffn)
            rms_stats(lambda i: h[:, i, :], t_h, NT)
            for i in range(NT):
                P.dve(lambda e, i=i: e.scalar_tensor_tensor(out=a3f, in0=h[:, i, :], scalar=stat[:, 3, i:i + 1],
                                                            in1=gbc, op0=ALU.mult, op1=ALU.mult),
                      reads=[t_h[i], t_stat, t_gbc], writes=[t_a3f, t_atok])
                P.act(lambda e, i=i: e.activation(out=a3_tok[:, i, :], in_=a3f, func=AF.Copy), reads=[t_a3f], writes=[t_a3[i]])
                for q4 in range(4):
                    bank = 2 + (q4 % 2)
                    for j in range(4):
                        kt = q4 * 4 + j
                        P.pe(lambda e, bank=bank, j=j, kt=kt: e.transpose(out=psf[bank][:, j * 128:(j + 1) * 128],
                                                                         in_=a3f[:, kt * 128:(kt + 1) * 128], identity=ident_f),
                             reads=[t_a3f, t_const], writes=[t_psf[bank]])
                    copy_evac(a3fT, psf[bank][:].rearrange("p (j c) -> p j c", c=128), [t_psf[bank]], [t_a3fT])
                    for j in range(4):
                        kt = q4 * 4 + j
                        P.pe(lambda e, j=j, kt=kt: e.matmul(psf[4][:, 0:36], lhsT=a3fT[:, j, :], rhs=wr_sb[:, kt, :],
                                                           start=(kt == 0), stop=(kt == KT - 1)),
                             reads=[t_a3fT, t_wr], writes=[t_psf[4]])
                P.dve(lambda e: e.tensor_tensor(out=lg, in0=psf[4][:, 0:36], in1=br_bc, op=ALU.add),
                      reads=[t_psf[4], t_wr], writes=[t_lg])
                P.dve(lambda e: e.tensor_reduce(out=rt[:, 0:1], in_=lg[:, 0:4], axis=AX.X, op=ALU.max), reads=[t_lg], writes=[t_rt])
                P.dve(lambda e: e.tensor_scalar(out=rt[:, 1:2], in0=rt[:, 0:1], scalar1=-1.0, scalar2=None, op0=ALU.mult),
                      reads=[t_rt], writes=[t_rt])
                P.act(lambda e: e.activation(out=rt[:, 4:8], in_=lg[:, 0:4], func=AF.Exp, bias=rt[:, 1:2], accum_out=rt[:, 2:3]),
                      reads=[t_lg, t_rt], writes=[t_rt])
                P.dve(lambda e: e.reciprocal(out=rt[:, 3:4], in_=rt[:, 2:3]), reads=[t_rt], writes=[t_rt])
                P.dve(lambda e: e.tensor_scalar(out=rt[:, 8:12], in0=lg[:, 0:4], scalar1=rt[:, 0:1], scalar2=None, op0=ALU.is_equal),
                      reads=[t_lg, t_rt], writes=[t_rt])
                P.dve(lambda e: e.tensor_scalar(out=rt[:, 12:16], in0=rt[:, 8:12], scalar1=-1.0, scalar2=1e30, op0=ALU.add, op1=ALU.mult),
                      reads=[t_rt], writes=[t_rt])
                P.dve(lambda e: e.tensor_tensor(out=ml.rearrange("p (g k) -> p g k", k=8),
                                                in0=lg[:, 4:36].rearrange("p (g k) -> p g k", k=8),
                                                in1=rt[:, 8:12].unsqueeze(2).to_broadcast([128, 4, 8]), op=ALU.mult),
                      reads=[t_lg, t_rt], writes=[t_ml])
                P.dve(lambda e: e.tensor_tensor(out=ml.rearrange("p (g k) -> p g k", k=8),
                                                in0=ml.rearrange("p (g k) -> p g k", k=8),
                                                in1=rt[:, 12:16].unsqueeze(2).to_broadcast([128, 4, 8]), op=ALU.add),
                      reads=[t_ml, t_rt], writes=[t_ml])
                P.dve(lambda e: e.max(out=top8, in_=ml), reads=[t_ml], writes=[t_top8])
                P.dve(lambda e: e.tensor_scalar(out=rt[:, 16:17], in0=top8[:, 0:1], scalar1=-1.0, scalar2=None, op0=ALU.mult),
                      reads=[t_top8], writes=[t_rt])
                P.act(lambda e: e.activation(out=rt[:, 17:18], in_=top8[:, 1:2], func=AF.Exp, bias=rt[:, 16:17]),
                      reads=[t_top8, t_rt], writes=[t_rt])
                P.dve(lambda e: e.tensor_scalar(out=rt[:, 18:19], in0=rt[:, 17:18], scalar1=1.0, scalar2=None, op0=ALU.add),
                      reads=[t_rt], writes=[t_rt])
                P.dve(lambda e: e.reciprocal(out=rt[:, 19:20], in_=rt[:, 18:19]), reads=[t_rt], writes=[t_rt])
                P.dve(lambda e: e.tensor_tensor(out=rt[:, 20:21], in0=rt[:, 19:20], in1=rt[:, 3:4], op=ALU.mult),
                      reads=[t_rt], writes=[t_rt])
                P.dve(lambda e: e.tensor_tensor(out=rt[:, 21:22], in0=rt[:, 20:21], in1=rt[:, 17:18], op=ALU.mult),
                      reads=[t_rt], writes=[t_rt])
                P.dve(lambda e, i=i: e.tensor_scalar(out=G_all[:, i, :], in0=ml, scalar1=top8[:, 0:1], scalar2=rt[:, 20:21],
                                                     op0=ALU.is_equal, op1=ALU.mult), reads=[t_ml, t_top8, t_rt], writes=[t_G])
                P.dve(lambda e: e.tensor_scalar(out=rt[:, 32:64], in0=ml, scalar1=top8[:, 1:2], scalar2=rt[:, 21:22],
                                                op0=ALU.is_equal, op1=ALU.mult), reads=[t_ml, t_top8, t_rt], writes=[t_rt])
                P.dve(lambda e, i=i: e.tensor_tensor(out=G_all[:, i, :], in0=G_all[:, i, :], in1=rt[:, 32:64], op=ALU.add),
                      reads=[t_G, t_rt], writes=[t_G])
                P.dve(lambda e, i=i: e.tensor_scalar(out=ind_all[:, i, :], in0=G_all[:, i, :], scalar1=0.0, scalar2=None, op0=ALU.is_gt),
                      reads=[t_G], writes=[t_ind])
                P.dve(lambda e, i=i: e.tensor_copy(out=ind_bf[:, i, :], in_=ind_all[:, i, :]), reads=[t_ind], writes=[t_ind])
            for i in range(NT):
                P.pe(lambda e, i=i: e.matmul(psf[5][:, 0:32], lhsT=ltri, rhs=ind_bf[:, i, :], start=True, stop=(i == 0)),
                     reads=[t_ind, t_const], writes=[t_psf[5]])
                for i2 in range(i):
                    P.pe(lambda e, i2=i2, i=i: e.matmul(psf[5][:, 0:32], lhsT=ones_bf, rhs=ind_bf[:, i2, :], start=False, stop=(i2 == i - 1)),
                         reads=[t_ind, t_const], writes=[t_psf[5]])
                copy_evac(rank_all[:, i, :], psf[5][:, 0:32], [t_psf[5]], [t_rank])
            for (src_all, src_t, dst, dst_t) in ((G_all, t_G, GT, t_GT), (rank_all, t_rank, rankT, t_rankT)):
                for half in range(2):
                    bank = 2 + half
                    for j in range(4):
                        i = half * 4 + j
                        P.pe(lambda e, bank=bank, j=j, i=i, src_all=src_all: e.transpose(out=psf[bank][0:32, j * 128:(j + 1) * 128],
                                                                                         in_=src_all[:, i, :], identity=ident_f),
                             reads=[src_t, t_const], writes=[t_psf[bank]])
                    copy_evac(dst[:, half * 512:(half + 1) * 512], psf[bank][0:32, :], [t_psf[bank]], [dst_t])

            P.barrier()
            slot_rr = {"i": 0}

            def next_slot():
                s_ = slot_rr["i"] % NSLOT
                slot_rr["i"] += 1
                return s_

            def eload(dst, w32, w16, c0, n, r0, rows, key, tok, conv):
                if conv:
                    src = w16[r0:r0 + rows, c0:c0 + n].rearrange("(kt p) n -> p kt n", p=128)
                    P.dma("sp", lambda e: e.dma_start(out=dst, in_=src), "H" + key, reads=[conv_state["last_tok"]], writes=[tok])
                else:
                    src = w32[r0:r0 + rows, c0:c0 + n].rearrange("(kt p) n -> p kt n", p=128)
                    P.dma("pool", lambda e: e.dma_start(out=dst, in_=src), key, writes=[tok])

            Sel_b = [Sel, gj[:, 0:NT * 128].rearrange("p (a b) -> p a b", b=128)]
            GselT_b = [GselT, gj[:, NT * 128:NT * 128 + T]]
            esel_b = [esel, gj[:, 2 * T:2 * T + 256].bitcast(F32)[0:32]]
            t_Sel_b = [t_Sel, P.tok("Sel2")]
            t_GselT_b = [t_GselT, P.tok("GselT2")]
            t_esel_b = [t_esel, P.tok("esel2")]

            def expert_prep(ex):
                s_ = ex % 2
                Sel_, GselT_, esel_ = Sel_b[s_], GselT_b[s_], esel_b[s_]
                for i in range(NT):
                    P.dve(lambda e, i=i: e.tensor_scalar(out=Sel_[:, i, :], in0=iota_row, scalar1=rank_all[:, i, ex:ex + 1],
                                                         scalar2=ind_all[:, i, ex:ex + 1], op0=ALU.is_equal, op1=ALU.mult),
                          reads=[t_rank, t_ind, t_const], writes=[t_Sel_b[s_]])
                P.pool(lambda e: e.tensor_copy(out=esel_, in_=ident_f[0:32, ex:ex + 1].to_broadcast([32, 128])),
                       reads=[t_const], writes=[t_esel_b[s_]])
                for half in range(2):
                    sl = slice(half * 512, (half + 1) * 512)
                    P.pe(lambda e, sl=sl: e.matmul(psf[4][:], lhsT=esel_, rhs=GT[:, sl], start=True, stop=True),
                         reads=[t_esel_b[s_], t_GT], writes=[t_psf[4]])
                    P.act(lambda e, sl=sl: e.activation(out=gbc_sb[:, sl], in_=psf[4][:], func=AF.Copy),
                          reads=[t_psf[4]], writes=[t_gbcsb])
                    P.pe(lambda e, sl=sl: e.matmul(psf[5][:], lhsT=esel_, rhs=rankT[:, sl], start=True, stop=True),
                         reads=[t_esel_b[s_], t_rankT], writes=[t_psf[5]])
                    P.dve(lambda e, sl=sl: e.scalar_tensor_tensor(out=GselT_[:, sl], in0=psf[5][:], scalar=iota_p[:, 0:1],
                                                                  in1=gbc_sb[:, sl], op0=ALU.is_equal, op1=ALU.mult),
                          reads=[t_psf[5], t_gbcsb, t_const], writes=[t_GselT_b[s_]])

            def expert_main(ex):
                conv = (ex in CONV_SET) and NCONV > 0
                s_ = ex % 2
                Sel_, GselT_ = Sel_b[s_], GselT_b[s_]
                for kt in range(KT):
                    bank = kt // 4
                    for i in range(NT):
                        P.pe(lambda e, kt=kt, i=i, bank=bank: e.matmul(psf[bank][:, (kt % 4) * 128:(kt % 4 + 1) * 128],
                                                                       lhsT=a3_tok[:, i, kt * 128:(kt + 1) * 128], rhs=Sel_[:, i, :],
                                                                       start=(i == 0), stop=(i == NT - 1)),
                             reads=[t_a3[i], t_Sel_b[s_]], writes=[t_psf[bank]])
                for bank in range(4):
                    copy_evac(xeT[:, bank * 4:(bank + 1) * 4, :], psf[bank][:].rearrange("p (j c) -> p j c", c=128),
                              [t_psf[bank]], [t_xeT])
                for c in range(2):
                    sg_ = next_slot()
                    eload(slots[sg_], w_eg, wbf_g if conv else None, c * 512, 512, ex * D, D, "slot%d" % sg_, t_slot[sg_], conv)
                    su_ = next_slot()
                    eload(slots[su_], w_eu, wbf_u if conv else None, c * 512, 512, ex * D, D, "slot%d" % su_, t_slot[su_], conv)
                    for (bank, sl_i) in ((4, sg_), (5, su_)):
                        for kt in range(KT):
                            P.pe(lambda e, kt=kt, bank=bank, sl_i=sl_i: e.matmul(psf[bank][:], lhsT=xeT[:, kt, :], rhs=slots[sl_i][:, kt, :],
                                                                                 start=(kt == 0), stop=(kt == KT - 1)),
                                 reads=[t_xeT, t_slot[sl_i]], writes=[t_psf[bank]])
                    silu_to(hb_tmp, psf[4][:], [t_psf[4]], t_hbtmp)
                    P.dve(lambda e, c=c: e.tensor_tensor(out=hb[:, c * 512:(c + 1) * 512], in0=psf[5][:], in1=hb_tmp, op=ALU.mult),
                          reads=[t_psf[5], t_hbtmp], writes=[t_hb])
                pb = 6
                for kt in range(8):
                    P.pe(lambda e, kt=kt: e.transpose(out=psb(pb)[:, kt * 128:(kt + 1) * 128], in_=hb[:, kt * 128:(kt + 1) * 128],
                                                     identity=ident_bf), reads=[t_hb, t_const], writes=[t_psf[pb]])
                copy_evac(hbT, psb(pb).rearrange("p (n c) -> p n c", c=128), [t_psf[pb]], [t_hbT])
                if ex + 1 < N_EXP:
                    expert_prep(ex + 1)
                for c2 in range(2):
                    sd_ = next_slot()
                    sl_v = slots[sd_].rearrange("p a b -> p (a b)").rearrange("p (a b) -> p a b", b=1024)
                    eload(sl_v, w_ed, wbf_d if conv else None, c2 * 1024, 1024, ex * 1024, 1024, "slot%d" % sd_, t_slot[sd_], conv)
                    for c3 in range(2):
                        dc = c2 * 2 + c3
                        for kt in range(8):
                            P.pe(lambda e, kt=kt, c3=c3, sl_v=sl_v: e.matmul(psf[4][:], lhsT=hbT[:, kt, :],
                                                                             rhs=sl_v[:, kt, c3 * 512:(c3 + 1) * 512],
                                                                             start=(kt == 0), stop=(kt == 7)),
                                 reads=[t_hbT, t_slot[sd_]], writes=[t_psf[4]])
                        P.act(lambda e: e.activation(out=Y_sb, in_=psf[4][:], func=AF.Copy), reads=[t_psf[4]], writes=[t_Y])
                        for i in range(NT):
                            bank = 5 + (i % 3)
                            P.pe(lambda e, i=i, bank=bank: e.matmul(psf[bank][:], lhsT=GselT_[:, i * 128:(i + 1) * 128], rhs=Y_sb,
                                                                    start=True, stop=True),
                                 reads=[t_GselT_b[s_], t_Y], writes=[t_psf[bank]])
                            P.dve(lambda e, i=i, bank=bank, dc=dc: e.tensor_tensor(out=h[:, i, dc * 512:(dc + 1) * 512], in0=psf[bank][:],
                                                                                   in1=h[:, i, dc * 512:(dc + 1) * 512], op=ALU.add),
                                  reads=[t_psf[bank], t_h[i]], writes=[t_h[i]])

            expert_prep(0)
            for ex in range(N_EXP):
                expert_main(ex)

        if stage >= 3:
            P.barrier()
        if final_norm:
            load_gbc(g_fin)
            rms_stats(lambda i: h[:, i, :], t_h, NT)
        for i in range(NT):
            if final_norm:
                P.dve(lambda e, i=i: e.scalar_tensor_tensor(out=h[:, i, :], in0=h[:, i, :], scalar=stat[:, 3, i:i + 1],
                                                            in1=gbc, op0=ALU.mult, op1=ALU.mult),
                      reads=[t_h[i], t_stat, t_gbc], writes=[t_h[i]])
            P.dma("sp", lambda e, i=i: e.dma_start(out=out[i * 128:(i + 1) * 128, :], in_=h[:, i, :]), "out",
                  reads=[t_h[i]], final=True)

        P.arena_high = A.high
        P.build()
    return nc, P, names


def make_in_maps(inputs, names):
    f = lambda a: np.ascontiguousarray(np.asarray(a, dtype=np.float32))
    x = f(inputs["x"])
    shapes = {
        "norm_mix_g": (D,), "w_in": (D, 7184), "w_gla_alpha_up": (16, 512), "b_gla_alpha": (512, 1),
        "gla_out_norm_g": (256,), "hgrn_lb_logits": (2, 1024), "hgrn_out_norm_g": (128,), "w_mix_out": (D, D),
        "norm_xattn_g": (D,), "norm_mem_g": (D,), "w_xattn_q": (D, D), "w_xattn_kv": (D, 2 * D), "w_xattn_out": (D, D),
        "norm_ffn_g": (D,), "w_router_group": (D, 4), "b_router_group": (4,), "w_router_expert": (D, 32),
        "b_router_expert": (32,), "w_expert_gate": (N_EXP * D, 1024), "w_expert_up": (N_EXP * D, 1024),
        "w_expert_down": (N_EXP * 1024, D), "norm_final_g": (D,),
    }
    shared = {k: f(inputs[k]).reshape(shp) for k, shp in shapes.items() if k in names}
    in_maps = []
    for c in range(8):
        b, s = c // 4, c % 4
        xp = np.zeros((NPREV * T, D), np.float32)
        if s > 0:
            xp[(NPREV - s) * T:] = x[b, 0:s * T]
        m = dict(shared)
        m["x_own"] = np.ascontiguousarray(x[b, s * T:(s + 1) * T])
        m["x_prev"] = xp
        if "mem" in names:
            m["mem"] = f(inputs["mem"])[b]
        in_maps.append(m)
    return in_maps


def run(inputs, stage=3, final_norm=True, trace=False, cores=8):
    nc, P, names = build_nc(stage=stage, final_norm=final_norm)
    in_maps = make_in_maps(inputs, names)[:cores]
    res = run_bass_kernel_spmd(nc, in_maps, core_ids=list(range(cores)), trace=trace)
    outp = np.zeros((2, 4096, D), np.float32)
    for c in range(cores):
        b, s = c // 4, c % 4
        outp[b, s * T:(s + 1) * T] = res.results[c]["out"]
    return outp, res


def kernel(**inputs):
    outp, _ = run(inputs)
    return outp
```

```python
import contextlib
import numpy as np
import concourse.bass as bass
import concourse.mybir as mybir
from concourse.bass_utils import run_bass_kernel_spmd

F32 = mybir.dt.float32
BF16 = mybir.dt.bfloat16
I32 = mybir.dt.int32
ALU = mybir.AluOpType
AF = mybir.ActivationFunctionType
AX = mybir.AxisListType

SAME_ENGINE_SYNC = True
D = 2048
T = 1024
NT = 8
KT = 16
EPS = 1e-6
NPREV = 3
N_EXP = 32
CAP = 128


class Tok:
    __slots__ = ("name", "last_write", "readers", "excl")

    def __init__(self, name, excl=False):
        self.name = name
        self.last_write = None
        self.readers = []
        self.excl = excl


class Op:
    __slots__ = ("eng", "fn", "reads", "writes", "dma_key", "idx", "signal", "seq",
                 "waits", "dma_cum", "deps", "barrier")

    def __init__(self, eng, fn, reads, writes, dma_key):
        self.eng = eng
        self.fn = fn
        self.reads = reads
        self.writes = writes
        self.dma_key = dma_key
        self.signal = False
        self.seq = 0
        self.waits = []
        self.dma_cum = 0
        self.deps = []
        self.barrier = False


class Prog:
    ENGS = ("pe", "act", "dve", "pool", "sp")

    def __init__(self, nc):
        self.nc = nc
        self.ops = []
        self.final_dma = []

    def tok(self, name="t"):
        return Tok(name)

    def toks(self, name, n, excl=False):
        return [Tok("%s%d" % (name, i), excl) for i in range(n)]

    def add(self, eng, fn, reads=(), writes=(), dma_key=None):
        op = Op(eng, fn, [t for t in reads if t is not None],
                [t for t in writes if t is not None], dma_key)
        op.idx = len(self.ops)
        self.ops.append(op)
        return op

    def pe(self, fn, reads=(), writes=()):
        return self.add("pe", fn, reads, writes)

    def act(self, fn, reads=(), writes=()):
        return self.add("act", fn, reads, writes)

    def dve(self, fn, reads=(), writes=()):
        return self.add("dve", fn, reads, writes)

    def pool(self, fn, reads=(), writes=()):
        return self.add("pool", fn, reads, writes)

    def dma(self, q, fn, key, reads=(), writes=(), final=False):
        op = self.add(q, fn, reads, writes, dma_key=key)
        if final:
            self.final_dma.append(op)
        return op

    def barrier(self):
        op = Op("sp", lambda e: e.nop(), [], [], None)
        op.barrier = True
        op.idx = len(self.ops)
        self.ops.append(op)
        return op

    def build(self):
        nc = self.nc
        ops = self.ops
        last_eng = {}
        last_dma = {}
        cur_barrier = None
        seen_after = set()
        for op in ops:
            if op.barrier:
                op.deps = [d for d in last_eng.values() if d.eng != "sp"] + \
                          [d for k_, d in last_dma.items() if not str(k_).startswith("cv")]
                for d in op.deps:
                    if d.dma_key is None:
                        d.signal = True
                cur_barrier = op
                seen_after = set()
                continue
            if op.dma_key is not None:
                last_dma[op.dma_key] = op
            else:
                last_eng[op.eng] = op
            if cur_barrier is not None and op.eng not in seen_after:
                seen_after.add(op.eng)
                if op.eng != "sp":
                    op.deps.append(cur_barrier)
                    cur_barrier.signal = True
            deps = {}
            for t in op.reads:
                if t.last_write is not None:
                    deps[t.last_write.idx] = ("raw", t.last_write)
                if t.excl:
                    for r in t.readers:
                        if r.idx not in deps and r.eng != op.eng:
                            deps[r.idx] = ("war", r)
            for t in op.writes:
                if t.last_write is not None:
                    deps.setdefault(t.last_write.idx, ("waw", t.last_write))
                for r in t.readers:
                    if r.idx not in deps:
                        deps[r.idx] = ("war", r)
            for t in op.reads:
                t.readers.append(op)
            for t in op.writes:
                t.last_write = op
                t.readers = []
            best = {}
            for kind, d in deps.values():
                if d is op:
                    continue
                if d.dma_key is None and d.eng == op.eng:
                    if op.eng == "pe" or op.eng == "sp":
                        continue
                    if not SAME_ENGINE_SYNC:
                        continue
                if d.dma_key is not None:
                    op.deps.append(d)
                else:
                    b = best.get(d.eng)
                    if b is None or d.idx > b.idx:
                        best[d.eng] = d
            for d in best.values():
                op.deps.append(d)
                d.signal = True
        cnt = {e: 0 for e in self.ENGS}
        dma_cnt = {}
        for op in ops:
            if op.dma_key is not None:
                dma_cnt[op.dma_key] = dma_cnt.get(op.dma_key, 0) + 16
                op.dma_cum = dma_cnt[op.dma_key]
            elif op.signal:
                cnt[op.eng] += 1
                op.seq = cnt[op.eng]
        waited = {e: {} for e in self.ENGS}
        dma_cnt2 = {}
        for op in ops:
            if op.dma_key is not None:
                dma_cnt2[op.dma_key] = dma_cnt2.get(op.dma_key, 0) + 16
            need = {}
            for d in op.deps:
                if d.dma_key is not None:
                    k = ("dma", d.dma_key)
                    v = dma_cnt2[d.dma_key] if d.dma_key != op.dma_key else d.dma_cum
                else:
                    k = ("eng", d.eng)
                    v = d.seq
                if v > need.get(k, 0):
                    need[k] = v
            w = waited[op.eng]
            for k, v in need.items():
                if w.get(k, 0) >= v:
                    continue
                w[k] = v
                op.waits.append((k, v))
        dma_keys = sorted(dma_cnt.keys(), key=str)
        self.n_sems = len(dma_keys) + len(self.ENGS)
        self.counts = dict(cnt)
        sems = {}
        with contextlib.ExitStack() as st:
            for e in self.ENGS:
                sems[("eng", e)] = st.enter_context(nc.semaphore("s_" + e))
            for i, k in enumerate(dma_keys):
                sems[("dma", k)] = st.enter_context(nc.semaphore("d_%d" % i))
            block = st.enter_context(nc.Block())
            per = {e: [o for o in ops if o.eng == e] for e in self.ENGS}
            final = [(("dma", o.dma_key), dma_cnt[o.dma_key]) for o in self.final_dma]

            def emit(engine, name):
                for op in per[name]:
                    for k, v in op.waits:
                        engine.wait_ge(sems[k], v)
                    ins = op.fn(engine)
                    if op.dma_key is not None:
                        ins.then_inc(sems[("dma", op.dma_key)], 16)
                    elif op.signal:
                        ins.then_inc(sems[("eng", name)], 1)
                if name == "sp":
                    done = set()
                    for k, v in final:
                        if k in done:
                            continue
                        done.add(k)
                        engine.wait_ge(sems[k], v)

            @block.tensor
            def _(e):
                emit(e, "pe")

            @block.scalar
            def _(e):
                emit(e, "act")

            @block.vector
            def _(e):
                emit(e, "dve")

            @block.gpsimd
            def _(e):
                emit(e, "pool")

            @block.sync
            def _(e):
                emit(e, "sp")
        return nc


ARENA_BYTES = 207 * 1024
NCONV = 0
CONV_SET = set(e for e in range(32) if e % 4 != 0)
DBG = {"units": 12, "seg0": 0, "proj": True, "full": True, "skip": "", "cut": 99}


class Arena:
    def __init__(self, ap):
        self.ap = ap
        self.top = 0
        self.high = 0

    def alloc(self, shape, dt=F32):
        assert shape[0] <= 128
        n = 1
        for s_ in shape[1:]:
            n *= s_
        esz = 2 if dt == BF16 else 4
        nbytes = (n * esz + 63) // 64 * 64
        off = self.top
        self.top += nbytes
        self.high = max(self.high, self.top)
        assert self.top <= ARENA_BYTES, ("arena overflow", self.top)
        v = self.ap[:, off // 2:(off + n * esz) // 2]
        if dt != BF16:
            v = v.bitcast(dt)
        if shape[0] < 128:
            v = v[0:shape[0]]
        if len(shape) == 3:
            v = v.rearrange("p (a b) -> p a b", b=shape[2])
        elif len(shape) == 4:
            v = v.rearrange("p (a b c) -> p a b c", b=shape[2], c=shape[3])
        return v


def build_nc(stage=3, final_norm=True):
    nc = bass.Bass("TRN2", target_bir_lowering=False)
    P = Prog(nc)
    st = contextlib.ExitStack()
    names = []

    def din(name, shape, dt=F32):
        names.append(name)
        return nc.dram_tensor(name, list(shape), dt, kind="ExternalInput").ap()

    x_own = din("x_own", [T, D])
    x_prev = din("x_prev", [NPREV * T, D])
    g_mix = din("norm_mix_g", [D])
    w_in = din("w_in", [D, 7184])
    w_up = din("w_gla_alpha_up", [16, 512])
    b_alpha = din("b_gla_alpha", [512, 1])
    gla_g = din("gla_out_norm_g", [256])
    lb_logits = din("hgrn_lb_logits", [2, 1024])
    hgrn_g = din("hgrn_out_norm_g", [128])
    w_mo = din("w_mix_out", [D, D])
    g_fin = din("norm_final_g", [D])
    if stage >= 2:
        mem = din("mem", [256, D])
        g_xa = din("norm_xattn_g", [D])
        g_mem = din("norm_mem_g", [D])
        w_xq = din("w_xattn_q", [D, D])
        w_xkv = din("w_xattn_kv", [D, 2 * D])
        w_xo = din("w_xattn_out", [D, D])
    if stage >= 3:
        g_ffn = din("norm_ffn_g", [D])
        w_rg = din("w_router_group", [D, 4])
        b_rg = din("b_router_group", [4])
        w_re = din("w_router_expert", [D, 32])
        b_re = din("b_router_expert", [32])
        w_eg = din("w_expert_gate", [N_EXP * D, 1024])
        w_eu = din("w_expert_up", [N_EXP * D, 1024])
        w_ed = din("w_expert_down", [N_EXP * 1024, D])
    out = nc.dram_tensor("out", [T, D], F32, kind="ExternalOutput").ap()
    conv_list = []
    if stage >= 3 and NCONV > 0:
        wbf_g = nc.dram_tensor("wbf_g", [N_EXP * D, 1024], BF16).ap()
        wbf_u = nc.dram_tensor("wbf_u", [N_EXP * D, 1024], BF16).ap()
        wbf_d = nc.dram_tensor("wbf_d", [N_EXP * 1024, D], BF16).ap()
        for ex in range(N_EXP):
            if ex in CONV_SET:
                conv_list.append((wbf_g[ex * D:(ex + 1) * D, :], w_eg[ex * D:(ex + 1) * D, :]))
                conv_list.append((wbf_u[ex * D:(ex + 1) * D, :], w_eu[ex * D:(ex + 1) * D, :]))
                conv_list.append((wbf_d[ex * 1024:(ex + 1) * 1024, :], w_ed[ex * 1024:(ex + 1) * 1024, :]))

    with st:
        arena_t = st.enter_context(nc.sbuf_tensor("arena", [128, ARENA_BYTES // 2], BF16))
        A = Arena(arena_t[:])
        psf = [st.enter_context(nc.psum_tensor("psf%d" % i, [128, 512], F32)) for i in range(8)]
        t_psf = P.toks("psf", 8, excl=True)

        def psb(i):
            return psf[i][:].bitcast(BF16)

        ones_f = A.alloc([128, 128])
        ident_bf = A.alloc([128, 128], BF16)
        ident_f = A.alloc([128, 128])
        maskT = A.alloc([128, 128])
        ltri = A.alloc([128, 128], BF16)
        ones_bf = A.alloc([128, 128], BF16)
        iota_row = A.alloc([128, 128])
        iota_p = A.alloc([128, 1])
        scanmask = A.alloc([128, T])
        gj = A.alloc([128, 4 * D], BF16)
        gbc = gj[:, 0:2 * D].bitcast(F32)
        ja = gj[:, 2 * D:4 * D]
        junk = ja[:, 0:D]
        a_tok = ja[:, D:2 * D]
        stat = A.alloc([128, 4, NT])
        gla_gbc = A.alloc([128, 256])
        hgrn_gbc = A.alloc([128, 128])
        wup_sb = A.alloc([16, 512])
        balpha_sb = A.alloc([128, 4])
        nbalpha = A.alloc([128, 4])
        lbl_sb = A.alloc([128, 2, 8])
        lb_sb = A.alloc([128, 8])
        oml_sb = A.alloc([128, 8])
        aT = A.alloc([128, KT, T], BF16)
        t_const, t_gbc, t_junk, t_atok, t_stat, t_par = P.toks("pp", 6)
        t_aT = P.toks("aT", NT)
        M_PERSIST = A.top

        P.pool(lambda e: e.memset(ones_f, 1.0), writes=[t_const])
        P.pool(lambda e: e.memset(ones_bf, 1.0), writes=[t_const])
        for dst in (ident_bf, ident_f) if "asel" not in DBG["skip"] else ():
            P.pool(lambda e, dst=dst: e.affine_select(out=dst, in_=ones_f, pattern=[[-1, 128]],
                                                      compare_op=ALU.is_equal, fill=0.0, base=0, channel_multiplier=1),
                   reads=[t_const], writes=[t_const])
        if "asel" not in DBG["skip"]:
            P.pool(lambda e: e.affine_select(out=maskT, in_=ones_f, pattern=[[1, 128]],
                                             compare_op=ALU.is_ge, fill=0.0, base=0, channel_multiplier=-1),
                   reads=[t_const], writes=[t_const])
            P.pool(lambda e: e.affine_select(out=ltri, in_=ones_f, pattern=[[1, 128]],
                                             compare_op=ALU.is_gt, fill=0.0, base=0, channel_multiplier=-1),
                   reads=[t_const], writes=[t_const])
        if "iota" not in DBG["skip"]:
            P.pool(lambda e: e.iota(iota_row, pattern=[[1, 128]], base=0, channel_multiplier=0,
                                    allow_small_or_imprecise_dtypes=True), writes=[t_const])
            P.pool(lambda e: e.iota(iota_p, pattern=[[0, 1]], base=0, channel_multiplier=1,
                                    allow_small_or_imprecise_dtypes=True), writes=[t_const])
        P.pool(lambda e: e.memset(scanmask, 1.0), writes=[t_const])
        P.pool(lambda e: e.memset(scanmask.rearrange("p (c j) -> p c j", j=128)[:, :, 0:1], 0.0),
               reads=[t_const], writes=[t_const])

        def load_gbc(src):
            P.dma("sp", lambda e: e.dma_start(out=gbc, in_=src.partition_broadcast(128)), "gbc", writes=[t_gbc])

        P.dma("sp", lambda e: e.dma_start(out=gla_gbc, in_=gla_g.partition_broadcast(128)), "par", writes=[t_par])
        P.dma("sp", lambda e: e.dma_start(out=hgrn_gbc, in_=hgrn_g.partition_broadcast(128)), "par", writes=[t_par])
        P.dma("sp", lambda e: e.dma_start(out=wup_sb, in_=w_up), "par", writes=[t_par])
        for hh in range(4):
            P.dma("sp", lambda e, hh=hh: e.dma_start(out=balpha_sb[:, hh:hh + 1], in_=b_alpha[hh * 128:(hh + 1) * 128, :]),
                  "par", writes=[t_par])
        for s_ in range(2) if "lbl" not in DBG["skip"] else ():
            for hh in range(8):
                P.dma("sp", lambda e, s_=s_, hh=hh: e.dma_start(
                    out=lbl_sb[:, s_, hh:hh + 1],
                    in_=lb_logits[s_:s_ + 1, hh * 128:(hh + 1) * 128].rearrange("o p -> p o")),
                    "par", writes=[t_par])
        P.dve(lambda e: e.tensor_sub(out=lb_sb, in0=lbl_sb[:, 0, :], in1=lbl_sb[:, 1, :]), reads=[t_par], writes=[t_par])
        P.act(lambda e: e.activation(out=lb_sb, in_=lb_sb, func=AF.Exp, scale=-1.0), reads=[t_par], writes=[t_par])
        P.dve(lambda e: e.tensor_scalar(out=lb_sb, in0=lb_sb, scalar1=1.0, scalar2=None, op0=ALU.add), reads=[t_par], writes=[t_par])
        P.dve(lambda e: e.reciprocal(out=lb_sb, in_=lb_sb), reads=[t_par], writes=[t_par])
        P.dve(lambda e: e.tensor_scalar(out=oml_sb, in0=lb_sb, scalar1=-1.0, scalar2=1.0,
                                        op0=ALU.mult, op1=ALU.add), reads=[t_par], writes=[t_par])
        P.dve(lambda e: e.tensor_scalar(out=nbalpha, in0=balpha_sb, scalar1=-1.0, scalar2=None,
                                        op0=ALU.mult), reads=[t_par], writes=[t_par])

        rr = {"ev": 0, "pa": 0, "pb": 0, "pool": [0, 1]}

        def evac(fn_act, fn_dve, reads, writes):
            rr["ev"] ^= 1
            if rr["ev"]:
                P.act(fn_act, reads, writes)
            else:
                P.dve(fn_dve, reads, writes)

        def copy_evac(out_ap, in_ap, reads, writes):
            evac(lambda e: e.activation(out=out_ap, in_=in_ap, func=AF.Copy),
                 lambda e: e.tensor_copy(out=out_ap, in_=in_ap), reads, writes)

        def sigmoid_to(out_ap, in_ap, reads, tok):
            P.act(lambda e: e.activation(out=out_ap, in_=in_ap, func=AF.Sigmoid), reads=reads, writes=[tok])

        def silu_to(out_ap, in_ap, reads, tok):
            P.act(lambda e: e.activation(out=out_ap, in_=in_ap, func=AF.Silu), reads=reads, writes=[tok])

        def sumsq(src_ap, src_tok, dst_ap, n):
            P.dve(lambda e: e.scalar_tensor_tensor(out=junk[:, 0:n], in0=src_ap, scalar=1.0, in1=src_ap,
                                                   op0=ALU.mult, op1=ALU.mult, accum_out=dst_ap),
                  reads=[src_tok], writes=[t_junk, t_stat])

        def rstd_of(dst_ap, ss_ap, tmp_ap, dim, tok):
            P.act(lambda e: e.activation(out=tmp_ap, in_=ss_ap, func=AF.Ln, scale=1.0 / dim, bias=EPS), reads=[tok], writes=[tok])
            P.act(lambda e: e.activation(out=dst_ap, in_=tmp_ap, func=AF.Exp, scale=-0.5), reads=[tok], writes=[tok])

        def next_pa():
            rr["pa"] = (rr["pa"] + 1) % len(rr["pool"])
            return rr["pool"][rr["pa"]]

        def next_pb():
            rr["pb"] ^= 1
            return 6 + rr["pb"]

        def transpose_tile(src, src_tok, dstT, dst_tok, cols):
            for half in range(2):
                pb = next_pb()
                for j in range(8):
                    kt = half * 8 + j
                    P.pe(lambda e, pb=pb, j=j, kt=kt: e.transpose(out=psb(pb)[:, j * 128:(j + 1) * 128],
                                                                   in_=src[:, kt * 128:(kt + 1) * 128],
                                                                   identity=ident_bf),
                         reads=[src_tok, t_const], writes=[t_psf[pb]])
                copy_evac(dstT[:, half * 8:(half + 1) * 8, cols],
                          psb(pb).rearrange("p (j c) -> p j c", c=128),
                          [t_psf[pb]], [dst_tok])

        def rms_stats(src_fn, src_toks, ntile, dim=D):
            for i in range(ntile):
                sumsq(src_fn(i), src_toks[i], stat[:, 0, i:i + 1], dim)
            rstd_of(stat[:, 3, 0:ntile], stat[:, 0, 0:ntile], stat[:, 1, 0:ntile], dim, t_stat)

        def norm_to_T(src_fn, src_toks, ntile, dstT, dst_toks):
            rms_stats(src_fn, src_toks, ntile)
            for i in range(ntile):
                P.dve(lambda e, i=i: e.scalar_tensor_tensor(out=a_tok, in0=src_fn(i), scalar=stat[:, 3, i:i + 1],
                                                            in1=gbc, op0=ALU.mult, op1=ALU.mult),
                      reads=[src_toks[i], t_stat, t_gbc], writes=[t_atok])
                transpose_tile(a_tok, t_atok, dstT, dst_toks[i], slice(i * 128, (i + 1) * 128))

        def wload(dst_ap, src_ap, key, tok):
            P.dma("pool", lambda e: e.dma_start(out=dst_ap, in_=src_ap), key, writes=[tok])

        t_cv = P.tok("cv")
        conv_state = {"i": 0}

        def emit_conv(n):
            for _ in range(n):
                if conv_state["i"] >= len(conv_list):
                    return
                dst, src = conv_list[conv_state["i"]]
                conv_state["i"] += 1
                tk = P.tok("cvi")
                conv_state["last_tok"] = tk
                P.dma("pool", lambda e, dst=dst, src=src: e.dma_start(out=dst, in_=src), "cv", writes=[tk])

        def wview(w, c0, n, r0=0, rows=D):
            return w[r0:r0 + rows, c0:c0 + n].rearrange("(kt p) n -> p kt n", p=128)

        def proj_fm(wap, wtok, src_T, src_toks, half, pa, M=128):
            for kt in range(KT):
                P.pe(lambda e, kt=kt: e.matmul(psf[pa][0:M, :], lhsT=wap[:, kt, :],
                                               rhs=src_T[:, kt, half * 512:(half + 1) * 512],
                                               start=(kt == 0), stop=(kt == KT - 1)),
                     reads=[wtok] + src_toks[half * 4:(half + 1) * 4], writes=[t_psf[pa]])

        o_tok = A.alloc([128, NT, D], BF16)
        t_otok = P.toks("otok", NT)
        M_AFTER_OTOK = A.top
        wq_b = [A.alloc([128, KT, 128], BF16) for _ in range(2)]
        wk_b = [A.alloc([128, KT, 128], BF16) for _ in range(2)]
        wvg_b = [A.alloc([128, KT, 512], BF16) for _ in range(1)]
        wlr = A.alloc([128, KT, 16], BF16)
        t_wq = P.toks("wq", 2)
        t_wk = P.toks("wk", 2)
        t_wvgh = P.toks("wvgh", 2)
        t_wlr = P.tok("wlr")
        qf = A.alloc([128, T])
        kf = A.alloc([128, T])
        t1 = A.alloc([128, T])
        t2 = A.alloc([128, T])
        t3 = A.alloc([128, T])
        qt = A.alloc([128, T], BF16)
        ktl = A.alloc([128, T], BF16)
        qs = A.alloc([128, T], BF16)
        khT = A.alloc([128, T], BF16)
        t_qf, t_kf, t_t1, t_t2, t_t3, t_qt, t_ktl, t_qs, t_khT = P.toks("mx", 9)
        sm = A.alloc([128, 4, NT])
        sm2 = A.alloc([128, 2, NT])
        t_sm = P.tok("sm")
        v_tok = A.alloc([128, NT, 256], BF16)
        sg_tok = A.alloc([128, NT, 256], BF16)
        sg_tmp = A.alloc([128, 256])
        khat = A.alloc([128, NT, 128], BF16)
        AT_sb = A.alloc([128, NT, 128], BF16)
        S_all = A.alloc([128, 2048])
        S_bf = A.alloc([128, NT + 1, 256], BF16)
        glrT = A.alloc([16, T])
        ost = A.alloc([128, 4, NT])
        o_all = A.alloc([128, NT, 256])
        S_tmp = A.alloc([128, 256])
        t_osb = P.toks("osb", NT)
        t_Stmp = P.tok("Stmp")
        xs = [A.alloc([128, D]) for _ in range(2)]
        t_v, t_sg, t_sgtmp, t_khat, t_AT, t_Sbf, t_glr, t_ost = P.toks("mb", 8)
        t_S = P.toks("S", 12)
        t_xs = P.toks("xs", 2)

        P.pool(lambda e: e.memset(S_all, 0.0), writes=t_S)
        P.pool(lambda e: e.memset(AT_sb, 0.0), writes=[t_AT])

        def capture(fn):
            start = len(P.ops)
            fn()
            ops_ = P.ops[start:]
            del P.ops[start:]
            return ops_

        def interleave(a, b):
            na, nb = len(a), len(b)
            ia = ib = 0
            while ia < na or ib < nb:
                if ib >= nb or (ia < na and ia * nb <= ib * na):
                    op = a[ia]
                    ia += 1
                else:
                    op = b[ib]
                    ib += 1
                op.idx = len(P.ops)
                P.ops.append(op)

        def full_cols(u):
            if u < 4:
                return u * 128, 512 + u * 128, 1024 + u * 256, 2048 + u * 256, 256
            hu = u - 4
            return 3088 + hu * 128, 4112 + hu * 128, 5136 + hu * 128, 6160 + hu * 128, 128

        def full_loads_qk(u):
            cq, ck, cv, cg, dv = full_cols(u)
            wb = u % 2
            wload(wq_b[wb], wview(w_in, cq, 128), "wq%d" % wb, t_wq[wb])
            wload(wk_b[wb], wview(w_in, ck, 128), "wk%d" % wb, t_wk[wb])

        def full_loads_vg(u):
            cq, ck, cv, cg, dv = full_cols(u)
            wvg = wvg_b[0]
            wload(wvg[:, :, 0:dv], wview(w_in, cv, dv), "wvg0", t_wvgh[0])
            wload(wvg[:, :, dv:2 * dv], wview(w_in, cg, dv), "wvg1", t_wvgh[1])

        def mixer_unit(u, full):
            gla = u < 4
            wb = u % 2
            dv = 256 if gla else 128
            nv = 2 * dv if full else dv
            sc = (-1.0 / 16.0) if gla else 1.0
            if gla:
                cq, ck, cv, cg = u * 128, 512 + u * 128, 1024 + u * 256, 2048 + u * 256
                scol = u * 256
            else:
                hu = u - 4
                cq, ck, cv, cg = 3088 + hu * 128, 4112 + hu * 128, 5136 + hu * 128, 6160 + hu * 128
                scol = 1024 + hu * 128
            tS = t_S[u]
            if full:
                wvg = wvg_b[0]
                twvg_r = list(t_wvgh)
            else:
                hsel = u % 2
                wvg = wvg_b[0][:, :, hsel * 256:(hsel + 1) * 256]
                twvg_r = [t_wvgh[hsel]]
            if full:
                if u == 0:
                    full_loads_qk(0)
                    full_loads_vg(0)
                if u + 1 < DBG["units"]:
                    full_loads_qk(u + 1)
            else:
                wload(wk_b[wb], wview(w_in, ck, 128), "wk%d" % wb, t_wk[wb])
                wload(wvg[:, :, 0:dv], wview(w_in, cv, dv), "wvg%d" % hsel, t_wvgh[hsel])
            if full:
                for half in range(2):
                    pa = next_pa()
                    proj_fm(wq_b[wb], t_wq[wb], aT, t_aT, half, pa)
                    sl = slice(half * 512, (half + 1) * 512)
                    if gla:
                        P.act(lambda e, pa=pa, sl=sl: e.activation(out=qf[:, sl], in_=psf[pa][:], func=AF.Copy,
                                                                  scale=128.0 ** -0.5),
                              reads=[t_psf[pa]], writes=[t_qf])
                    else:
                        silu_to(qf[:, sl], psf[pa][:], [t_psf[pa]], t_qf)
            for half in range(2):
                pa = next_pa()
                sl = slice(half * 512, (half + 1) * 512)
                proj_fm(wk_b[wb], t_wk[wb], aT, t_aT, half, pa)
                if gla:
                    copy_evac(kf[:, sl], psf[pa][:], [t_psf[pa]], [t_kf])
                    pz = next_pa()
                    P.pe(lambda e, pz=pz, sl=sl: e.matmul(psf[pz][:], lhsT=wup_sb[:, u * 128:(u + 1) * 128],
                                                         rhs=glrT[:, sl], start=True, stop=True),
                         reads=[t_par, t_glr], writes=[t_psf[pz]])
                    P.act(lambda e, pz=pz, sl=sl: e.activation(out=t1[:, sl], in_=psf[pz][:], func=AF.Exp,
                                                              scale=-1.0, bias=nbalpha[:, u:u + 1]),
                          reads=[t_psf[pz], t_par], writes=[t_t1])
                else:
                    sigmoid_to(t1[:, sl], psf[pa][:], [t_psf[pa]], t_t1)
            def chain_part():
                if gla:
                    for half in range(2):
                        P.act(lambda e, half=half: e.activation(out=t1[:, half * 512:(half + 1) * 512], in_=t1[:, half * 512:(half + 1) * 512],
                                                                func=AF.Ln, bias=1.0), reads=[t_t1], writes=[t_t1])
                else:
                    P.dve(lambda e: e.tensor_scalar(out=t2, in0=t1, scalar1=oml_sb[:, hu:hu + 1],
                                                    scalar2=lb_sb[:, hu:hu + 1], op0=ALU.mult, op1=ALU.add),
                          reads=[t_t1, t_par], writes=[t_t2])
                    P.dve(lambda e: e.tensor_scalar(out=kf, in0=t2, scalar1=-1.0, scalar2=1.0,
                                                    op0=ALU.mult, op1=ALU.add), reads=[t_t2], writes=[t_kf])
                    P.act(lambda e: e.activation(out=t1, in_=t2, func=AF.Ln), reads=[t_t2], writes=[t_t1])
                P.dve(lambda e: e.tensor_tensor_scan(out=t2, data0=scanmask, data1=t1, initial=0.0,
                                                     op0=ALU.mult, op1=ALU.add),
                      reads=[t_t1, t_const], writes=[t_t2])
                c3 = t2.rearrange("p (n j) -> p n j", j=128)
                cref = c3[:, :, 63:64]
                clast = c3[:, :, 127:128]
                cref2 = cref.rearrange("p n o -> p (n o)")
                clast2 = clast.rearrange("p n o -> p (n o)")
                P.dve(lambda e: e.tensor_tensor(out=t1.rearrange("p (n j) -> p n j", j=128), in0=c3,
                                                in1=cref.to_broadcast([128, NT, 128]), op=ALU.subtract),
                      reads=[t_t2], writes=[t_t1])
                if full:
                    P.act(lambda e: e.activation(out=sm[:, 0, :], in_=cref2, func=AF.Exp, scale=sc),
                          reads=[t_t2], writes=[t_sm])
                P.dve(lambda e: e.tensor_tensor(out=sm[:, 3, :], in0=clast2, in1=cref2, op=ALU.subtract),
                      reads=[t_t2], writes=[t_sm])
                P.act(lambda e: e.activation(out=sm[:, 1, :], in_=sm[:, 3, :], func=AF.Exp, scale=sc),
                      reads=[t_sm], writes=[t_sm])
                P.act(lambda e: e.activation(out=sm[:, 2, :], in_=clast2, func=AF.Exp, scale=sc),
                      reads=[t_t2], writes=[t_sm])
                P.act(lambda e: e.activation(out=t3, in_=t1, func=AF.Exp, scale=-sc), reads=[t_t1], writes=[t_t3])
                P.dve(lambda e: e.tensor_tensor(out=ktl, in0=kf, in1=t3, op=ALU.mult),
                      reads=[t_kf, t_t3], writes=[t_ktl])
                P.pool(lambda e: e.tensor_tensor(out=khT.rearrange("p (n j) -> p n j", j=128),
                                                 in0=ktl.rearrange("p (n j) -> p n j", j=128),
                                                 in1=sm[:, 1, :].unsqueeze(2).to_broadcast([128, NT, 128]), op=ALU.mult),
                       reads=[t_ktl, t_sm], writes=[t_khT])
                if full:
                    P.act(lambda e: e.activation(out=t3, in_=t1, func=AF.Exp, scale=sc), reads=[t_t1], writes=[t_t3])
                    P.dve(lambda e: e.tensor_tensor(out=qt, in0=qf, in1=t3, op=ALU.mult),
                          reads=[t_qf, t_t3], writes=[t_qt])
                    P.pool(lambda e: e.tensor_tensor(out=qs.rearrange("p (n j) -> p n j", j=128),
                                                     in0=qt.rearrange("p (n j) -> p n j", j=128),
                                                     in1=sm[:, 0, :].unsqueeze(2).to_broadcast([128, NT, 128]), op=ALU.mult),
                           reads=[t_qt, t_sm], writes=[t_qs])

            def vg_part():
                gb = gla_gbc if gla else hgrn_gbc
                for i in range(NT):
                    pa = next_pa()
                    for kt in range(KT):
                        P.pe(lambda e, kt=kt, i=i, pa=pa: e.matmul(psf[pa][:, 0:nv], lhsT=aT[:, kt, i * 128:(i + 1) * 128],
                                                                   rhs=wvg[:, kt, 0:nv],
                                                                   start=(kt == 0), stop=(kt == KT - 1)),
                             reads=twvg_r + [t_aT[i]], writes=[t_psf[pa]])
                    P.act(lambda e, i=i, pa=pa: e.activation(out=v_tok[:, i, 0:dv], in_=psf[pa][:, 0:dv], func=AF.Copy),
                          reads=[t_psf[pa]], writes=[t_v])
                    if full:
                        silu_to(sg_tmp[:, 0:dv], psf[pa][:, dv:2 * dv], [t_psf[pa]], t_sgtmp)
                        P.pool(lambda e, i=i: e.tensor_tensor(out=sg_tok[:, i, 0:dv], in0=sg_tmp[:, 0:dv],
                                                              in1=gb[:, 0:dv], op=ALU.mult),
                               reads=[t_sgtmp, t_par], writes=[t_sg])

            if full:
                ops_a = capture(chain_part)
                ops_b = capture(vg_part)
                interleave(ops_a, ops_b)
            else:
                chain_part()
                vg_part()
            if full and u + 1 < DBG["units"]:
                full_loads_vg(u + 1)
            if full:
                emit_conv(2)
            pb = next_pb()
            for n in range(NT):
                P.pe(lambda e, n=n: e.transpose(out=psb(pb)[:, n * 128:(n + 1) * 128],
                                               in_=khT[:, n * 128:(n + 1) * 128], identity=ident_bf),
                     reads=[t_khT, t_const], writes=[t_psf[pb]])
            copy_evac(khat, psb(pb).rearrange("p (n c) -> p n c", c=128), [t_psf[pb]], [t_khat])
            if full:
                for g2 in range(2):
                    for n4 in range(4):
                        n = g2 * 4 + n4
                        P.pe(lambda e, n=n, n4=n4, g2=g2: e.matmul(psf[2 + g2][:, n4 * 128 + 64:(n4 + 1) * 128],
                                                                  lhsT=ktl[:, n * 128:(n + 1) * 128],
                                                                  rhs=qt[:, n * 128 + 64:(n + 1) * 128], start=True, stop=True),
                             reads=[t_ktl, t_qt], writes=[t_psf[2 + g2]])
                        P.pe(lambda e, n=n, n4=n4, g2=g2: e.matmul(psf[2 + g2][0:64, n4 * 128:n4 * 128 + 64],
                                                                  lhsT=ktl[:, n * 128:n * 128 + 64],
                                                                  rhs=qt[:, n * 128:n * 128 + 64], start=True, stop=True),
                             reads=[t_ktl, t_qt], writes=[t_psf[2 + g2]])
                    pv = psf[2 + g2][:].rearrange("p (n c) -> p n c", c=128)
                    P.dve(lambda e, g2=g2, pv=pv: e.tensor_tensor(out=AT_sb[:, g2 * 4:(g2 + 1) * 4, 64:128], in0=pv[:, :, 64:128],
                                                                 in1=maskT[:, 64:128].unsqueeze(1).to_broadcast([128, 4, 64]),
                                                                 op=ALU.mult),
                          reads=[t_psf[2 + g2], t_const], writes=[t_AT])
                    P.dve(lambda e, g2=g2, pv=pv: e.tensor_tensor(out=AT_sb[0:64, g2 * 4:(g2 + 1) * 4, 0:64], in0=pv[0:64, :, 0:64],
                                                                 in1=maskT[0:64, 0:64].unsqueeze(1).to_broadcast([64, 4, 64]),
                                                                 op=ALU.mult),
                          reads=[t_psf[2 + g2], t_const], writes=[t_AT])
            Su = S_all[:, scol:scol + dv]
            St = S_tmp[:, 0:dv]
            if full:
                P.act(lambda e: e.activation(out=S_bf[:, 0, 0:dv], in_=Su, func=AF.Copy), reads=[tS], writes=[t_Sbf])
            per_bank = 512 // dv
            kvp = [4, 5, 2, 3] if full else [3, 4, 5]
            for n in range(NT):
                bank = kvp[(n // per_bank) % len(kvp)]
                off = (n % per_bank) * dv
                P.pe(lambda e, n=n, bank=bank, off=off: e.matmul(psf[bank][:, off:off + dv], lhsT=khat[:, n, :],
                                                                rhs=v_tok[:, n, 0:dv], start=True, stop=True),
                     reads=[t_khat, t_v], writes=[t_psf[bank]])
            for n in range(NT):
                bank = kvp[(n // per_bank) % len(kvp)]
                off = (n % per_bank) * dv
                if full:
                    src, dst = (Su, St) if n % 2 == 0 else (St, Su)
                    tsrc, tdst = (tS, t_Stmp) if n % 2 == 0 else (t_Stmp, tS)
                else:
                    src, dst, tsrc, tdst = Su, Su, tS, tS
                P.dve(lambda e, n=n, bank=bank, off=off, src=src, dst=dst: e.scalar_tensor_tensor(
                    out=dst, in0=src, scalar=sm[:, 2, n:n + 1], in1=psf[bank][:, off:off + dv],
                    op0=ALU.mult, op1=ALU.add), reads=[tsrc, t_sm, t_psf[bank]], writes=[tdst])
                if full and n < NT - 1:
                    P.act(lambda e, n=n, dst=dst: e.activation(out=S_bf[:, n + 1, 0:dv], in_=dst, func=AF.Copy),
                          reads=[tdst], writes=[t_Sbf])
            if full:
                for n in range(NT):
                    pa = next_pa()
                    P.pe(lambda e, n=n, pa=pa: e.matmul(psf[pa][:, 0:dv], lhsT=AT_sb[:, n, :], rhs=v_tok[:, n, 0:dv],
                                                        start=True, stop=False),
                         reads=[t_AT, t_v], writes=[t_psf[pa]])
                    P.pe(lambda e, n=n, pa=pa: e.matmul(psf[pa][:, 0:dv], lhsT=qs[:, n * 128:(n + 1) * 128],
                                                        rhs=S_bf[:, n, 0:dv], start=False, stop=True),
                         reads=[t_qs, t_Sbf], writes=[t_psf[pa]])
                    P.act(lambda e, n=n, pa=pa: e.activation(out=o_all[:, n, 0:dv], in_=psf[pa][:, 0:dv], func=AF.Copy),
                          reads=[t_psf[pa]], writes=[t_osb[n]])
                    P.dve(lambda e, n=n: e.scalar_tensor_tensor(out=junk[:, 0:dv], in0=o_all[:, n, 0:dv], scalar=1.0, in1=o_all[:, n, 0:dv],
                                                                op0=ALU.mult, op1=ALU.mult, accum_out=ost[:, 0, n:n + 1]),
                          reads=[t_osb[n]], writes=[t_junk, t_ost])
                rstd_of(ost[:, 3, :], ost[:, 0, :], ost[:, 1, :], dv, t_ost)
                for n in range(NT):
                    P.dve(lambda e, n=n: e.scalar_tensor_tensor(
                        out=o_tok[:, n, scol:scol + dv], in0=o_all[:, n, 0:dv], scalar=ost[:, 3, n:n + 1],
                        in1=sg_tok[:, n, 0:dv], op0=ALU.mult, op1=ALU.mult),
                        reads=[t_osb[n], t_ost, t_sg], writes=[t_otok[n]])

        o_flat = o_tok.rearrange("p a b -> p (a b)")
        KF = [kf, o_flat[:, 0:2 * T].bitcast(F32)]
        T1 = [t1, o_flat[:, 2 * T:4 * T].bitcast(F32)]
        VT = [v_tok, o_flat[:, 4 * T:4 * T + NT * 256].rearrange("p (a b) -> p a b", b=256)]
        t_KF = [t_kf, P.tok("kf2")]
        t_T1 = [t_t1, P.tok("t1b")]
        t_VT = [t_v, P.tok("v2")]

        def unit_cfg(u):
            gla = u < 4
            dv = 256 if gla else 128
            sc = (-1.0 / 16.0) if gla else 1.0
            if gla:
                ck, cv, scol = 512 + u * 128, 1024 + u * 256, u * 256
            else:
                hu = u - 4
                ck, cv, scol = 4112 + hu * 128, 5136 + hu * 128, 1024 + hu * 128
            return gla, dv, sc, ck, cv, scol

        def state_A_load(u):
            gla, dv, sc, ck, cv, scol = unit_cfg(u)
            s_ = u % 2
            wvg = wvg_b[0][:, :, s_ * 256:(s_ + 1) * 256]
            wload(wk_b[s_], wview(w_in, ck, 128), "wk%d" % s_, t_wk[s_])
            wload(wvg[:, :, 0:dv], wview(w_in, cv, dv), "wvg%d" % s_, t_wvgh[s_])

        def state_A_pe(u):
            gla, dv, sc, ck, cv, scol = unit_cfg(u)
            s_ = u % 2
            wvg = wvg_b[0][:, :, s_ * 256:(s_ + 1) * 256]
            for half in range(2):
                proj_fm(wk_b[s_], t_wk[s_], aT, t_aT, half, half)
            per_bank = 512 // dv
            for i in range(NT):
                bank = 2 + i // per_bank
                off = (i % per_bank) * dv
                for kt in range(KT):
                    P.pe(lambda e, kt=kt, i=i, bank=bank, off=off: e.matmul(psf[bank][:, off:off + dv], lhsT=aT[:, kt, i * 128:(i + 1) * 128],
                                                                            rhs=wvg[:, kt, 0:dv], start=(kt == 0), stop=(kt == KT - 1)),
                         reads=[t_wvgh[s_], t_aT[i]], writes=[t_psf[bank]])

        def state_A_kevac(u):
            gla, dv, sc, ck, cv, scol = unit_cfg(u)
            s_ = u % 2
            for half in range(2):
                pa = half
                sl = slice(half * 512, (half + 1) * 512)
                if gla:
                    copy_evac(KF[s_][:, sl], psf[pa][:], [t_psf[pa]], [t_KF[s_]])
                    P.pe(lambda e, pa=pa, sl=sl: e.matmul(psf[pa][:], lhsT=wup_sb[:, u * 128:(u + 1) * 128],
                                                         rhs=glrT[:, sl], start=True, stop=True),
                         reads=[t_par, t_glr], writes=[t_psf[pa]])
                    P.act(lambda e, pa=pa, sl=sl: e.activation(out=T1[s_][:, sl], in_=psf[pa][:], func=AF.Exp,
                                                              scale=-1.0, bias=nbalpha[:, u:u + 1]),
                          reads=[t_psf[pa], t_par], writes=[t_T1[s_]])
                else:
                    sigmoid_to(T1[s_][:, sl], psf[pa][:], [t_psf[pa]], t_T1[s_])

        def state_A_evac(u):
            gla, dv, sc, ck, cv, scol = unit_cfg(u)
            s_ = u % 2
            per_bank = 512 // dv
            for b in range(NT // per_bank):
                bank = 2 + b
                copy_evac(VT[s_][:, b * per_bank:(b + 1) * per_bank, 0:dv],
                          psf[bank][:].rearrange("p (a b) -> p a b", b=dv), [t_psf[bank]], [t_VT[s_]])

        def state_B_chain(u):
            gla, dv, sc, ck, cv, scol = unit_cfg(u)
            s_ = u % 2
            kf_, t1_, tkf_, tt1_ = KF[s_], T1[s_], t_KF[s_], t_T1[s_]
            if gla:
                for half in range(2):
                    P.act(lambda e, half=half: e.activation(out=t1_[:, half * 512:(half + 1) * 512], in_=t1_[:, half * 512:(half + 1) * 512],
                                                            func=AF.Ln, bias=1.0), reads=[tt1_], writes=[tt1_])
            else:
                hu = u - 4
                P.dve(lambda e: e.tensor_scalar(out=t2, in0=t1_, scalar1=oml_sb[:, hu:hu + 1],
                                                scalar2=lb_sb[:, hu:hu + 1], op0=ALU.mult, op1=ALU.add),
                      reads=[tt1_, t_par], writes=[t_t2])
                P.dve(lambda e: e.tensor_scalar(out=kf_, in0=t2, scalar1=-1.0, scalar2=1.0,
                                                op0=ALU.mult, op1=ALU.add), reads=[t_t2], writes=[tkf_])
                P.act(lambda e: e.activation(out=t1_, in_=t2, func=AF.Ln), reads=[t_t2], writes=[tt1_])
            P.dve(lambda e: e.tensor_tensor_scan(out=t2, data0=scanmask, data1=t1_, initial=0.0,
                                                 op0=ALU.mult, op1=ALU.add),
                  reads=[tt1_, t_const], writes=[t_t2])
            c3 = t2.rearrange("p (n j) -> p n j", j=128)
            cref = c3[:, :, 63:64]
            clast = c3[:, :, 127:128]
            cref2 = cref.rearrange("p n o -> p (n o)")
            clast2 = clast.rearrange("p n o -> p (n o)")
            P.dve(lambda e: e.tensor_tensor(out=t1_.rearrange("p (n j) -> p n j", j=128), in0=c3,
                                            in1=cref.to_broadcast([128, NT, 128]), op=ALU.subtract),
                  reads=[t_t2], writes=[tt1_])
            P.dve(lambda e: e.tensor_tensor(out=sm[:, 3, :], in0=clast2, in1=cref2, op=ALU.subtract),
                  reads=[t_t2], writes=[t_sm])
            P.dve(lambda e: e.tensor_tensor_scan(out=sm2[:, 0, :], data0=scanmask[:, 1:1 + NT], data1=clast2, initial=0.0,
                                                 op0=ALU.mult, op1=ALU.add), reads=[t_t2, t_const], writes=[t_sm])
            P.dve(lambda e: e.tensor_scalar(out=sm2[:, 1, :], in0=sm2[:, 0, :], scalar1=-1.0, scalar2=sm2[:, 0, NT - 1:NT],
                                            op0=ALU.mult, op1=ALU.add), reads=[t_sm], writes=[t_sm])
            P.dve(lambda e: e.tensor_tensor(out=sm[:, 3, :], in0=sm[:, 3, :], in1=sm2[:, 1, :], op=ALU.add),
                  reads=[t_sm], writes=[t_sm])
            P.act(lambda e: e.activation(out=sm[:, 1, :], in_=sm[:, 3, :], func=AF.Exp, scale=sc),
                  reads=[t_sm], writes=[t_sm])
            P.act(lambda e: e.activation(out=sm[:, 2, 0:1], in_=sm2[:, 0, NT - 1:NT], func=AF.Exp, scale=sc),
                  reads=[t_sm], writes=[t_sm])
            P.act(lambda e: e.activation(out=t3, in_=t1_, func=AF.Exp, scale=-sc), reads=[tt1_], writes=[t_t3])
            P.dve(lambda e: e.tensor_tensor(out=ktl, in0=kf_, in1=t3, op=ALU.mult),
                  reads=[tkf_, t_t3], writes=[t_ktl])
            P.pool(lambda e: e.tensor_tensor(out=khT.rearrange("p (n j) -> p n j", j=128),
                                             in0=ktl.rearrange("p (n j) -> p n j", j=128),
                                             in1=sm[:, 1, :].unsqueeze(2).to_broadcast([128, NT, 128]), op=ALU.mult),
                   reads=[t_ktl, t_sm], writes=[t_khT])

        def state_B_pe(u):
            gla, dv, sc, ck, cv, scol = unit_cfg(u)
            s_ = u % 2
            tS = t_S[u]
            pb = 7
            for n in range(NT):
                P.pe(lambda e, n=n: e.transpose(out=psb(pb)[:, n * 128:(n + 1) * 128],
                                               in_=khT[:, n * 128:(n + 1) * 128], identity=ident_bf),
                     reads=[t_khT, t_const], writes=[t_psf[pb]])
            copy_evac(khat, psb(pb).rearrange("p (n c) -> p n c", c=128), [t_psf[pb]], [t_khat])
            Su = S_all[:, scol:scol + dv]
            for n in range(NT):
                P.pe(lambda e, n=n: e.matmul(psf[6][:, 0:dv], lhsT=khat[:, n, :], rhs=VT[s_][:, n, 0:dv],
                                            start=(n == 0), stop=(n == NT - 1)),
                     reads=[t_khat, t_VT[s_]], writes=[t_psf[6]])
            P.dve(lambda e: e.scalar_tensor_tensor(out=Su, in0=Su, scalar=sm[:, 2, 0:1], in1=psf[6][:, 0:dv],
                                                   op0=ALU.mult, op1=ALU.add), reads=[tS, t_sm, t_psf[6]], writes=[tS])

        def mixer_segment_state():
            wload(wlr, wview(w_in, 3072, 16), "wlr", t_wlr)
            for half in range(2):
                pa = half
                proj_fm(wlr, t_wlr, aT, t_aT, half, pa, M=16)
                copy_evac(glrT[:, half * 512:(half + 1) * 512], psf[pa][0:16, :], [t_psf[pa]], [t_glr])
            nu = DBG["units"]
            if nu == 0:
                return
            state_A_load(0)
            if nu > 1:
                state_A_load(1)
            state_A_pe(0)
            state_A_kevac(0)
            state_A_evac(0)
            for u in range(nu):
                if u + 2 < nu:
                    state_A_load(u + 2)
                if u + 1 < nu:
                    state_A_pe(u + 1)
                state_B_chain(u)
                emit_conv(1)
                if u + 1 < nu:
                    state_A_kevac(u + 1)
                    state_A_evac(u + 1)
                state_B_pe(u)

        def mixer_segment(full):
            if not full and "nopipe" not in DBG["skip"]:
                mixer_segment_state()
                return
            rr["pool"] = [0, 1] if full else [0, 1, 2]
            rr["pa"] = 0
            wload(wlr, wview(w_in, 3072, 16), "wlr", t_wlr)
            for half in range(2):
                pa = next_pa()
                proj_fm(wlr, t_wlr, aT, t_aT, half, pa, M=16)
                copy_evac(glrT[:, half * 512:(half + 1) * 512], psf[pa][0:16, :], [t_psf[pa]], [t_glr])
            for u in range(DBG["units"]):
                mixer_unit(u, full and DBG["full"])

        load_gbc(g_mix)
        for seg in range(DBG["seg0"], NPREV + 1):
            src = x_prev if seg < NPREV else x_own
            r0 = seg * T if seg < NPREV else 0
            for i in range(NT):
                P.dma("sp", lambda e, i=i, src=src, r0=r0: e.dma_start(out=xs[i % 2], in_=src[r0 + i * 128: r0 + (i + 1) * 128, :]),
                      "xs%d" % (i % 2), writes=[t_xs[i % 2]])
                sumsq(xs[i % 2], t_xs[i % 2], stat[:, 0, i:i + 1], D)
                rstd_of(stat[:, 3, i:i + 1], stat[:, 0, i:i + 1], stat[:, 1, i:i + 1], D, t_stat)
                P.dve(lambda e, i=i: e.scalar_tensor_tensor(out=a_tok, in0=xs[i % 2], scalar=stat[:, 3, i:i + 1],
                                                            in1=gbc, op0=ALU.mult, op1=ALU.mult),
                      reads=[t_xs[i % 2], t_stat, t_gbc], writes=[t_atok])
                transpose_tile(a_tok, t_atok, aT, t_aT[i], slice(i * 128, (i + 1) * 128))
            if seg == NPREV and seg > DBG["seg0"]:
                P.barrier()
            mixer_segment(full=(seg == NPREV))

        if "barrier" not in DBG["skip"]:
            P.barrier()
        A.top = M_AFTER_OTOK
        h = A.alloc([128, NT, D])
        t_h = P.toks("h", NT)
        wbig = [A.alloc([128, KT, 512], BF16) for _ in range(2)]
        t_wbig = P.toks("wbig", 2)
        M_AFTER_WBIG = A.top
        for i in range(NT):
            P.dma("sp", lambda e, i=i: e.dma_start(out=h[:, i, :], in_=x_own[i * 128:(i + 1) * 128, :]), "h%d" % i,
                  writes=[t_h[i]])

        def proj_residual(w, srcT, src_toks):
            for c in range(4):
                wb = c % 2
                wload(wbig[wb], wview(w, c * 512, 512), "wbig%d" % wb, t_wbig[wb])
                emit_conv(1)
                for i in range(NT):
                    pa = next_pa()
                    for kt in range(KT):
                        P.pe(lambda e, kt=kt, i=i, pa=pa, wb=wb: e.matmul(psf[pa][:], lhsT=srcT[:, kt, i * 128:(i + 1) * 128],
                                                                          rhs=wbig[wb][:, kt, :],
                                                                          start=(kt == 0), stop=(kt == KT - 1)),
                             reads=[t_wbig[wb], src_toks[i]], writes=[t_psf[pa]])
                    P.dve(lambda e, i=i, pa=pa, c=c: e.tensor_tensor(out=h[:, i, c * 512:(c + 1) * 512],
                                                                    in0=psf[pa][:], in1=h[:, i, c * 512:(c + 1) * 512],
                                                                    op=ALU.add),
                          reads=[t_psf[pa], t_h[i]], writes=[t_h[i]])

        if DBG["proj"]:
            for i in range(NT):
                transpose_tile(o_tok[:, i, :], t_otok[i], aT, t_aT[i], slice(i * 128, (i + 1) * 128))
            proj_residual(w_mo, aT, t_aT)

        if stage >= 2:
            P.barrier()
            xa_base = M_PERSIST
            A.top = xa_base
            mem_f = A.alloc([128, 2, D])
            memT = A.alloc([128, KT, 256], BF16)
            assert A.top <= M_AFTER_OTOK
            A.top = M_AFTER_WBIG
            kT = A.alloc([128, KT, 256], BF16)
            v_mem = A.alloc([128, 2, D], BF16)
            p_sb = A.alloc([128, 4, 256], BF16)
            pT_sb = A.alloc([128, 8, 128], BF16)
            xst = A.alloc([128, 16])
            t_kT, t_vmem, t_p, t_pT, t_xst = P.toks("xa", 5)
            t_memf = P.toks("memf", 2)
            t_memT = P.toks("memT", 2)
            for i in range(2):
                P.dma("sp", lambda e, i=i: e.dma_start(out=mem_f[:, i, :], in_=mem[i * 128:(i + 1) * 128, :]), "memf%d" % i,
                      writes=[t_memf[i]])
            load_gbc(g_mem)
            norm_to_T(lambda i: mem_f[:, i, :], t_memf, 2, memT, t_memT)
            for c in range(8):
                wb = c % 2
                wload(wbig[wb], wview(w_xkv, c * 512, 512), "wbig%d" % wb, t_wbig[wb])
                if c < 4:
                    for j in range(4):
                        pa = next_pa()
                        for kt in range(KT):
                            P.pe(lambda e, kt=kt, j=j, pa=pa, wb=wb: e.matmul(psf[pa][:, 0:256], lhsT=wbig[wb][:, kt, j * 128:(j + 1) * 128],
                                                                              rhs=memT[:, kt, :], start=(kt == 0), stop=(kt == KT - 1)),
                                 reads=[t_wbig[wb]] + t_memT, writes=[t_psf[pa]])
                        copy_evac(kT[:, c * 4 + j, :], psf[pa][:, 0:256], [t_psf[pa]], [t_kT])
                else:
                    for mt in range(2):
                        pa = next_pa()
                        for kt in range(KT):
                            P.pe(lambda e, kt=kt, mt=mt, pa=pa, wb=wb: e.matmul(psf[pa][:], lhsT=memT[:, kt, mt * 128:(mt + 1) * 128],
                                                                                rhs=wbig[wb][:, kt, :], start=(kt == 0), stop=(kt == KT - 1)),
                                 reads=[t_wbig[wb], t_memT[mt]], writes=[t_psf[pa]])
                        copy_evac(v_mem[:, mt, (c - 4) * 512:(c - 3) * 512], psf[pa][:], [t_psf[pa]], [t_vmem])
            P.barrier()
            A.top = xa_base
            qT = A.alloc([128, KT, T], BF16)
            t_qT = P.toks("qT", 2)
            assert A.top <= M_AFTER_OTOK
            load_gbc(g_xa)
            norm_to_T(lambda i: h[:, i, :], t_h, NT, aT, t_aT)
            for c in range(4):
                wb = c % 2
                wload(wbig[wb], wview(w_xq, c * 512, 512), "wbig%d" % wb, t_wbig[wb])
                emit_conv(1)
                for j in range(4):
                    for half in range(2):
                        pa = next_pa()
                        proj_fm(wbig[wb][:, :, j * 128:(j + 1) * 128], t_wbig[wb], aT, t_aT, half, pa)
                        evac(lambda e, pa=pa, c=c, j=j, half=half: e.activation(out=qT[:, c * 4 + j, half * 512:(half + 1) * 512],
                                                                                in_=psf[pa][:], func=AF.Copy, scale=512.0 ** -0.5),
                             lambda e, pa=pa, c=c, j=j, half=half: e.tensor_scalar(out=qT[:, c * 4 + j, half * 512:(half + 1) * 512],
                                                                                   in0=psf[pa][:], scalar1=512.0 ** -0.5, scalar2=None,
                                                                                   op0=ALU.mult),
                             [t_psf[pa]], [t_qT[half]])
            for i in range(NT):
                tsl = slice(i * 128, (i + 1) * 128)
                for pr in range(2):
                    bank = 2 + pr
                    for hh in range(2):
                        hd = pr * 2 + hh
                        for j in range(4):
                            P.pe(lambda e, bank=bank, hh=hh, hd=hd, j=j, tsl=tsl: e.matmul(psf[bank][:, hh * 256:(hh + 1) * 256],
                                                                                  lhsT=qT[:, hd * 4 + j, tsl], rhs=kT[:, hd * 4 + j, :],
                                                                                  start=(j == 0), stop=(j == 3)),
                                 reads=[t_qT[i // 4], t_kT], writes=[t_psf[bank]])
                    P.dve(lambda e, bank=bank, pr=pr: e.tensor_reduce(out=xst[:, pr * 2:pr * 2 + 2],
                                                                     in_=psf[bank][:].rearrange("p (a b) -> p a b", b=256),
                                                                     axis=AX.X, op=ALU.max),
                          reads=[t_psf[bank]], writes=[t_xst])
                    P.dve(lambda e, pr=pr: e.tensor_scalar(out=xst[:, 4 + pr * 2:6 + pr * 2], in0=xst[:, pr * 2:pr * 2 + 2],
                                                           scalar1=-1.0, scalar2=None, op0=ALU.mult),
                          reads=[t_xst], writes=[t_xst])
                    for hh in range(2):
                        hd = pr * 2 + hh
                        P.act(lambda e, bank=bank, hh=hh, hd=hd: e.activation(out=p_sb[:, hd, :], in_=psf[bank][:, hh * 256:(hh + 1) * 256],
                                                                             func=AF.Exp, bias=xst[:, 4 + hd:5 + hd],
                                                                             accum_out=xst[:, 8 + hd:9 + hd]),
                              reads=[t_psf[bank], t_xst], writes=[t_p, t_xst])
                P.dve(lambda e: e.reciprocal(out=xst[:, 12:16], in_=xst[:, 8:12]), reads=[t_xst], writes=[t_xst])
                pb = next_pb()
                for hd in range(4):
                    for mt in range(2):
                        P.pe(lambda e, hd=hd, mt=mt, pb=pb: e.transpose(out=psb(pb)[:, (hd * 2 + mt) * 128:(hd * 2 + mt + 1) * 128],
                                                                in_=p_sb[:, hd, mt * 128:(mt + 1) * 128], identity=ident_bf),
                             reads=[t_p, t_const], writes=[t_psf[pb]])
                copy_evac(pT_sb, psb(pb).rearrange("p (n c) -> p n c", c=128), [t_psf[pb]], [t_pT])
                for hd in range(4):
                    pa = next_pa()
                    for mt in range(2):
                        P.pe(lambda e, hd=hd, mt=mt, pa=pa: e.matmul(psf[pa][:], lhsT=pT_sb[:, hd * 2 + mt, :],
                                                                     rhs=v_mem[:, mt, hd * 512:(hd + 1) * 512],
                                                                     start=(mt == 0), stop=(mt == 1)),
                             reads=[t_pT, t_vmem], writes=[t_psf[pa]])
                    evac(lambda e, hd=hd, pa=pa: e.activation(out=a_tok[:, hd * 512:(hd + 1) * 512], in_=psf[pa][:], func=AF.Copy,
                                                              scale=xst[:, 12 + hd:13 + hd]),
                         lambda e, hd=hd, pa=pa: e.tensor_scalar(out=a_tok[:, hd * 512:(hd + 1) * 512], in0=psf[pa][:],
                                                                 scalar1=xst[:, 12 + hd:13 + hd], scalar2=None, op0=ALU.mult),
                         [t_psf[pa], t_xst], [t_atok])
                transpose_tile(a_tok, t_atok, aT, t_aT[i], tsl)
            proj_residual(w_xo, aT, t_aT)

        if stage >= 3:
            P.barrier()
            A.top = M_PERSIST - KT * T * 2
            moe_lo = A.top
            a3_tok = A.alloc([128, NT, D], BF16)
            t_a3 = P.toks("a3", NT)
            G_all = A.alloc([128, NT, 32])
            ind_all = A.alloc([128, NT, 32])
            ind_bf = A.alloc([128, NT, 32], BF16)
            rank_all = A.alloc([128, NT, 32])
            GT = A.alloc([32, T])
            rankT = A.alloc([32, T])
            moe_ov = A.top
            wr_sb = A.alloc([128, KT, 36])
            br_bc = A.alloc([128, 36])
            lg = A.alloc([128, 36])
            rt = A.alloc([128, 64])
            ml = A.alloc([128, 32])
            top8 = A.alloc([128, 8])
            a3fT = A.alloc([128, 4, 128])
            a3f = ja.bitcast(F32)
            A.top = moe_ov
            esel = A.alloc([32, 128])
            Sel = A.alloc([128, NT, 128], BF16)
            gbc_sb = A.alloc([128, T])
            GselT = A.alloc([128, T], BF16)
            xeT = A.alloc([128, KT, 128], BF16)
            hb_tmp = A.alloc([128, 512])
            hb = A.alloc([128, 1024], BF16)
            hbT = A.alloc([128, 8, 128], BF16)
            Y_sb = A.alloc([128, 512], BF16)
            assert A.top <= M_AFTER_OTOK, ("moe work region overflow", A.top, M_AFTER_OTOK)
            A.top = M_AFTER_OTOK + NT * D * 4
            NSLOT = (ARENA_BYTES - A.top) // (KT * 512 * 2)
            slots = [A.alloc([128, KT, 512], BF16) for _ in range(NSLOT)]
            slots.append(gj.rearrange("p (a b) -> p a b", b=512))
            NSLOT += 1
            t_slot = P.toks("slot", NSLOT)
            (t_G, t_ind, t_rank, t_GT, t_rankT, t_esel, t_wr, t_lg, t_rt, t_ml, t_top8, t_a3f_, t_a3fT, t_Sel,
             t_gbcsb, t_GselT, t_xeT, t_hbtmp, t_hb, t_hbT, t_Y) = P.toks("moe", 21)
            t_a3f = t_junk

            emit_conv(len(conv_list))
            P.dma("sp", lambda e: e.dma_start(out=wr_sb[:, :, 0:4], in_=w_rg.rearrange("(kt p) n -> p kt n", p=128)), "wr", writes=[t_wr])
            P.dma("sp", lambda e: e.dma_start(out=wr_sb[:, :, 4:36], in_=w_re.rearrange("(kt p) n -> p kt n", p=128)), "wr", writes=[t_wr])
            P.dma("sp", lambda e: e.dma_start(out=br_bc[:, 0:4], in_=b_rg.partition_broadcast(128)), "wr", writes=[t_wr])
            P.dma("sp", lambda e: e.dma_start(out=br_bc[:, 4:36], in_=b_re.partition_broadcast(128)), "wr", writes=[t_wr])
            load_gbc(g_ffn)
            rms_stats(lambda i: h[:, i, :], t_h, NT)
            for i in range(NT):
                P.dve(lambda e, i=i: e.scalar_tensor_tensor(out=a3f, in0=h[:, i, :], scalar=stat[:, 3, i:i + 1],
                                                            in1=gbc, op0=ALU.mult, op1=ALU.mult),
                      reads=[t_h[i], t_stat, t_gbc], writes=[t_a3f, t_atok])
                P.act(lambda e, i=i: e.activation(out=a3_tok[:, i, :], in_=a3f, func=AF.Copy), reads=[t_a3f], writes=[t_a3[i]])
                for q4 in range(4):
                    bank = 2 + (q4 % 2)
                    for j in range(4):
                        kt = q4 * 4 + j
                        P.pe(lambda e, bank=bank, j=j, kt=kt: e.transpose(out=psf[bank][:, j * 128:(j + 1) * 128],
                                                                         in_=a3f[:, kt * 128:(kt + 1) * 128], identity=ident_f),
                             reads=[t_a3f, t_const], writes=[t_psf[bank]])
                    copy_evac(a3fT, psf[bank][:].rearrange("p (j c) -> p j c", c=128), [t_psf[bank]], [t_a3fT])
                    for j in range(4):
                        kt = q4 * 4 + j
                        P.pe(lambda e, j=j, kt=kt: e.matmul(psf[4][:, 0:36], lhsT=a3fT[:, j, :], rhs=wr_sb[:, kt, :],
                                                           start=(kt == 0), stop=(kt == KT - 1)),
                             reads=[t_a3fT, t_wr], writes=[t_psf[4]])
                P.dve(lambda e: e.tensor_tensor(out=lg, in0=psf[4][:, 0:36], in1=br_bc, op=ALU.add),
                      reads=[t_psf[4], t_wr], writes=[t_lg])
                P.dve(lambda e: e.tensor_reduce(out=rt[:, 0:1], in_=lg[:, 0:4], axis=AX.X, op=ALU.max), reads=[t_lg], writes=[t_rt])
                P.dve(lambda e: e.tensor_scalar(out=rt[:, 1:2], in0=rt[:, 0:1], scalar1=-1.0, scalar2=None, op0=ALU.mult),
                      reads=[t_rt], writes=[t_rt])
                P.act(lambda e: e.activation(out=rt[:, 4:8], in_=lg[:, 0:4], func=AF.Exp, bias=rt[:, 1:2], accum_out=rt[:, 2:3]),
                      reads=[t_lg, t_rt], writes=[t_rt])
                P.dve(lambda e: e.reciprocal(out=rt[:, 3:4], in_=rt[:, 2:3]), reads=[t_rt], writes=[t_rt])
                P.dve(lambda e: e.tensor_scalar(out=rt[:, 8:12], in0=lg[:, 0:4], scalar1=rt[:, 0:1], scalar2=None, op0=ALU.is_equal),
                      reads=[t_lg, t_rt], writes=[t_rt])
                P.dve(lambda e: e.tensor_scalar(out=rt[:, 12:16], in0=rt[:, 8:12], scalar1=-1.0, scalar2=1e30, op0=ALU.add, op1=ALU.mult),
                      reads=[t_rt], writes=[t_rt])
                P.dve(lambda e: e.tensor_tensor(out=ml.rearrange("p (g k) -> p g k", k=8),
                                                in0=lg[:, 4:36].rearrange("p (g k) -> p g k", k=8),
                                                in1=rt[:, 8:12].unsqueeze(2).to_broadcast([128, 4, 8]), op=ALU.mult),
                      reads=[t_lg, t_rt], writes=[t_ml])
                P.dve(lambda e: e.tensor_tensor(out=ml.rearrange("p (g k) -> p g k", k=8),
                                                in0=ml.rearrange("p (g k) -> p g k", k=8),
                                                in1=rt[:, 12:16].unsqueeze(2).to_broadcast([128, 4, 8]), op=ALU.add),
                      reads=[t_ml, t_rt], writes=[t_ml])
                P.dve(lambda e: e.max(out=top8, in_=ml), reads=[t_ml], writes=[t_top8])
                P.dve(lambda e: e.tensor_scalar(out=rt[:, 16:17], in0=top8[:, 0:1], scalar1=-1.0, scalar2=None, op0=ALU.mult),
                      reads=[t_top8], writes=[t_rt])
                P.act(lambda e: e.activation(out=rt[:, 17:18], in_=top8[:, 1:2], func=AF.Exp, bias=rt[:, 16:17]),
                      reads=[t_top8, t_rt], writes=[t_rt])
                P.dve(lambda e: e.tensor_scalar(out=rt[:, 18:19], in0=rt[:, 17:18], scalar1=1.0, scalar2=None, op0=ALU.add),
                      reads=[t_rt], writes=[t_rt])
                P.dve(lambda e: e.reciprocal(out=rt[:, 19:20], in_=rt[:, 18:19]), reads=[t_rt], writes=[t_rt])
                P.dve(lambda e: e.tensor_tensor(out=rt[:, 20:21], in0=rt[:, 19:20], in1=rt[:, 3:4], op=ALU.mult),
                      reads=[t_rt], writes=[t_rt])
                P.dve(lambda e: e.tensor_tensor(out=rt[:, 21:22], in0=rt[:, 20:21], in1=rt[:, 17:18], op=ALU.mult),
                      reads=[t_rt], writes=[t_rt])
                P.dve(lambda e, i=i: e.tensor_scalar(out=G_all[:, i, :], in0=ml, scalar1=top8[:, 0:1], scalar2=rt[:, 20:21],
                                                     op0=ALU.is_equal, op1=ALU.mult), reads=[t_ml, t_top8, t_rt], writes=[t_G])
                P.dve(lambda e: e.tensor_scalar(out=rt[:, 32:64], in0=ml, scalar1=top8[:, 1:2], scalar2=rt[:, 21:22],
                                                op0=ALU.is_equal, op1=ALU.mult), reads=[t_ml, t_top8, t_rt], writes=[t_rt])
                P.dve(lambda e, i=i: e.tensor_tensor(out=G_all[:, i, :], in0=G_all[:, i, :], in1=rt[:, 32:64], op=ALU.add),
                      reads=[t_G, t_rt], writes=[t_G])
                P.dve(lambda e, i=i: e.tensor_scalar(out=ind_all[:, i, :], in0=G_all[:, i, :], scalar1=0.0, scalar2=None, op0=ALU.is_gt),
                      reads=[t_G], writes=[t_ind])
                P.dve(lambda e, i=i: e.tensor_copy(out=ind_bf[:, i, :], in_=ind_all[:, i, :]), reads=[t_ind], writes=[t_ind])
            for i in range(NT):
                P.pe(lambda e, i=i: e.matmul(psf[5][:, 0:32], lhsT=ltri, rhs=ind_bf[:, i, :], start=True, stop=(i == 0)),
                     reads=[t_ind, t_const], writes=[t_psf[5]])
                for i2 in range(i):
                    P.pe(lambda e, i2=i2, i=i: e.matmul(psf[5][:, 0:32], lhsT=ones_bf, rhs=ind_bf[:, i2, :], start=False, stop=(i2 == i - 1)),
                         reads=[t_ind, t_const], writes=[t_psf[5]])
                copy_evac(rank_all[:, i, :], psf[5][:, 0:32], [t_psf[5]], [t_rank])
            for (src_all, src_t, dst, dst_t) in ((G_all, t_G, GT, t_GT), (rank_all, t_rank, rankT, t_rankT)):
                for half in range(2):
                    bank = 2 + half
                    for j in range(4):
                        i = half * 4 + j
                        P.pe(lambda e, bank=bank, j=j, i=i, src_all=src_all: e.transpose(out=psf[bank][0:32, j * 128:(j + 1) * 128],
                                                                                         in_=src_all[:, i, :], identity=ident_f),
                             reads=[src_t, t_const], writes=[t_psf[bank]])
                    copy_evac(dst[:, half * 512:(half + 1) * 512], psf[bank][0:32, :], [t_psf[bank]], [dst_t])

            P.barrier()
            slot_rr = {"i": 0}

            def next_slot():
                s_ = slot_rr["i"] % NSLOT
                slot_rr["i"] += 1
                return s_

            def eload(dst, w32, w16, c0, n, r0, rows, key, tok, conv):
                if conv:
                    src = w16[r0:r0 + rows, c0:c0 + n].rearrange("(kt p) n -> p kt n", p=128)
                    P.dma("sp", lambda e: e.dma_start(out=dst, in_=src), "H" + key, reads=[conv_state["last_tok"]], writes=[tok])
                else:
                    src = w32[r0:r0 + rows, c0:c0 + n].rearrange("(kt p) n -> p kt n", p=128)
                    P.dma("pool", lambda e: e.dma_start(out=dst, in_=src), key, writes=[tok])

            def expert(ex):
                conv = (ex in CONV_SET) and NCONV > 0
                for i in range(NT):
                    P.dve(lambda e, i=i: e.tensor_scalar(out=Sel[:, i, :], in0=iota_row, scalar1=rank_all[:, i, ex:ex + 1],
                                                         scalar2=ind_all[:, i, ex:ex + 1], op0=ALU.is_equal, op1=ALU.mult),
                          reads=[t_rank, t_ind, t_const], writes=[t_Sel])
                P.pool(lambda e: e.tensor_copy(out=esel, in_=ident_f[0:32, ex:ex + 1].to_broadcast([32, 128])),
                       reads=[t_const], writes=[t_esel])
                for kt in range(KT):
                    bank = kt // 4
                    for i in range(NT):
                        P.pe(lambda e, kt=kt, i=i, bank=bank: e.matmul(psf[bank][:, (kt % 4) * 128:(kt % 4 + 1) * 128],
                                                                       lhsT=a3_tok[:, i, kt * 128:(kt + 1) * 128], rhs=Sel[:, i, :],
                                                                       start=(i == 0), stop=(i == NT - 1)),
                             reads=[t_a3[i], t_Sel], writes=[t_psf[bank]])
                for bank in range(4):
                    copy_evac(xeT[:, bank * 4:(bank + 1) * 4, :], psf[bank][:].rearrange("p (j c) -> p j c", c=128),
                              [t_psf[bank]], [t_xeT])
                for half in range(2):
                    sl = slice(half * 512, (half + 1) * 512)
                    P.pe(lambda e, sl=sl: e.matmul(psf[4][:], lhsT=esel, rhs=GT[:, sl], start=True, stop=True),
                         reads=[t_esel, t_GT], writes=[t_psf[4]])
                    P.act(lambda e, sl=sl: e.activation(out=gbc_sb[:, sl], in_=psf[4][:], func=AF.Copy),
                          reads=[t_psf[4]], writes=[t_gbcsb])
                    P.pe(lambda e, sl=sl: e.matmul(psf[5][:], lhsT=esel, rhs=rankT[:, sl], start=True, stop=True),
                         reads=[t_esel, t_rankT], writes=[t_psf[5]])
                    P.dve(lambda e, sl=sl: e.scalar_tensor_tensor(out=GselT[:, sl], in0=psf[5][:], scalar=iota_p[:, 0:1],
                                                                  in1=gbc_sb[:, sl], op0=ALU.is_equal, op1=ALU.mult),
                          reads=[t_psf[5], t_gbcsb, t_const], writes=[t_GselT])
                for c in range(2):
                    sg_ = next_slot()
                    eload(slots[sg_], w_eg, wbf_g if conv else None, c * 512, 512, ex * D, D, "slot%d" % sg_, t_slot[sg_], conv)
                    su_ = next_slot()
                    eload(slots[su_], w_eu, wbf_u if conv else None, c * 512, 512, ex * D, D, "slot%d" % su_, t_slot[su_], conv)
                    for (bank, s_) in ((4, sg_), (5, su_)):
                        for kt in range(KT):
                            P.pe(lambda e, kt=kt, bank=bank, s_=s_: e.matmul(psf[bank][:], lhsT=xeT[:, kt, :], rhs=slots[s_][:, kt, :],
                                                                             start=(kt == 0), stop=(kt == KT - 1)),
                                 reads=[t_xeT, t_slot[s_]], writes=[t_psf[bank]])
                    silu_to(hb_tmp, psf[4][:], [t_psf[4]], t_hbtmp)
                    P.dve(lambda e, c=c: e.tensor_tensor(out=hb[:, c * 512:(c + 1) * 512], in0=psf[5][:], in1=hb_tmp, op=ALU.mult),
                          reads=[t_psf[5], t_hbtmp], writes=[t_hb])
                pb = next_pb()
                for kt in range(8):
                    P.pe(lambda e, kt=kt: e.transpose(out=psb(pb)[:, kt * 128:(kt + 1) * 128], in_=hb[:, kt * 128:(kt + 1) * 128],
                                                     identity=ident_bf), reads=[t_hb, t_const], writes=[t_psf[pb]])
                copy_evac(hbT, psb(pb).rearrange("p (n c) -> p n c", c=128), [t_psf[pb]], [t_hbT])
                for c2 in range(2):
                    sd_ = next_slot()
                    sl_v = slots[sd_].rearrange("p a b -> p (a b)").rearrange("p (a b) -> p a b", b=1024)
                    eload(sl_v, w_ed, wbf_d if conv else None, c2 * 1024, 1024, ex * 1024, 1024, "slot%d" % sd_, t_slot[sd_], conv)
                    for c3 in range(2):
                        dc = c2 * 2 + c3
                        for kt in range(8):
                            P.pe(lambda e, kt=kt, c3=c3, sl_v=sl_v: e.matmul(psf[0][:], lhsT=hbT[:, kt, :],
                                                                             rhs=sl_v[:, kt, c3 * 512:(c3 + 1) * 512],
                                                                             start=(kt == 0), stop=(kt == 7)),
                                 reads=[t_hbT, t_slot[sd_]], writes=[t_psf[0]])
                        copy_evac(Y_sb, psf[0][:], [t_psf[0]], [t_Y])
                        for i in range(NT):
                            bank = 1 + (i % 3)
                            P.pe(lambda e, i=i, bank=bank: e.matmul(psf[bank][:], lhsT=GselT[:, i * 128:(i + 1) * 128], rhs=Y_sb,
                                                                    start=True, stop=True),
                                 reads=[t_GselT, t_Y], writes=[t_psf[bank]])
                            P.dve(lambda e, i=i, bank=bank, dc=dc: e.tensor_tensor(out=h[:, i, dc * 512:(dc + 1) * 512], in0=psf[bank][:],
                                                                                   in1=h[:, i, dc * 512:(dc + 1) * 512], op=ALU.add),
                                  reads=[t_psf[bank], t_h[i]], writes=[t_h[i]])

            for ex in range(N_EXP):
                expert(ex)

        if stage >= 3:
            P.barrier()
        if final_norm:
            load_gbc(g_fin)
            rms_stats(lambda i: h[:, i, :], t_h, NT)
        for i in range(NT):
            if final_norm:
                P.dve(lambda e, i=i: e.scalar_tensor_tensor(out=h[:, i, :], in0=h[:, i, :], scalar=stat[:, 3, i:i + 1],
                                                            in1=gbc, op0=ALU.mult, op1=ALU.mult),
                      reads=[t_h[i], t_stat, t_gbc], writes=[t_h[i]])
            P.dma("sp", lambda e, i=i: e.dma_start(out=out[i * 128:(i + 1) * 128, :], in_=h[:, i, :]), "out",
                  reads=[t_h[i]], final=True)

        P.arena_high = A.high
        P.build()
    return nc, P, names


def make_in_maps(inputs, names):
    f = lambda a: np.ascontiguousarray(np.asarray(a, dtype=np.float32))
    x = f(inputs["x"])
    shapes = {
        "norm_mix_g": (D,), "w_in": (D, 7184), "w_gla_alpha_up": (16, 512), "b_gla_alpha": (512, 1),
        "gla_out_norm_g": (256,), "hgrn_lb_logits": (2, 1024), "hgrn_out_norm_g": (128,), "w_mix_out": (D, D),
        "norm_xattn_g": (D,), "norm_mem_g": (D,), "w_xattn_q": (D, D), "w_xattn_kv": (D, 2 * D), "w_xattn_out": (D, D),
        "norm_ffn_g": (D,), "w_router_group": (D, 4), "b_router_group": (4,), "w_router_expert": (D, 32),
        "b_router_expert": (32,), "w_expert_gate": (N_EXP * D, 1024), "w_expert_up": (N_EXP * D, 1024),
        "w_expert_down": (N_EXP * 1024, D), "norm_final_g": (D,),
    }
    shared = {k: f(inputs[k]).reshape(shp) for k, shp in shapes.items() if k in names}
    in_maps = []
    for c in range(8):
        b, s = c // 4, c % 4
        xp = np.zeros((NPREV * T, D), np.float32)
        if s > 0:
            xp[(NPREV - s) * T:] = x[b, 0:s * T]
        m = dict(shared)
        m["x_own"] = np.ascontiguousarray(x[b, s * T:(s + 1) * T])
        m["x_prev"] = xp
        if "mem" in names:
            m["mem"] = f(inputs["mem"])[b]
        in_maps.append(m)
    return in_maps


def run(inputs, stage=3, final_norm=True, trace=False, cores=8):
    nc, P, names = build_nc(stage=stage, final_norm=final_norm)
    in_maps = make_in_maps(inputs, names)[:cores]
    res = run_bass_kernel_spmd(nc, in_maps, core_ids=list(range(cores)), trace=trace)
    outp = np.zeros((2, 4096, D), np.float32)
    for c in range(cores):
        b, s = c // 4, c % 4
        outp[b, s * T:(s + 1) * T] = res.results[c]["out"]
    return outp, res


def kernel(**inputs):
    outp, _ = run(inputs)
    return outp
```

```python
import contextlib
import numpy as np
import concourse.bass as bass
import concourse.mybir as mybir
from concourse.bass_utils import run_bass_kernel_spmd

F32 = mybir.dt.float32
BF16 = mybir.dt.bfloat16
I32 = mybir.dt.int32
ALU = mybir.AluOpType
AF = mybir.ActivationFunctionType
AX = mybir.AxisListType

SAME_ENGINE_SYNC = True
D = 2048
T = 1024
NT = 8
KT = 16
EPS = 1e-6
NPREV = 3
N_EXP = 32
CAP = 128


class Tok:
    __slots__ = ("name", "last_write", "readers", "excl")

    def __init__(self, name, excl=False):
        self.name = name
        self.last_write = None
        self.readers = []
        self.excl = excl


class Op:
    __slots__ = ("eng", "fn", "reads", "writes", "dma_key", "idx", "signal", "seq",
                 "waits", "dma_cum", "deps", "barrier")

    def __init__(self, eng, fn, reads, writes, dma_key):
        self.eng = eng
        self.fn = fn
        self.reads = reads
        self.writes = writes
        self.dma_key = dma_key
        self.signal = False
        self.seq = 0
        self.waits = []
        self.dma_cum = 0
        self.deps = []
        self.barrier = False


class Prog:
    ENGS = ("pe", "act", "dve", "pool", "sp")

    def __init__(self, nc):
        self.nc = nc
        self.ops = []
        self.final_dma = []

    def tok(self, name="t"):
        return Tok(name)

    def toks(self, name, n, excl=False):
        return [Tok("%s%d" % (name, i), excl) for i in range(n)]

    def add(self, eng, fn, reads=(), writes=(), dma_key=None):
        op = Op(eng, fn, [t for t in reads if t is not None],
                [t for t in writes if t is not None], dma_key)
        op.idx = len(self.ops)
        self.ops.append(op)
        return op

    def pe(self, fn, reads=(), writes=()):
        return self.add("pe", fn, reads, writes)

    def act(self, fn, reads=(), writes=()):
        return self.add("act", fn, reads, writes)

    def dve(self, fn, reads=(), writes=()):
        return self.add("dve", fn, reads, writes)

    def pool(self, fn, reads=(), writes=()):
        return self.add("pool", fn, reads, writes)

    def dma(self, q, fn, key, reads=(), writes=(), final=False):
        op = self.add(q, fn, reads, writes, dma_key=key)
        if final:
            self.final_dma.append(op)
        return op

    def barrier(self):
        op = Op("sp", lambda e: e.nop(), [], [], None)
        op.barrier = True
        op.idx = len(self.ops)
        self.ops.append(op)
        return op

    def build(self):
        nc = self.nc
        ops = self.ops
        last_eng = {}
        last_dma = {}
        cur_barrier = None
        seen_after = set()
        for op in ops:
            if op.barrier:
                op.deps = [d for d in last_eng.values() if d.eng != "sp"] + \
                          [d for k_, d in last_dma.items() if not str(k_).startswith("cv")]
                for d in op.deps:
                    if d.dma_key is None:
                        d.signal = True
                cur_barrier = op
                seen_after = set()
                continue
            if op.dma_key is not None:
                last_dma[op.dma_key] = op
            else:
                last_eng[op.eng] = op
            if cur_barrier is not None and op.eng not in seen_after:
                seen_after.add(op.eng)
                if op.eng != "sp":
                    op.deps.append(cur_barrier)
                    cur_barrier.signal = True
            deps = {}
            for t in op.reads:
                if t.last_write is not None:
                    deps[t.last_write.idx] = ("raw", t.last_write)
                if t.excl:
                    for r in t.readers:
                        if r.idx not in deps and r.eng != op.eng:
                            deps[r.idx] = ("war", r)
            for t in op.writes:
                if t.last_write is not None:
                    deps.setdefault(t.last_write.idx, ("waw", t.last_write))
                for r in t.readers:
                    if r.idx not in deps:
                        deps[r.idx] = ("war", r)
            for t in op.reads:
                t.readers.append(op)
            for t in op.writes:
                t.last_write = op
                t.readers = []
            best = {}
            for kind, d in deps.values():
                if d is op:
                    continue
                if d.dma_key is None and d.eng == op.eng:
                    if op.eng == "pe" or op.eng == "sp":
                        continue
                    if not SAME_ENGINE_SYNC:
                        continue
                if d.dma_key is not None:
                    op.deps.append(d)
                else:
                    b = best.get(d.eng)
                    if b is None or d.idx > b.idx:
                        best[d.eng] = d
            for d in best.values():
                op.deps.append(d)
                d.signal = True
        cnt = {e: 0 for e in self.ENGS}
        dma_cnt = {}
        for op in ops:
            if op.dma_key is not None:
                dma_cnt[op.dma_key] = dma_cnt.get(op.dma_key, 0) + 16
                op.dma_cum = dma_cnt[op.dma_key]
            elif op.signal:
                cnt[op.eng] += 1
                op.seq = cnt[op.eng]
        waited = {e: {} for e in self.ENGS}
        dma_cnt2 = {}
        for op in ops:
            if op.dma_key is not None:
                dma_cnt2[op.dma_key] = dma_cnt2.get(op.dma_key, 0) + 16
            need = {}
            for d in op.deps:
                if d.dma_key is not None:
                    k = ("dma", d.dma_key)
                    v = dma_cnt2[d.dma_key] if d.dma_key != op.dma_key else d.dma_cum
                else:
                    k = ("eng", d.eng)
                    v = d.seq
                if v > need.get(k, 0):
                    need[k] = v
            w = waited[op.eng]
            for k, v in need.items():
                if w.get(k, 0) >= v:
                    continue
                w[k] = v
                op.waits.append((k, v))
        dma_keys = sorted(dma_cnt.keys(), key=str)
        self.n_sems = len(dma_keys) + len(self.ENGS)
        self.counts = dict(cnt)
        sems = {}
        with contextlib.ExitStack() as st:
            for e in self.ENGS:
                sems[("eng", e)] = st.enter_context(nc.semaphore("s_" + e))
            for i, k in enumerate(dma_keys):
                sems[("dma", k)] = st.enter_context(nc.semaphore("d_%d" % i))
            block = st.enter_context(nc.Block())
            per = {e: [o for o in ops if o.eng == e] for e in self.ENGS}
            final = [(("dma", o.dma_key), dma_cnt[o.dma_key]) for o in self.final_dma]

            def emit(engine, name):
                for op in per[name]:
                    for k, v in op.waits:
                        engine.wait_ge(sems[k], v)
                    ins = op.fn(engine)
                    if op.dma_key is not None:
                        ins.then_inc(sems[("dma", op.dma_key)], 16)
                    elif op.signal:
                        ins.then_inc(sems[("eng", name)], 1)
                if name == "sp":
                    done = set()
                    for k, v in final:
                        if k in done:
                            continue
                        done.add(k)
                        engine.wait_ge(sems[k], v)

            @block.tensor
            def _(e):
                emit(e, "pe")

            @block.scalar
            def _(e):
                emit(e, "act")

            @block.vector
            def _(e):
                emit(e, "dve")

            @block.gpsimd
            def _(e):
                emit(e, "pool")

            @block.sync
            def _(e):
                emit(e, "sp")
        return nc


ARENA_BYTES = 207 * 1024
NCONV = 0
CONV_SET = set(e for e in range(32) if e % 4 != 0)
DBG = {"units": 12, "seg0": 0, "proj": True, "full": True, "skip": "", "cut": 99}


class Arena:
    def __init__(self, ap):
        self.ap = ap
        self.top = 0
        self.high = 0

    def alloc(self, shape, dt=F32):
        assert shape[0] <= 128
        n = 1
        for s_ in shape[1:]:
            n *= s_
        esz = 2 if dt == BF16 else 4
        nbytes = (n * esz + 63) // 64 * 64
        off = self.top
        self.top += nbytes
        self.high = max(self.high, self.top)
        assert self.top <= ARENA_BYTES, ("arena overflow", self.top)
        v = self.ap[:, off // 2:(off + n * esz) // 2]
        if dt != BF16:
            v = v.bitcast(dt)
        if shape[0] < 128:
            v = v[0:shape[0]]
        if len(shape) == 3:
            v = v.rearrange("p (a b) -> p a b", b=shape[2])
        elif len(shape) == 4:
            v = v.rearrange("p (a b c) -> p a b c", b=shape[2], c=shape[3])
        return v


def build_nc(stage=3, final_norm=True):
    nc = bass.Bass("TRN2", target_bir_lowering=False)
    P = Prog(nc)
    st = contextlib.ExitStack()
    names = []

    def din(name, shape, dt=F32):
        names.append(name)
        return nc.dram_tensor(name, list(shape), dt, kind="ExternalInput").ap()

    x_own = din("x_own", [T, D])
    x_prev = din("x_prev", [NPREV * T, D])
    g_mix = din("norm_mix_g", [D])
    w_in = din("w_in", [D, 7184])
    w_up = din("w_gla_alpha_up", [16, 512])
    b_alpha = din("b_gla_alpha", [512, 1])
    gla_g = din("gla_out_norm_g", [256])
    lb_logits = din("hgrn_lb_logits", [2, 1024])
    hgrn_g = din("hgrn_out_norm_g", [128])
    w_mo = din("w_mix_out", [D, D])
    g_fin = din("norm_final_g", [D])
    if stage >= 2:
        mem = din("mem", [256, D])
        g_xa = din("norm_xattn_g", [D])
        g_mem = din("norm_mem_g", [D])
        w_xq = din("w_xattn_q", [D, D])
        w_xkv = din("w_xattn_kv", [D, 2 * D])
        w_xo = din("w_xattn_out", [D, D])
    if stage >= 3:
        g_ffn = din("norm_ffn_g", [D])
        w_rg = din("w_router_group", [D, 4])
        b_rg = din("b_router_group", [4])
        w_re = din("w_router_expert", [D, 32])
        b_re = din("b_router_expert", [32])
        w_eg = din("w_expert_gate", [N_EXP * D, 1024])
        w_eu = din("w_expert_up", [N_EXP * D, 1024])
        w_ed = din("w_expert_down", [N_EXP * 1024, D])
    out = nc.dram_tensor("out", [T, D], F32, kind="ExternalOutput").ap()
    conv_list = []
    if stage >= 3 and NCONV > 0:
        wbf_g = nc.dram_tensor("wbf_g", [N_EXP * D, 1024], BF16).ap()
        wbf_u = nc.dram_tensor("wbf_u", [N_EXP * D, 1024], BF16).ap()
        wbf_d = nc.dram_tensor("wbf_d", [N_EXP * 1024, D], BF16).ap()
        for ex in range(N_EXP):
            if ex in CONV_SET:
                conv_list.append((wbf_g[ex * D:(ex + 1) * D, :], w_eg[ex * D:(ex + 1) * D, :]))
                conv_list.append((wbf_u[ex * D:(ex + 1) * D, :], w_eu[ex * D:(ex + 1) * D, :]))
                conv_list.append((wbf_d[ex * 1024:(ex + 1) * 1024, :], w_ed[ex * 1024:(ex + 1) * 1024, :]))

    with st:
        arena_t = st.enter_context(nc.sbuf_tensor("arena", [128, ARENA_BYTES // 2], BF16))
        A = Arena(arena_t[:])
        psf = [st.enter_context(nc.psum_tensor("psf%d" % i, [128, 512], F32)) for i in range(8)]
        t_psf = P.toks("psf", 8, excl=True)

        def psb(i):
            return psf[i][:].bitcast(BF16)

        ones_f = A.alloc([128, 128])
        ident_bf = A.alloc([128, 128], BF16)
        ident_f = A.alloc([128, 128])
        maskT = A.alloc([128, 128])
        ltri = A.alloc([128, 128], BF16)
        ones_bf = A.alloc([128, 128], BF16)
        iota_row = A.alloc([128, 128])
        iota_p = A.alloc([128, 1])
        scanmask = A.alloc([128, T])
        gj = A.alloc([128, 4 * D], BF16)
        gbc = gj[:, 0:2 * D].bitcast(F32)
        ja = gj[:, 2 * D:4 * D]
        junk = ja[:, 0:D]
        a_tok = ja[:, D:2 * D]
        stat = A.alloc([128, 4, NT])
        gla_gbc = A.alloc([128, 256])
        hgrn_gbc = A.alloc([128, 128])
        wup_sb = A.alloc([16, 512])
        balpha_sb = A.alloc([128, 4])
        nbalpha = A.alloc([128, 4])
        lbl_sb = A.alloc([128, 2, 8])
        lb_sb = A.alloc([128, 8])
        oml_sb = A.alloc([128, 8])
        aT = A.alloc([128, KT, T], BF16)
        t_const, t_gbc, t_junk, t_atok, t_stat, t_par = P.toks("pp", 6)
        t_aT = P.toks("aT", NT)
        M_PERSIST = A.top

        P.pool(lambda e: e.memset(ones_f, 1.0), writes=[t_const])
        P.pool(lambda e: e.memset(ones_bf, 1.0), writes=[t_const])
        for dst in (ident_bf, ident_f) if "asel" not in DBG["skip"] else ():
            P.pool(lambda e, dst=dst: e.affine_select(out=dst, in_=ones_f, pattern=[[-1, 128]],
                                                      compare_op=ALU.is_equal, fill=0.0, base=0, channel_multiplier=1),
                   reads=[t_const], writes=[t_const])
        if "asel" not in DBG["skip"]:
            P.pool(lambda e: e.affine_select(out=maskT, in_=ones_f, pattern=[[1, 128]],
                                             compare_op=ALU.is_ge, fill=0.0, base=0, channel_multiplier=-1),
                   reads=[t_const], writes=[t_const])
            P.pool(lambda e: e.affine_select(out=ltri, in_=ones_f, pattern=[[1, 128]],
                                             compare_op=ALU.is_gt, fill=0.0, base=0, channel_multiplier=-1),
                   reads=[t_const], writes=[t_const])
        if "iota" not in DBG["skip"]:
            P.pool(lambda e: e.iota(iota_row, pattern=[[1, 128]], base=0, channel_multiplier=0,
                                    allow_small_or_imprecise_dtypes=True), writes=[t_const])
            P.pool(lambda e: e.iota(iota_p, pattern=[[0, 1]], base=0, channel_multiplier=1,
                                    allow_small_or_imprecise_dtypes=True), writes=[t_const])
        P.pool(lambda e: e.memset(scanmask, 1.0), writes=[t_const])
        P.pool(lambda e: e.memset(scanmask.rearrange("p (c j) -> p c j", j=128)[:, :, 0:1], 0.0),
               reads=[t_const], writes=[t_const])

        def load_gbc(src):
            P.dma("sp", lambda e: e.dma_start(out=gbc, in_=src.partition_broadcast(128)), "gbc", writes=[t_gbc])

        P.dma("sp", lambda e: e.dma_start(out=gla_gbc, in_=gla_g.partition_broadcast(128)), "par", writes=[t_par])
        P.dma("sp", lambda e: e.dma_start(out=hgrn_gbc, in_=hgrn_g.partition_broadcast(128)), "par", writes=[t_par])
        P.dma("sp", lambda e: e.dma_start(out=wup_sb, in_=w_up), "par", writes=[t_par])
        for hh in range(4):
            P.dma("sp", lambda e, hh=hh: e.dma_start(out=balpha_sb[:, hh:hh + 1], in_=b_alpha[hh * 128:(hh + 1) * 128, :]),
                  "par", writes=[t_par])
        for s_ in range(2) if "lbl" not in DBG["skip"] else ():
            for hh in range(8):
                P.dma("sp", lambda e, s_=s_, hh=hh: e.dma_start(
                    out=lbl_sb[:, s_, hh:hh + 1],
                    in_=lb_logits[s_:s_ + 1, hh * 128:(hh + 1) * 128].rearrange("o p -> p o")),
                    "par", writes=[t_par])
        P.dve(lambda e: e.tensor_sub(out=lb_sb, in0=lbl_sb[:, 0, :], in1=lbl_sb[:, 1, :]), reads=[t_par], writes=[t_par])
        P.act(lambda e: e.activation(out=lb_sb, in_=lb_sb, func=AF.Exp, scale=-1.0), reads=[t_par], writes=[t_par])
        P.dve(lambda e: e.tensor_scalar(out=lb_sb, in0=lb_sb, scalar1=1.0, scalar2=None, op0=ALU.add), reads=[t_par], writes=[t_par])
        P.dve(lambda e: e.reciprocal(out=lb_sb, in_=lb_sb), reads=[t_par], writes=[t_par])
        P.dve(lambda e: e.tensor_scalar(out=oml_sb, in0=lb_sb, scalar1=-1.0, scalar2=1.0,
                                        op0=ALU.mult, op1=ALU.add), reads=[t_par], writes=[t_par])
        P.dve(lambda e: e.tensor_scalar(out=nbalpha, in0=balpha_sb, scalar1=-1.0, scalar2=None,
                                        op0=ALU.mult), reads=[t_par], writes=[t_par])

        rr = {"ev": 0, "pa": 0, "pb": 0, "pool": [0, 1]}

        def evac(fn_act, fn_dve, reads, writes):
            rr["ev"] ^= 1
            if rr["ev"]:
                P.act(fn_act, reads, writes)
            else:
                P.dve(fn_dve, reads, writes)

        def copy_evac(out_ap, in_ap, reads, writes):
            evac(lambda e: e.activation(out=out_ap, in_=in_ap, func=AF.Copy),
                 lambda e: e.tensor_copy(out=out_ap, in_=in_ap), reads, writes)

        def sigmoid_to(out_ap, in_ap, reads, tok):
            P.act(lambda e: e.activation(out=out_ap, in_=in_ap, func=AF.Sigmoid), reads=reads, writes=[tok])

        def silu_to(out_ap, in_ap, reads, tok):
            P.act(lambda e: e.activation(out=out_ap, in_=in_ap, func=AF.Silu), reads=reads, writes=[tok])

        def sumsq(src_ap, src_tok, dst_ap, n):
            P.dve(lambda e: e.scalar_tensor_tensor(out=junk[:, 0:n], in0=src_ap, scalar=1.0, in1=src_ap,
                                                   op0=ALU.mult, op1=ALU.mult, accum_out=dst_ap),
                  reads=[src_tok], writes=[t_junk, t_stat])

        def rstd_of(dst_ap, ss_ap, tmp_ap, dim, tok):
            P.act(lambda e: e.activation(out=tmp_ap, in_=ss_ap, func=AF.Ln, scale=1.0 / dim, bias=EPS), reads=[tok], writes=[tok])
            P.act(lambda e: e.activation(out=dst_ap, in_=tmp_ap, func=AF.Exp, scale=-0.5), reads=[tok], writes=[tok])

        def next_pa():
            rr["pa"] = (rr["pa"] + 1) % len(rr["pool"])
            return rr["pool"][rr["pa"]]

        def next_pb():
            rr["pb"] ^= 1
            return 6 + rr["pb"]

        def transpose_tile(src, src_tok, dstT, dst_tok, cols):
            for half in range(2):
                pb = next_pb()
                for j in range(8):
                    kt = half * 8 + j
                    P.pe(lambda e, pb=pb, j=j, kt=kt: e.transpose(out=psb(pb)[:, j * 128:(j + 1) * 128],
                                                                   in_=src[:, kt * 128:(kt + 1) * 128],
                                                                   identity=ident_bf),
                         reads=[src_tok, t_const], writes=[t_psf[pb]])
                copy_evac(dstT[:, half * 8:(half + 1) * 8, cols],
                          psb(pb).rearrange("p (j c) -> p j c", c=128),
                          [t_psf[pb]], [dst_tok])

        def rms_stats(src_fn, src_toks, ntile, dim=D):
            for i in range(ntile):
                sumsq(src_fn(i), src_toks[i], stat[:, 0, i:i + 1], dim)
            rstd_of(stat[:, 3, 0:ntile], stat[:, 0, 0:ntile], stat[:, 1, 0:ntile], dim, t_stat)

        def norm_to_T(src_fn, src_toks, ntile, dstT, dst_toks):
            rms_stats(src_fn, src_toks, ntile)
            for i in range(ntile):
                P.dve(lambda e, i=i: e.scalar_tensor_tensor(out=a_tok, in0=src_fn(i), scalar=stat[:, 3, i:i + 1],
                                                            in1=gbc, op0=ALU.mult, op1=ALU.mult),
                      reads=[src_toks[i], t_stat, t_gbc], writes=[t_atok])
                transpose_tile(a_tok, t_atok, dstT, dst_toks[i], slice(i * 128, (i + 1) * 128))

        def wload(dst_ap, src_ap, key, tok):
            P.dma("pool", lambda e: e.dma_start(out=dst_ap, in_=src_ap), key, writes=[tok])

        t_cv = P.tok("cv")
        conv_state = {"i": 0}

        def emit_conv(n):
            for _ in range(n):
                if conv_state["i"] >= len(conv_list):
                    return
                dst, src = conv_list[conv_state["i"]]
                conv_state["i"] += 1
                tk = P.tok("cvi")
                conv_state["last_tok"] = tk
                P.dma("pool", lambda e, dst=dst, src=src: e.dma_start(out=dst, in_=src), "cv", writes=[tk])

        def wview(w, c0, n, r0=0, rows=D):
            return w[r0:r0 + rows, c0:c0 + n].rearrange("(kt p) n -> p kt n", p=128)

        def proj_fm(wap, wtok, src_T, src_toks, half, pa, M=128):
            for kt in range(KT):
                P.pe(lambda e, kt=kt: e.matmul(psf[pa][0:M, :], lhsT=wap[:, kt, :],
                                               rhs=src_T[:, kt, half * 512:(half + 1) * 512],
                                               start=(kt == 0), stop=(kt == KT - 1)),
                     reads=[wtok] + src_toks[half * 4:(half + 1) * 4], writes=[t_psf[pa]])

        o_tok = A.alloc([128, NT, D], BF16)
        t_otok = P.toks("otok", NT)
        M_AFTER_OTOK = A.top
        wq_b = [A.alloc([128, KT, 128], BF16) for _ in range(2)]
        wk_b = [A.alloc([128, KT, 128], BF16) for _ in range(2)]
        wvg_b = [A.alloc([128, KT, 512], BF16) for _ in range(1)]
        wlr = A.alloc([128, KT, 16], BF16)
        t_wq = P.toks("wq", 2)
        t_wk = P.toks("wk", 2)
        t_wvgh = P.toks("wvgh", 2)
        t_wlr = P.tok("wlr")
        qf = A.alloc([128, T])
        kf = A.alloc([128, T])
        t1 = A.alloc([128, T])
        t2 = A.alloc([128, T])
        t3 = A.alloc([128, T])
        qt = A.alloc([128, T], BF16)
        ktl = A.alloc([128, T], BF16)
        qs = A.alloc([128, T], BF16)
        khT = A.alloc([128, T], BF16)
        t_qf, t_kf, t_t1, t_t2, t_t3, t_qt, t_ktl, t_qs, t_khT = P.toks("mx", 9)
        sm = A.alloc([128, 4, NT])
        sm2 = A.alloc([128, 2, NT])
        t_sm = P.tok("sm")
        v_tok = A.alloc([128, NT, 256], BF16)
        sg_tok = A.alloc([128, NT, 256], BF16)
        sg_tmp = A.alloc([128, 256])
        khat = A.alloc([128, NT, 128], BF16)
        AT_sb = A.alloc([128, NT, 128], BF16)
        S_all = A.alloc([128, 2048])
        S_bf = A.alloc([128, NT + 1, 256], BF16)
        glrT = A.alloc([16, T])
        ost = A.alloc([128, 4, NT])
        o_all = A.alloc([128, NT, 256])
        S_tmp = A.alloc([128, 256])
        t_osb = P.toks("osb", NT)
        t_Stmp = P.tok("Stmp")
        xs = [A.alloc([128, D]) for _ in range(2)]
        t_v, t_sg, t_sgtmp, t_khat, t_AT, t_Sbf, t_glr, t_ost = P.toks("mb", 8)
        t_S = P.toks("S", 12)
        t_xs = P.toks("xs", 2)

        P.pool(lambda e: e.memset(S_all, 0.0), writes=t_S)
        P.pool(lambda e: e.memset(AT_sb, 0.0), writes=[t_AT])

        def capture(fn):
            start = len(P.ops)
            fn()
            ops_ = P.ops[start:]
            del P.ops[start:]
            return ops_

        def interleave(a, b):
            na, nb = len(a), len(b)
            ia = ib = 0
            while ia < na or ib < nb:
                if ib >= nb or (ia < na and ia * nb <= ib * na):
                    op = a[ia]
                    ia += 1
                else:
                    op = b[ib]
                    ib += 1
                op.idx = len(P.ops)
                P.ops.append(op)

        def full_cols(u):
            if u < 4:
                return u * 128, 512 + u * 128, 1024 + u * 256, 2048 + u * 256, 256
            hu = u - 4
            return 3088 + hu * 128, 4112 + hu * 128, 5136 + hu * 128, 6160 + hu * 128, 128

        def full_loads_qk(u):
            cq, ck, cv, cg, dv = full_cols(u)
            wb = u % 2
            wload(wq_b[wb], wview(w_in, cq, 128), "wq%d" % wb, t_wq[wb])
            wload(wk_b[wb], wview(w_in, ck, 128), "wk%d" % wb, t_wk[wb])

        def full_loads_vg(u):
            cq, ck, cv, cg, dv = full_cols(u)
            wvg = wvg_b[0]
            wload(wvg[:, :, 0:dv], wview(w_in, cv, dv), "wvg0", t_wvgh[0])
            wload(wvg[:, :, dv:2 * dv], wview(w_in, cg, dv), "wvg1", t_wvgh[1])

        def mixer_unit(u, full):
            gla = u < 4
            wb = u % 2
            dv = 256 if gla else 128
            nv = 2 * dv if full else dv
            sc = (-1.0 / 16.0) if gla else 1.0
            if gla:
                cq, ck, cv, cg = u * 128, 512 + u * 128, 1024 + u * 256, 2048 + u * 256
                scol = u * 256
            else:
                hu = u - 4
                cq, ck, cv, cg = 3088 + hu * 128, 4112 + hu * 128, 5136 + hu * 128, 6160 + hu * 128
                scol = 1024 + hu * 128
            tS = t_S[u]
            if full:
                wvg = wvg_b[0]
                twvg_r = list(t_wvgh)
            else:
                hsel = u % 2
                wvg = wvg_b[0][:, :, hsel * 256:(hsel + 1) * 256]
                twvg_r = [t_wvgh[hsel]]
            if full:
                if u == 0:
                    full_loads_qk(0)
                    full_loads_vg(0)
                if u + 1 < DBG["units"]:
                    full_loads_qk(u + 1)
            else:
                wload(wk_b[wb], wview(w_in, ck, 128), "wk%d" % wb, t_wk[wb])
                wload(wvg[:, :, 0:dv], wview(w_in, cv, dv), "wvg%d" % hsel, t_wvgh[hsel])
            if full:
                for half in range(2):
                    pa = next_pa()
                    proj_fm(wq_b[wb], t_wq[wb], aT, t_aT, half, pa)
                    sl = slice(half * 512, (half + 1) * 512)
                    if gla:
                        P.act(lambda e, pa=pa, sl=sl: e.activation(out=qf[:, sl], in_=psf[pa][:], func=AF.Copy,
                                                                  scale=128.0 ** -0.5),
                              reads=[t_psf[pa]], writes=[t_qf])
                    else:
                        silu_to(qf[:, sl], psf[pa][:], [t_psf[pa]], t_qf)
            for half in range(2):
                pa = next_pa()
                sl = slice(half * 512, (half + 1) * 512)
                proj_fm(wk_b[wb], t_wk[wb], aT, t_aT, half, pa)
                if gla:
                    copy_evac(kf[:, sl], psf[pa][:], [t_psf[pa]], [t_kf])
                    pz = next_pa()
                    P.pe(lambda e, pz=pz, sl=sl: e.matmul(psf[pz][:], lhsT=wup_sb[:, u * 128:(u + 1) * 128],
                                                         rhs=glrT[:, sl], start=True, stop=True),
                         reads=[t_par, t_glr], writes=[t_psf[pz]])
                    P.act(lambda e, pz=pz, sl=sl: e.activation(out=t1[:, sl], in_=psf[pz][:], func=AF.Exp,
                                                              scale=-1.0, bias=nbalpha[:, u:u + 1]),
                          reads=[t_psf[pz], t_par], writes=[t_t1])
                else:
                    sigmoid_to(t1[:, sl], psf[pa][:], [t_psf[pa]], t_t1)
            def chain_part():
                if gla:
                    for half in range(2):
                        P.act(lambda e, half=half: e.activation(out=t1[:, half * 512:(half + 1) * 512], in_=t1[:, half * 512:(half + 1) * 512],
                                                                func=AF.Ln, bias=1.0), reads=[t_t1], writes=[t_t1])
                else:
                    P.dve(lambda e: e.tensor_scalar(out=t2, in0=t1, scalar1=oml_sb[:, hu:hu + 1],
                                                    scalar2=lb_sb[:, hu:hu + 1], op0=ALU.mult, op1=ALU.add),
                          reads=[t_t1, t_par], writes=[t_t2])
                    P.dve(lambda e: e.tensor_scalar(out=kf, in0=t2, scalar1=-1.0, scalar2=1.0,
                                                    op0=ALU.mult, op1=ALU.add), reads=[t_t2], writes=[t_kf])
                    P.act(lambda e: e.activation(out=t1, in_=t2, func=AF.Ln), reads=[t_t2], writes=[t_t1])
                P.dve(lambda e: e.tensor_tensor_scan(out=t2, data0=scanmask, data1=t1, initial=0.0,
                                                     op0=ALU.mult, op1=ALU.add),
                      reads=[t_t1, t_const], writes=[t_t2])
                c3 = t2.rearrange("p (n j) -> p n j", j=128)
                cref = c3[:, :, 63:64]
                clast = c3[:, :, 127:128]
                cref2 = cref.rearrange("p n o -> p (n o)")
                clast2 = clast.rearrange("p n o -> p (n o)")
                P.dve(lambda e: e.tensor_tensor(out=t1.rearrange("p (n j) -> p n j", j=128), in0=c3,
                                                in1=cref.to_broadcast([128, NT, 128]), op=ALU.subtract),
                      reads=[t_t2], writes=[t_t1])
                if full:
                    P.act(lambda e: e.activation(out=sm[:, 0, :], in_=cref2, func=AF.Exp, scale=sc),
                          reads=[t_t2], writes=[t_sm])
                P.dve(lambda e: e.tensor_tensor(out=sm[:, 3, :], in0=clast2, in1=cref2, op=ALU.subtract),
                      reads=[t_t2], writes=[t_sm])
                P.act(lambda e: e.activation(out=sm[:, 1, :], in_=sm[:, 3, :], func=AF.Exp, scale=sc),
                      reads=[t_sm], writes=[t_sm])
                P.act(lambda e: e.activation(out=sm[:, 2, :], in_=clast2, func=AF.Exp, scale=sc),
                      reads=[t_t2], writes=[t_sm])
                P.act(lambda e: e.activation(out=t3, in_=t1, func=AF.Exp, scale=-sc), reads=[t_t1], writes=[t_t3])
                P.dve(lambda e: e.tensor_tensor(out=ktl, in0=kf, in1=t3, op=ALU.mult),
                      reads=[t_kf, t_t3], writes=[t_ktl])
                P.pool(lambda e: e.tensor_tensor(out=khT.rearrange("p (n j) -> p n j", j=128),
                                                 in0=ktl.rearrange("p (n j) -> p n j", j=128),
                                                 in1=sm[:, 1, :].unsqueeze(2).to_broadcast([128, NT, 128]), op=ALU.mult),
                       reads=[t_ktl, t_sm], writes=[t_khT])
                if full:
                    P.act(lambda e: e.activation(out=t3, in_=t1, func=AF.Exp, scale=sc), reads=[t_t1], writes=[t_t3])
                    P.dve(lambda e: e.tensor_tensor(out=qt, in0=qf, in1=t3, op=ALU.mult),
                          reads=[t_qf, t_t3], writes=[t_qt])
                    P.pool(lambda e: e.tensor_tensor(out=qs.rearrange("p (n j) -> p n j", j=128),
                                                     in0=qt.rearrange("p (n j) -> p n j", j=128),
                                                     in1=sm[:, 0, :].unsqueeze(2).to_broadcast([128, NT, 128]), op=ALU.mult),
                           reads=[t_qt, t_sm], writes=[t_qs])

            def vg_part():
                gb = gla_gbc if gla else hgrn_gbc
                for i in range(NT):
                    pa = next_pa()
                    for kt in range(KT):
                        P.pe(lambda e, kt=kt, i=i, pa=pa: e.matmul(psf[pa][:, 0:nv], lhsT=aT[:, kt, i * 128:(i + 1) * 128],
                                                                   rhs=wvg[:, kt, 0:nv],
                                                                   start=(kt == 0), stop=(kt == KT - 1)),
                             reads=twvg_r + [t_aT[i]], writes=[t_psf[pa]])
                    P.act(lambda e, i=i, pa=pa: e.activation(out=v_tok[:, i, 0:dv], in_=psf[pa][:, 0:dv], func=AF.Copy),
                          reads=[t_psf[pa]], writes=[t_v])
                    if full:
                        silu_to(sg_tmp[:, 0:dv], psf[pa][:, dv:2 * dv], [t_psf[pa]], t_sgtmp)
                        P.pool(lambda e, i=i: e.tensor_tensor(out=sg_tok[:, i, 0:dv], in0=sg_tmp[:, 0:dv],
                                                              in1=gb[:, 0:dv], op=ALU.mult),
                               reads=[t_sgtmp, t_par], writes=[t_sg])

            if full:
                ops_a = capture(chain_part)
                ops_b = capture(vg_part)
                interleave(ops_a, ops_b)
            else:
                chain_part()
                vg_part()
            if full and u + 1 < DBG["units"]:
                full_loads_vg(u + 1)
            if full:
                emit_conv(2)
            pb = next_pb()
            for n in range(NT):
                P.pe(lambda e, n=n: e.transpose(out=psb(pb)[:, n * 128:(n + 1) * 128],
                                               in_=khT[:, n * 128:(n + 1) * 128], identity=ident_bf),
                     reads=[t_khT, t_const], writes=[t_psf[pb]])
            copy_evac(khat, psb(pb).rearrange("p (n c) -> p n c", c=128), [t_psf[pb]], [t_khat])
            if full:
                for g2 in range(2):
                    for n4 in range(4):
                        n = g2 * 4 + n4
                        P.pe(lambda e, n=n, n4=n4, g2=g2: e.matmul(psf[2 + g2][:, n4 * 128 + 64:(n4 + 1) * 128],
                                                                  lhsT=ktl[:, n * 128:(n + 1) * 128],
                                                                  rhs=qt[:, n * 128 + 64:(n + 1) * 128], start=True, stop=True),
                             reads=[t_ktl, t_qt], writes=[t_psf[2 + g2]])
                        P.pe(lambda e, n=n, n4=n4, g2=g2: e.matmul(psf[2 + g2][0:64, n4 * 128:n4 * 128 + 64],
                                                                  lhsT=ktl[:, n * 128:n * 128 + 64],
                                                                  rhs=qt[:, n * 128:n * 128 + 64], start=True, stop=True),
                             reads=[t_ktl, t_qt], writes=[t_psf[2 + g2]])
                    pv = psf[2 + g2][:].rearrange("p (n c) -> p n c", c=128)
                    P.dve(lambda e, g2=g2, pv=pv: e.tensor_tensor(out=AT_sb[:, g2 * 4:(g2 + 1) * 4, 64:128], in0=pv[:, :, 64:128],
                                                                 in1=maskT[:, 64:128].unsqueeze(1).to_broadcast([128, 4, 64]),
                                                                 op=ALU.mult),
                          reads=[t_psf[2 + g2], t_const], writes=[t_AT])
                    P.dve(lambda e, g2=g2, pv=pv: e.tensor_tensor(out=AT_sb[0:64, g2 * 4:(g2 + 1) * 4, 0:64], in0=pv[0:64, :, 0:64],
                                                                 in1=maskT[0:64, 0:64].unsqueeze(1).to_broadcast([64, 4, 64]),
                                                                 op=ALU.mult),
                          reads=[t_psf[2 + g2], t_const], writes=[t_AT])
            Su = S_all[:, scol:scol + dv]
            St = S_tmp[:, 0:dv]
            if full:
                P.act(lambda e: e.activation(out=S_bf[:, 0, 0:dv], in_=Su, func=AF.Copy), reads=[tS], writes=[t_Sbf])
            per_bank = 512 // dv
            kvp = [4, 5, 2, 3] if full else [3, 4, 5]
            for n in range(NT):
                bank = kvp[(n // per_bank) % len(kvp)]
                off = (n % per_bank) * dv
                P.pe(lambda e, n=n, bank=bank, off=off: e.matmul(psf[bank][:, off:off + dv], lhsT=khat[:, n, :],
                                                                rhs=v_tok[:, n, 0:dv], start=True, stop=True),
                     reads=[t_khat, t_v], writes=[t_psf[bank]])
            for n in range(NT):
                bank = kvp[(n // per_bank) % len(kvp)]
                off = (n % per_bank) * dv
                if full:
                    src, dst = (Su, St) if n % 2 == 0 else (St, Su)
                    tsrc, tdst = (tS, t_Stmp) if n % 2 == 0 else (t_Stmp, tS)
                else:
                    src, dst, tsrc, tdst = Su, Su, tS, tS
                P.dve(lambda e, n=n, bank=bank, off=off, src=src, dst=dst: e.scalar_tensor_tensor(
                    out=dst, in0=src, scalar=sm[:, 2, n:n + 1], in1=psf[bank][:, off:off + dv],
                    op0=ALU.mult, op1=ALU.add), reads=[tsrc, t_sm, t_psf[bank]], writes=[tdst])
                if full and n < NT - 1:
                    P.act(lambda e, n=n, dst=dst: e.activation(out=S_bf[:, n + 1, 0:dv], in_=dst, func=AF.Copy),
                          reads=[tdst], writes=[t_Sbf])
            if full:
                for n in range(NT):
                    pa = next_pa()
                    P.pe(lambda e, n=n, pa=pa: e.matmul(psf[pa][:, 0:dv], lhsT=AT_sb[:, n, :], rhs=v_tok[:, n, 0:dv],
                                                        start=True, stop=False),
                         reads=[t_AT, t_v], writes=[t_psf[pa]])
                    P.pe(lambda e, n=n, pa=pa: e.matmul(psf[pa][:, 0:dv], lhsT=qs[:, n * 128:(n + 1) * 128],
                                                        rhs=S_bf[:, n, 0:dv], start=False, stop=True),
                         reads=[t_qs, t_Sbf], writes=[t_psf[pa]])
                    P.act(lambda e, n=n, pa=pa: e.activation(out=o_all[:, n, 0:dv], in_=psf[pa][:, 0:dv], func=AF.Copy),
                          reads=[t_psf[pa]], writes=[t_osb[n]])
                    P.dve(lambda e, n=n: e.scalar_tensor_tensor(out=junk[:, 0:dv], in0=o_all[:, n, 0:dv], scalar=1.0, in1=o_all[:, n, 0:dv],
                                                                op0=ALU.mult, op1=ALU.mult, accum_out=ost[:, 0, n:n + 1]),
                          reads=[t_osb[n]], writes=[t_junk, t_ost])
                rstd_of(ost[:, 3, :], ost[:, 0, :], ost[:, 1, :], dv, t_ost)
                for n in range(NT):
                    P.dve(lambda e, n=n: e.scalar_tensor_tensor(
                        out=o_tok[:, n, scol:scol + dv], in0=o_all[:, n, 0:dv], scalar=ost[:, 3, n:n + 1],
                        in1=sg_tok[:, n, 0:dv], op0=ALU.mult, op1=ALU.mult),
                        reads=[t_osb[n], t_ost, t_sg], writes=[t_otok[n]])

        o_flat = o_tok.rearrange("p a b -> p (a b)")
        KF = [kf, o_flat[:, 0:2 * T].bitcast(F32)]
        T1 = [t1, o_flat[:, 2 * T:4 * T].bitcast(F32)]
        VT = [v_tok, o_flat[:, 4 * T:4 * T + NT * 256].rearrange("p (a b) -> p a b", b=256)]
        t_KF = [t_kf, P.tok("kf2")]
        t_T1 = [t_t1, P.tok("t1b")]
        t_VT = [t_v, P.tok("v2")]

        def unit_cfg(u):
            gla = u < 4
            dv = 256 if gla else 128
            sc = (-1.0 / 16.0) if gla else 1.0
            if gla:
                ck, cv, scol = 512 + u * 128, 1024 + u * 256, u * 256
            else:
                hu = u - 4
                ck, cv, scol = 4112 + hu * 128, 5136 + hu * 128, 1024 + hu * 128
            return gla, dv, sc, ck, cv, scol

        def state_A_load(u):
            gla, dv, sc, ck, cv, scol = unit_cfg(u)
            s_ = u % 2
            wvg = wvg_b[0][:, :, s_ * 256:(s_ + 1) * 256]
            wload(wk_b[s_], wview(w_in, ck, 128), "wk%d" % s_, t_wk[s_])
            wload(wvg[:, :, 0:dv], wview(w_in, cv, dv), "wvg%d" % s_, t_wvgh[s_])

        def state_A_pe(u):
            gla, dv, sc, ck, cv, scol = unit_cfg(u)
            s_ = u % 2
            wvg = wvg_b[0][:, :, s_ * 256:(s_ + 1) * 256]
            for half in range(2):
                proj_fm(wk_b[s_], t_wk[s_], aT, t_aT, half, half)
            per_bank = 512 // dv
            for i in range(NT):
                bank = 2 + i // per_bank
                off = (i % per_bank) * dv
                for kt in range(KT):
                    P.pe(lambda e, kt=kt, i=i, bank=bank, off=off: e.matmul(psf[bank][:, off:off + dv], lhsT=aT[:, kt, i * 128:(i + 1) * 128],
                                                                            rhs=wvg[:, kt, 0:dv], start=(kt == 0), stop=(kt == KT - 1)),
                         reads=[t_wvgh[s_], t_aT[i]], writes=[t_psf[bank]])

        def state_A_kevac(u):
            gla, dv, sc, ck, cv, scol = unit_cfg(u)
            s_ = u % 2
            for half in range(2):
                pa = half
                sl = slice(half * 512, (half + 1) * 512)
                if gla:
                    copy_evac(KF[s_][:, sl], psf[pa][:], [t_psf[pa]], [t_KF[s_]])
                    P.pe(lambda e, pa=pa, sl=sl: e.matmul(psf[pa][:], lhsT=wup_sb[:, u * 128:(u + 1) * 128],
                                                         rhs=glrT[:, sl], start=True, stop=True),
                         reads=[t_par, t_glr], writes=[t_psf[pa]])
                    P.act(lambda e, pa=pa, sl=sl: e.activation(out=T1[s_][:, sl], in_=psf[pa][:], func=AF.Exp,
                                                              scale=-1.0, bias=nbalpha[:, u:u + 1]),
                          reads=[t_psf[pa], t_par], writes=[t_T1[s_]])
                else:
                    sigmoid_to(T1[s_][:, sl], psf[pa][:], [t_psf[pa]], t_T1[s_])

        def state_A_evac(u):
            gla, dv, sc, ck, cv, scol = unit_cfg(u)
            s_ = u % 2
            per_bank = 512 // dv
            for b in range(NT // per_bank):
                bank = 2 + b
                copy_evac(VT[s_][:, b * per_bank:(b + 1) * per_bank, 0:dv],
                          psf[bank][:].rearrange("p (a b) -> p a b", b=dv), [t_psf[bank]], [t_VT[s_]])

        def state_B_chain(u):
            gla, dv, sc, ck, cv, scol = unit_cfg(u)
            s_ = u % 2
            kf_, t1_, tkf_, tt1_ = KF[s_], T1[s_], t_KF[s_], t_T1[s_]
            if gla:
                for half in range(2):
                    P.act(lambda e, half=half: e.activation(out=t1_[:, half * 512:(half + 1) * 512], in_=t1_[:, half * 512:(half + 1) * 512],
                                                            func=AF.Ln, bias=1.0), reads=[tt1_], writes=[tt1_])
            else:
                hu = u - 4
                P.dve(lambda e: e.tensor_scalar(out=t2, in0=t1_, scalar1=oml_sb[:, hu:hu + 1],
                                                scalar2=lb_sb[:, hu:hu + 1], op0=ALU.mult, op1=ALU.add),
                      reads=[tt1_, t_par], writes=[t_t2])
                P.dve(lambda e: e.tensor_scalar(out=kf_, in0=t2, scalar1=-1.0, scalar2=1.0,
                                                op0=ALU.mult, op1=ALU.add), reads=[t_t2], writes=[tkf_])
                P.act(lambda e: e.activation(out=t1_, in_=t2, func=AF.Ln), reads=[t_t2], writes=[tt1_])
            P.dve(lambda e: e.tensor_tensor_scan(out=t2, data0=scanmask, data1=t1_, initial=0.0,
                                                 op0=ALU.mult, op1=ALU.add),
                  reads=[tt1_, t_const], writes=[t_t2])
            c3 = t2.rearrange("p (n j) -> p n j", j=128)
            cref = c3[:, :, 63:64]
            clast = c3[:, :, 127:128]
            cref2 = cref.rearrange("p n o -> p (n o)")
            clast2 = clast.rearrange("p n o -> p (n o)")
            P.dve(lambda e: e.tensor_tensor(out=t1_.rearrange("p (n j) -> p n j", j=128), in0=c3,
                                            in1=cref.to_broadcast([128, NT, 128]), op=ALU.subtract),
                  reads=[t_t2], writes=[tt1_])
            P.dve(lambda e: e.tensor_tensor(out=sm[:, 3, :], in0=clast2, in1=cref2, op=ALU.subtract),
                  reads=[t_t2], writes=[t_sm])
            P.dve(lambda e: e.tensor_tensor_scan(out=sm2[:, 0, :], data0=scanmask[:, 1:1 + NT], data1=clast2, initial=0.0,
                                                 op0=ALU.mult, op1=ALU.add), reads=[t_t2, t_const], writes=[t_sm])
            P.dve(lambda e: e.tensor_scalar(out=sm2[:, 1, :], in0=sm2[:, 0, :], scalar1=-1.0, scalar2=sm2[:, 0, NT - 1:NT],
                                            op0=ALU.mult, op1=ALU.add), reads=[t_sm], writes=[t_sm])
            P.dve(lambda e: e.tensor_tensor(out=sm[:, 3, :], in0=sm[:, 3, :], in1=sm2[:, 1, :], op=ALU.add),
                  reads=[t_sm], writes=[t_sm])
            P.act(lambda e: e.activation(out=sm[:, 1, :], in_=sm[:, 3, :], func=AF.Exp, scale=sc),
                  reads=[t_sm], writes=[t_sm])
            P.act(lambda e: e.activation(out=sm[:, 2, 0:1], in_=sm2[:, 0, NT - 1:NT], func=AF.Exp, scale=sc),
                  reads=[t_sm], writes=[t_sm])
            P.act(lambda e: e.activation(out=t3, in_=t1_, func=AF.Exp, scale=-sc), reads=[tt1_], writes=[t_t3])
            P.dve(lambda e: e.tensor_tensor(out=ktl, in0=kf_, in1=t3, op=ALU.mult),
                  reads=[tkf_, t_t3], writes=[t_ktl])
            P.pool(lambda e: e.tensor_tensor(out=khT.rearrange("p (n j) -> p n j", j=128),
                                             in0=ktl.rearrange("p (n j) -> p n j", j=128),
                                             in1=sm[:, 1, :].unsqueeze(2).to_broadcast([128, NT, 128]), op=ALU.mult),
                   reads=[t_ktl, t_sm], writes=[t_khT])

        def state_B_pe(u):
            gla, dv, sc, ck, cv, scol = unit_cfg(u)
            s_ = u % 2
            tS = t_S[u]
            pb = 7
            for n in range(NT):
                P.pe(lambda e, n=n: e.transpose(out=psb(pb)[:, n * 128:(n + 1) * 128],
                                               in_=khT[:, n * 128:(n + 1) * 128], identity=ident_bf),
                     reads=[t_khT, t_const], writes=[t_psf[pb]])
            copy_evac(khat, psb(pb).rearrange("p (n c) -> p n c", c=128), [t_psf[pb]], [t_khat])
            Su = S_all[:, scol:scol + dv]
            for n in range(NT):
                P.pe(lambda e, n=n: e.matmul(psf[6][:, 0:dv], lhsT=khat[:, n, :], rhs=VT[s_][:, n, 0:dv],
                                            start=(n == 0), stop=(n == NT - 1)),
                     reads=[t_khat, t_VT[s_]], writes=[t_psf[6]])
            P.dve(lambda e: e.scalar_tensor_tensor(out=Su, in0=Su, scalar=sm[:, 2, 0:1], in1=psf[6][:, 0:dv],
                                                   op0=ALU.mult, op1=ALU.add), reads=[tS, t_sm, t_psf[6]], writes=[tS])

        def mixer_segment_state():
            wload(wlr, wview(w_in, 3072, 16), "wlr", t_wlr)
            for half in range(2):
                pa = half
                proj_fm(wlr, t_wlr, aT, t_aT, half, pa, M=16)
                copy_evac(glrT[:, half * 512:(half + 1) * 512], psf[pa][0:16, :], [t_psf[pa]], [t_glr])
            nu = DBG["units"]
            if nu == 0:
                return
            state_A_load(0)
            if nu > 1:
                state_A_load(1)
            state_A_pe(0)
            state_A_kevac(0)
            state_A_evac(0)
            for u in range(nu):
                if u + 2 < nu:
                    state_A_load(u + 2)
                if u + 1 < nu:
                    state_A_pe(u + 1)
                state_B_chain(u)
                emit_conv(1)
                if u + 1 < nu:
                    state_A_kevac(u + 1)
                    state_A_evac(u + 1)
                state_B_pe(u)

        def mixer_segment(full):
            if not full and "nopipe" not in DBG["skip"]:
                mixer_segment_state()
                return
            rr["pool"] = [0, 1] if full else [0, 1, 2]
            rr["pa"] = 0
            wload(wlr, wview(w_in, 3072, 16), "wlr", t_wlr)
            for half in range(2):
                pa = next_pa()
                proj_fm(wlr, t_wlr, aT, t_aT, half, pa, M=16)
                copy_evac(glrT[:, half * 512:(half + 1) * 512], psf[pa][0:16, :], [t_psf[pa]], [t_glr])
            for u in range(DBG["units"]):
                mixer_unit(u, full and DBG["full"])

        load_gbc(g_mix)
        for seg in range(DBG["seg0"], NPREV + 1):
            src = x_prev if seg < NPREV else x_own
            r0 = seg * T if seg < NPREV else 0
            for i in range(NT):
                P.dma("sp", lambda e, i=i, src=src, r0=r0: e.dma_start(out=xs[i % 2], in_=src[r0 + i * 128: r0 + (i + 1) * 128, :]),
                      "xs%d" % (i % 2), writes=[t_xs[i % 2]])
                sumsq(xs[i % 2], t_xs[i % 2], stat[:, 0, i:i + 1], D)
                rstd_of(stat[:, 3, i:i + 1], stat[:, 0, i:i + 1], stat[:, 1, i:i + 1], D, t_stat)
                P.dve(lambda e, i=i: e.scalar_tensor_tensor(out=a_tok, in0=xs[i % 2], scalar=stat[:, 3, i:i + 1],
                                                            in1=gbc, op0=ALU.mult, op1=ALU.mult),
                      reads=[t_xs[i % 2], t_stat, t_gbc], writes=[t_atok])
                transpose_tile(a_tok, t_atok, aT, t_aT[i], slice(i * 128, (i + 1) * 128))
            if seg == NPREV and seg > DBG["seg0"]:
                P.barrier()
            mixer_segment(full=(seg == NPREV))

        if "barrier" not in DBG["skip"]:
            P.barrier()
        A.top = M_AFTER_OTOK
        h = A.alloc([128, NT, D])
        t_h = P.toks("h", NT)
        wbig = [A.alloc([128, KT, 512], BF16) for _ in range(2)]
        t_wbig = P.toks("wbig", 2)
        M_AFTER_WBIG = A.top
        for i in range(NT):
            P.dma("sp", lambda e, i=i: e.dma_start(out=h[:, i, :], in_=x_own[i * 128:(i + 1) * 128, :]), "h%d" % i,
                  writes=[t_h[i]])

        def proj_residual(w, srcT, src_toks):
            for c in range(4):
                wb = c % 2
                wload(wbig[wb], wview(w, c * 512, 512), "wbig%d" % wb, t_wbig[wb])
                emit_conv(1)
                for i in range(NT):
                    pa = next_pa()
                    for kt in range(KT):
                        P.pe(lambda e, kt=kt, i=i, pa=pa, wb=wb: e.matmul(psf[pa][:], lhsT=srcT[:, kt, i * 128:(i + 1) * 128],
                                                                          rhs=wbig[wb][:, kt, :],
                                                                          start=(kt == 0), stop=(kt == KT - 1)),
                             reads=[t_wbig[wb], src_toks[i]], writes=[t_psf[pa]])
                    P.dve(lambda e, i=i, pa=pa, c=c: e.tensor_tensor(out=h[:, i, c * 512:(c + 1) * 512],
                                                                    in0=psf[pa][:], in1=h[:, i, c * 512:(c + 1) * 512],
                                                                    op=ALU.add),
                          reads=[t_psf[pa], t_h[i]], writes=[t_h[i]])

        if DBG["proj"]:
            for i in range(NT):
                transpose_tile(o_tok[:, i, :], t_otok[i], aT, t_aT[i], slice(i * 128, (i + 1) * 128))
            proj_residual(w_mo, aT, t_aT)

        if stage >= 2:
            P.barrier()
            xa_base = M_PERSIST
            A.top = xa_base
            mem_f = A.alloc([128, 2, D])
            memT = A.alloc([128, KT, 256], BF16)
            assert A.top <= M_AFTER_OTOK
            A.top = M_AFTER_WBIG
            kT = A.alloc([128, KT, 256], BF16)
            v_mem = A.alloc([128, 2, D], BF16)
            p_sb = A.alloc([128, 4, 256], BF16)
            pT_sb = A.alloc([128, 8, 128], BF16)
            xst = A.alloc([128, 16])
            t_kT, t_vmem, t_p, t_pT, t_xst = P.toks("xa", 5)
            t_memf = P.toks("memf", 2)
            t_memT = P.toks("memT", 2)
            for i in range(2):
                P.dma("sp", lambda e, i=i: e.dma_start(out=mem_f[:, i, :], in_=mem[i * 128:(i + 1) * 128, :]), "memf%d" % i,
                      writes=[t_memf[i]])
            load_gbc(g_mem)
            norm_to_T(lambda i: mem_f[:, i, :], t_memf, 2, memT, t_memT)
            for c in range(8):
                wb = c % 2
                wload(wbig[wb], wview(w_xkv, c * 512, 512), "wbig%d" % wb, t_wbig[wb])
                if c < 4:
                    for j in range(4):
                        pa = next_pa()
                        for kt in range(KT):
                            P.pe(lambda e, kt=kt, j=j, pa=pa, wb=wb: e.matmul(psf[pa][:, 0:256], lhsT=wbig[wb][:, kt, j * 128:(j + 1) * 128],
                                                                              rhs=memT[:, kt, :], start=(kt == 0), stop=(kt == KT - 1)),
                                 reads=[t_wbig[wb]] + t_memT, writes=[t_psf[pa]])
                        copy_evac(kT[:, c * 4 + j, :], psf[pa][:, 0:256], [t_psf[pa]], [t_kT])
                else:
                    for mt in range(2):
                        pa = next_pa()
                        for kt in range(KT):
                            P.pe(lambda e, kt=kt, mt=mt, pa=pa, wb=wb: e.matmul(psf[pa][:], lhsT=memT[:, kt, mt * 128:(mt + 1) * 128],
                                                                                rhs=wbig[wb][:, kt, :], start=(kt == 0), stop=(kt == KT - 1)),
                                 reads=[t_wbig[wb], t_memT[mt]], writes=[t_psf[pa]])
                        copy_evac(v_mem[:, mt, (c - 4) * 512:(c - 3) * 512], psf[pa][:], [t_psf[pa]], [t_vmem])
            P.barrier()
            A.top = xa_base
            qT = A.alloc([128, KT, T], BF16)
            t_qT = P.toks("qT", 2)
            assert A.top <= M_AFTER_OTOK
            load_gbc(g_xa)
            norm_to_T(lambda i: h[:, i, :], t_h, NT, aT, t_aT)
            for c in range(4):
                wb = c % 2
                wload(wbig[wb], wview(w_xq, c * 512, 512), "wbig%d" % wb, t_wbig[wb])
                emit_conv(1)
                for j in range(4):
                    for half in range(2):
                        pa = next_pa()
                        proj_fm(wbig[wb][:, :, j * 128:(j + 1) * 128], t_wbig[wb], aT, t_aT, half, pa)
                        evac(lambda e, pa=pa, c=c, j=j, half=half: e.activation(out=qT[:, c * 4 + j, half * 512:(half + 1) * 512],
                                                                                in_=psf[pa][:], func=AF.Copy, scale=512.0 ** -0.5),
                             lambda e, pa=pa, c=c, j=j, half=half: e.tensor_scalar(out=qT[:, c * 4 + j, half * 512:(half + 1) * 512],
                                                                                   in0=psf[pa][:], scalar1=512.0 ** -0.5, scalar2=None,
                                                                                   op0=ALU.mult),
                             [t_psf[pa]], [t_qT[half]])
            for i in range(NT):
                tsl = slice(i * 128, (i + 1) * 128)
                for pr in range(2):
                    bank = 2 + pr
                    for hh in range(2):
                        hd = pr * 2 + hh
                        for j in range(4):
                            P.pe(lambda e, bank=bank, hh=hh, hd=hd, j=j, tsl=tsl: e.matmul(psf[bank][:, hh * 256:(hh + 1) * 256],
                                                                                  lhsT=qT[:, hd * 4 + j, tsl], rhs=kT[:, hd * 4 + j, :],
                                                                                  start=(j == 0), stop=(j == 3)),
                                 reads=[t_qT[i // 4], t_kT], writes=[t_psf[bank]])
                    P.dve(lambda e, bank=bank, pr=pr: e.tensor_reduce(out=xst[:, pr * 2:pr * 2 + 2],
                                                                     in_=psf[bank][:].rearrange("p (a b) -> p a b", b=256),
                                                                     axis=AX.X, op=ALU.max),
                          reads=[t_psf[bank]], writes=[t_xst])
                    P.dve(lambda e, pr=pr: e.tensor_scalar(out=xst[:, 4 + pr * 2:6 + pr * 2], in0=xst[:, pr * 2:pr * 2 + 2],
                                                           scalar1=-1.0, scalar2=None, op0=ALU.mult),
                          reads=[t_xst], writes=[t_xst])
                    for hh in range(2):
                        hd = pr * 2 + hh
                        P.act(lambda e, bank=bank, hh=hh, hd=hd: e.activation(out=p_sb[:, hd, :], in_=psf[bank][:, hh * 256:(hh + 1) * 256],
                                                                             func=AF.Exp, bias=xst[:, 4 + hd:5 + hd],
                                                                             accum_out=xst[:, 8 + hd:9 + hd]),
                              reads=[t_psf[bank], t_xst], writes=[t_p, t_xst])
                P.dve(lambda e: e.reciprocal(out=xst[:, 12:16], in_=xst[:, 8:12]), reads=[t_xst], writes=[t_xst])
                pb = next_pb()
                for hd in range(4):
                    for mt in range(2):
                        P.pe(lambda e, hd=hd, mt=mt, pb=pb: e.transpose(out=psb(pb)[:, (hd * 2 + mt) * 128:(hd * 2 + mt + 1) * 128],
                                                                in_=p_sb[:, hd, mt * 128:(mt + 1) * 128], identity=ident_bf),
                             reads=[t_p, t_const], writes=[t_psf[pb]])
                copy_evac(pT_sb, psb(pb).rearrange("p (n c) -> p n c", c=128), [t_psf[pb]], [t_pT])
                for hd in range(4):
                    pa = next_pa()
                    for mt in range(2):
                        P.pe(lambda e, hd=hd, mt=mt, pa=pa: e.matmul(psf[pa][:], lhsT=pT_sb[:, hd * 2 + mt, :],
                                                                     rhs=v_mem[:, mt, hd * 512:(hd + 1) * 512],
                                                                     start=(mt == 0), stop=(mt == 1)),
                             reads=[t_pT, t_vmem], writes=[t_psf[pa]])
                    evac(lambda e, hd=hd, pa=pa: e.activation(out=a_tok[:, hd * 512:(hd + 1) * 512], in_=psf[pa][:], func=AF.Copy,
                                                              scale=xst[:, 12 + hd:13 + hd]),
                         lambda e, hd=hd, pa=pa: e.tensor_scalar(out=a_tok[:, hd * 512:(hd + 1) * 512], in0=psf[pa][:],
                                                                 scalar1=xst[:, 12 + hd:13 + hd], scalar2=None, op0=ALU.mult),
                         [t_psf[pa], t_xst], [t_atok])
                transpose_tile(a_tok, t_atok, aT, t_aT[i], tsl)
            proj_residual(w_xo, aT, t_aT)

        if stage >= 3:
            P.barrier()
            A.top = M_PERSIST - KT * T * 2
            moe_lo = A.top
            a3_tok = A.alloc([128, NT, D], BF16)
            t_a3 = P.toks("a3", NT)
            G_all = A.alloc([128, NT, 32])
            ind_all = A.alloc([128, NT, 32])
            ind_bf = A.alloc([128, NT, 32], BF16)
            rank_all = A.alloc([128, NT, 32])
            GT = A.alloc([32, T])
            rankT = A.alloc([32, T])
            moe_ov = A.top
            wr_sb = A.alloc([128, KT, 36])
            br_bc = A.alloc([128, 36])
            lg = A.alloc([128, 36])
            rt = A.alloc([128, 64])
            ml = A.alloc([128, 32])
            top8 = A.alloc([128, 8])
            a3fT = A.alloc([128, 4, 128])
            a3f = ja.bitcast(F32)
            A.top = moe_ov
            esel = A.alloc([32, 128])
            Sel = A.alloc([128, NT, 128], BF16)
            gbc_sb = A.alloc([128, T])
            GselT = A.alloc([128, T], BF16)
            xeT = A.alloc([128, KT, 128], BF16)
            hb_tmp = A.alloc([128, 512])
            hb = A.alloc([128, 1024], BF16)
            hbT = A.alloc([128, 8, 128], BF16)
            Y_sb = A.alloc([128, 512], BF16)
            assert A.top <= M_AFTER_OTOK, ("moe work region overflow", A.top, M_AFTER_OTOK)
            A.top = M_AFTER_OTOK + NT * D * 4
            NSLOT = (ARENA_BYTES - A.top) // (KT * 512 * 2)
            slots = [A.alloc([128, KT, 512], BF16) for _ in range(NSLOT)]
            slots.append(gj.rearrange("p (a b) -> p a b", b=512))
            NSLOT += 1
            t_slot = P.toks("slot", NSLOT)
            (t_G, t_ind, t_rank, t_GT, t_rankT, t_esel, t_wr, t_lg, t_rt, t_ml, t_top8, t_a3f_, t_a3fT, t_Sel,
             t_gbcsb, t_GselT, t_xeT, t_hbtmp, t_hb, t_hbT, t_Y) = P.toks("moe", 21)
            t_a3f = t_junk

            emit_conv(len(conv_list))
            pre0 = {}
            if 0 not in CONV_SET or NCONV == 0:
                for k_, (wsrc, c_) in enumerate(((w_eg, 0), (w_eu, 0), (w_eg, 1))):
                    wload(slots[k_], wview(wsrc, c_ * 512, 512, r0=0), "slot%d" % k_, t_slot[k_])
                    pre0[k_] = True
            P.dma("sp", lambda e: e.dma_start(out=wr_sb[:, :, 0:4], in_=w_rg.rearrange("(kt p) n -> p kt n", p=128)), "wr", writes=[t_wr])
            P.dma("sp", lambda e: e.dma_start(out=wr_sb[:, :, 4:36], in_=w_re.rearrange("(kt p) n -> p kt n", p=128)), "wr", writes=[t_wr])
            P.dma("sp", lambda e: e.dma_start(out=br_bc[:, 0:4], in_=b_rg.partition_broadcast(128)), "wr", writes=[t_wr])
            P.dma("sp", lambda e: e.dma_start(out=br_bc[:, 4:36], in_=b_re.partition_broadcast(128)), "wr", writes=[t_wr])
            load_gbc(g_ffn)
            rms_stats(lambda i: h[:, i, :], t_h, NT)
            for i in range(NT):
                P.dve(lambda e, i=i: e.scalar_tensor_tensor(out=a3f, in0=h[:, i, :], scalar=stat[:, 3, i:i + 1],
                                                            in1=gbc, op0=ALU.mult, op1=ALU.mult),
                      reads=[t_h[i], t_stat, t_gbc], writes=[t_a3f, t_atok])
                P.act(lambda e, i=i: e.activation(out=a3_tok[:, i, :], in_=a3f, func=AF.Copy), reads=[t_a3f], writes=[t_a3[i]])
                for q4 in range(4):
                    bank = 2 + (q4 % 2)
                    for j in range(4):
                        kt = q4 * 4 + j
                        P.pe(lambda e, bank=bank, j=j, kt=kt: e.transpose(out=psf[bank][:, j * 128:(j + 1) * 128],
                                                                         in_=a3f[:, kt * 128:(kt + 1) * 128], identity=ident_f),
                             reads=[t_a3f, t_const], writes=[t_psf[bank]])
                    copy_evac(a3fT, psf[bank][:].rearrange("p (j c) -> p j c", c=128), [t_psf[bank]], [t_a3fT])
                    for j in range(4):
                        kt = q4 * 4 + j
                        P.pe(lambda e, j=j, kt=kt: e.matmul(psf[4][:, 0:36], lhsT=a3fT[:, j, :], rhs=wr_sb[:, kt, :],
                                                           start=(kt == 0), stop=(kt == KT - 1)),
                             reads=[t_a3fT, t_wr], writes=[t_psf[4]])
                P.dve(lambda e: e.tensor_tensor(out=lg, in0=psf[4][:, 0:36], in1=br_bc, op=ALU.add),
                      reads=[t_psf[4], t_wr], writes=[t_lg])
                P.dve(lambda e: e.tensor_reduce(out=rt[:, 0:1], in_=lg[:, 0:4], axis=AX.X, op=ALU.max), reads=[t_lg], writes=[t_rt])
                P.dve(lambda e: e.tensor_scalar(out=rt[:, 1:2], in0=rt[:, 0:1], scalar1=-1.0, scalar2=None, op0=ALU.mult),
                      reads=[t_rt], writes=[t_rt])
                P.act(lambda e: e.activation(out=rt[:, 4:8], in_=lg[:, 0:4], func=AF.Exp, bias=rt[:, 1:2], accum_out=rt[:, 2:3]),
                      reads=[t_lg, t_rt], writes=[t_rt])
                P.dve(lambda e: e.reciprocal(out=rt[:, 3:4], in_=rt[:, 2:3]), reads=[t_rt], writes=[t_rt])
                P.dve(lambda e: e.tensor_scalar(out=rt[:, 8:12], in0=lg[:, 0:4], scalar1=rt[:, 0:1], scalar2=None, op0=ALU.is_equal),
                      reads=[t_lg, t_rt], writes=[t_rt])
                P.dve(lambda e: e.tensor_scalar(out=rt[:, 12:16], in0=rt[:, 8:12], scalar1=-1.0, scalar2=1e30, op0=ALU.add, op1=ALU.mult),
                      reads=[t_rt], writes=[t_rt])
                P.dve(lambda e: e.tensor_tensor(out=ml.rearrange("p (g k) -> p g k", k=8),
                                                in0=lg[:, 4:36].rearrange("p (g k) -> p g k", k=8),
                                                in1=rt[:, 8:12].unsqueeze(2).to_broadcast([128, 4, 8]), op=ALU.mult),
                      reads=[t_lg, t_rt], writes=[t_ml])
                P.dve(lambda e: e.tensor_tensor(out=ml.rearrange("p (g k) -> p g k", k=8),
                                                in0=ml.rearrange("p (g k) -> p g k", k=8),
                                                in1=rt[:, 12:16].unsqueeze(2).to_broadcast([128, 4, 8]), op=ALU.add),
                      reads=[t_ml, t_rt], writes=[t_ml])
                P.dve(lambda e: e.max(out=top8, in_=ml), reads=[t_ml], writes=[t_top8])
                P.dve(lambda e: e.tensor_scalar(out=rt[:, 16:17], in0=top8[:, 0:1], scalar1=-1.0, scalar2=None, op0=ALU.mult),
                      reads=[t_top8], writes=[t_rt])
                P.act(lambda e: e.activation(out=rt[:, 17:18], in_=top8[:, 1:2], func=AF.Exp, bias=rt[:, 16:17]),
                      reads=[t_top8, t_rt], writes=[t_rt])
                P.dve(lambda e: e.tensor_scalar(out=rt[:, 18:19], in0=rt[:, 17:18], scalar1=1.0, scalar2=None, op0=ALU.add),
                      reads=[t_rt], writes=[t_rt])
                P.dve(lambda e: e.reciprocal(out=rt[:, 19:20], in_=rt[:, 18:19]), reads=[t_rt], writes=[t_rt])
                P.dve(lambda e: e.tensor_tensor(out=rt[:, 20:21], in0=rt[:, 19:20], in1=rt[:, 3:4], op=ALU.mult),
                      reads=[t_rt], writes=[t_rt])
                P.dve(lambda e: e.tensor_tensor(out=rt[:, 21:22], in0=rt[:, 20:21], in1=rt[:, 17:18], op=ALU.mult),
                      reads=[t_rt], writes=[t_rt])
                P.dve(lambda e, i=i: e.tensor_scalar(out=G_all[:, i, :], in0=ml, scalar1=top8[:, 0:1], scalar2=rt[:, 20:21],
                                                     op0=ALU.is_equal, op1=ALU.mult), reads=[t_ml, t_top8, t_rt], writes=[t_G])
                P.dve(lambda e: e.tensor_scalar(out=rt[:, 32:64], in0=ml, scalar1=top8[:, 1:2], scalar2=rt[:, 21:22],
                                                op0=ALU.is_equal, op1=ALU.mult), reads=[t_ml, t_top8, t_rt], writes=[t_rt])
                P.dve(lambda e, i=i: e.tensor_tensor(out=G_all[:, i, :], in0=G_all[:, i, :], in1=rt[:, 32:64], op=ALU.add),
                      reads=[t_G, t_rt], writes=[t_G])
                P.dve(lambda e, i=i: e.tensor_scalar(out=ind_all[:, i, :], in0=G_all[:, i, :], scalar1=0.0, scalar2=None, op0=ALU.is_gt),
                      reads=[t_G], writes=[t_ind])
                P.dve(lambda e, i=i: e.tensor_copy(out=ind_bf[:, i, :], in_=ind_all[:, i, :]), reads=[t_ind], writes=[t_ind])
            for i in range(NT):
                P.pe(lambda e, i=i: e.matmul(psf[5][:, 0:32], lhsT=ltri, rhs=ind_bf[:, i, :], start=True, stop=(i == 0)),
                     reads=[t_ind, t_const], writes=[t_psf[5]])
                for i2 in range(i):
                    P.pe(lambda e, i2=i2, i=i: e.matmul(psf[5][:, 0:32], lhsT=ones_bf, rhs=ind_bf[:, i2, :], start=False, stop=(i2 == i - 1)),
                         reads=[t_ind, t_const], writes=[t_psf[5]])
                copy_evac(rank_all[:, i, :], psf[5][:, 0:32], [t_psf[5]], [t_rank])
            for (src_all, src_t, dst, dst_t) in ((G_all, t_G, GT, t_GT), (rank_all, t_rank, rankT, t_rankT)):
                for half in range(2):
                    bank = 2 + half
                    for j in range(4):
                        i = half * 4 + j
                        P.pe(lambda e, bank=bank, j=j, i=i, src_all=src_all: e.transpose(out=psf[bank][0:32, j * 128:(j + 1) * 128],
                                                                                         in_=src_all[:, i, :], identity=ident_f),
                             reads=[src_t, t_const], writes=[t_psf[bank]])
                    copy_evac(dst[:, half * 512:(half + 1) * 512], psf[bank][0:32, :], [t_psf[bank]], [dst_t])

            P.barrier()
            slot_rr = {"i": 0}

            def next_slot():
                s_ = slot_rr["i"] % NSLOT
                slot_rr["i"] += 1
                return s_

            def eload(dst, w32, w16, c0, n, r0, rows, key, tok, conv):
                if conv:
                    src = w16[r0:r0 + rows, c0:c0 + n].rearrange("(kt p) n -> p kt n", p=128)
                    P.dma("sp", lambda e: e.dma_start(out=dst, in_=src), "H" + key, reads=[conv_state["last_tok"]], writes=[tok])
                else:
                    src = w32[r0:r0 + rows, c0:c0 + n].rearrange("(kt p) n -> p kt n", p=128)
                    P.dma("pool", lambda e: e.dma_start(out=dst, in_=src), key, writes=[tok])

            def expert(ex):
                conv = (ex in CONV_SET) and NCONV > 0
                for i in range(NT):
                    P.dve(lambda e, i=i: e.tensor_scalar(out=Sel[:, i, :], in0=iota_row, scalar1=rank_all[:, i, ex:ex + 1],
                                                         scalar2=ind_all[:, i, ex:ex + 1], op0=ALU.is_equal, op1=ALU.mult),
                          reads=[t_rank, t_ind, t_const], writes=[t_Sel])
                P.pool(lambda e: e.tensor_copy(out=esel, in_=ident_f[0:32, ex:ex + 1].to_broadcast([32, 128])),
                       reads=[t_const], writes=[t_esel])
                for kt in range(KT):
                    bank = kt // 4
                    for i in range(NT):
                        P.pe(lambda e, kt=kt, i=i, bank=bank: e.matmul(psf[bank][:, (kt % 4) * 128:(kt % 4 + 1) * 128],
                                                                       lhsT=a3_tok[:, i, kt * 128:(kt + 1) * 128], rhs=Sel[:, i, :],
                                                                       start=(i == 0), stop=(i == NT - 1)),
                             reads=[t_a3[i], t_Sel], writes=[t_psf[bank]])
                for bank in range(4):
                    copy_evac(xeT[:, bank * 4:(bank + 1) * 4, :], psf[bank][:].rearrange("p (j c) -> p j c", c=128),
                              [t_psf[bank]], [t_xeT])
                for half in range(2):
                    sl = slice(half * 512, (half + 1) * 512)
                    P.pe(lambda e, sl=sl: e.matmul(psf[4][:], lhsT=esel, rhs=GT[:, sl], start=True, stop=True),
                         reads=[t_esel, t_GT], writes=[t_psf[4]])
                    P.act(lambda e, sl=sl: e.activation(out=gbc_sb[:, sl], in_=psf[4][:], func=AF.Copy),
                          reads=[t_psf[4]], writes=[t_gbcsb])
                    P.pe(lambda e, sl=sl: e.matmul(psf[5][:], lhsT=esel, rhs=rankT[:, sl], start=True, stop=True),
                         reads=[t_esel, t_rankT], writes=[t_psf[5]])
                    P.dve(lambda e, sl=sl: e.scalar_tensor_tensor(out=GselT[:, sl], in0=psf[5][:], scalar=iota_p[:, 0:1],
                                                                  in1=gbc_sb[:, sl], op0=ALU.is_equal, op1=ALU.mult),
                          reads=[t_psf[5], t_gbcsb, t_const], writes=[t_GselT])
                for c in range(2):
                    sg_ = next_slot()
                    if not (ex == 0 and pre0.get(sg_) and slot_rr["i"] <= 3):
                        eload(slots[sg_], w_eg, wbf_g if conv else None, c * 512, 512, ex * D, D, "slot%d" % sg_, t_slot[sg_], conv)
                    su_ = next_slot()
                    if not (ex == 0 and pre0.get(su_) and slot_rr["i"] <= 3):
                        eload(slots[su_], w_eu, wbf_u if conv else None, c * 512, 512, ex * D, D, "slot%d" % su_, t_slot[su_], conv)
                    for (bank, s_) in ((4, sg_), (5, su_)):
                        for kt in range(KT):
                            P.pe(lambda e, kt=kt, bank=bank, s_=s_: e.matmul(psf[bank][:], lhsT=xeT[:, kt, :], rhs=slots[s_][:, kt, :],
                                                                             start=(kt == 0), stop=(kt == KT - 1)),
                                 reads=[t_xeT, t_slot[s_]], writes=[t_psf[bank]])
                    silu_to(hb_tmp, psf[4][:], [t_psf[4]], t_hbtmp)
                    P.dve(lambda e, c=c: e.tensor_tensor(out=hb[:, c * 512:(c + 1) * 512], in0=psf[5][:], in1=hb_tmp, op=ALU.mult),
                          reads=[t_psf[5], t_hbtmp], writes=[t_hb])
                pb = next_pb()
                for kt in range(8):
                    P.pe(lambda e, kt=kt: e.transpose(out=psb(pb)[:, kt * 128:(kt + 1) * 128], in_=hb[:, kt * 128:(kt + 1) * 128],
                                                     identity=ident_bf), reads=[t_hb, t_const], writes=[t_psf[pb]])
                copy_evac(hbT, psb(pb).rearrange("p (n c) -> p n c", c=128), [t_psf[pb]], [t_hbT])
                for c2 in range(2):
                    sd_ = next_slot()
                    sl_v = slots[sd_].rearrange("p a b -> p (a b)").rearrange("p (a b) -> p a b", b=1024)
                    eload(sl_v, w_ed, wbf_d if conv else None, c2 * 1024, 1024, ex * 1024, 1024, "slot%d" % sd_, t_slot[sd_], conv)
                    for c3 in range(2):
                        dc = c2 * 2 + c3
                        for kt in range(8):
                            P.pe(lambda e, kt=kt, c3=c3, sl_v=sl_v: e.matmul(psf[0][:], lhsT=hbT[:, kt, :],
                                                                             rhs=sl_v[:, kt, c3 * 512:(c3 + 1) * 512],
                                                                             start=(kt == 0), stop=(kt == 7)),
                                 reads=[t_hbT, t_slot[sd_]], writes=[t_psf[0]])
                        copy_evac(Y_sb, psf[0][:], [t_psf[0]], [t_Y])
                        for i in range(NT):
                            bank = 1 + (i % 3)
                            P.pe(lambda e, i=i, bank=bank: e.matmul(psf[bank][:], lhsT=GselT[:, i * 128:(i + 1) * 128], rhs=Y_sb,
                                                                    start=True, stop=True),
                                 reads=[t_GselT, t_Y], writes=[t_psf[bank]])
                            P.dve(lambda e, i=i, bank=bank, dc=dc: e.tensor_tensor(out=h[:, i, dc * 512:(dc + 1) * 512], in0=psf[bank][:],
                                                                                   in1=h[:, i, dc * 512:(dc + 1) * 512], op=ALU.add),
                                  reads=[t_psf[bank], t_h[i]], writes=[t_h[i]])

            for ex in range(N_EXP):
                expert(ex)

        if stage >= 3:
            P.barrier()
        if final_norm:
            load_gbc(g_fin)
            rms_stats(lambda i: h[:, i, :], t_h, NT)
        for i in range(NT):
            if final_norm:
                P.dve(lambda e, i=i: e.scalar_tensor_tensor(out=h[:, i, :], in0=h[:, i, :], scalar=stat[:, 3, i:i + 1],
                                                            in1=gbc, op0=ALU.mult, op1=ALU.mult),
                      reads=[t_h[i], t_stat, t_gbc], writes=[t_h[i]])
            P.dma("sp", lambda e, i=i: e.dma_start(out=out[i * 128:(i + 1) * 128, :], in_=h[:, i, :]), "out",
                  reads=[t_h[i]], final=True)

        P.arena_high = A.high
        P.build()
    return nc, P, names


def make_in_maps(inputs, names):
    f = lambda a: np.ascontiguousarray(np.asarray(a, dtype=np.float32))
    x = f(inputs["x"])
    shapes = {
        "norm_mix_g": (D,), "w_in": (D, 7184), "w_gla_alpha_up": (16, 512), "b_gla_alpha": (512, 1),
        "gla_out_norm_g": (256,), "hgrn_lb_logits": (2, 1024), "hgrn_out_norm_g": (128,), "w_mix_out": (D, D),
        "norm_xattn_g": (D,), "norm_mem_g": (D,), "w_xattn_q": (D, D), "w_xattn_kv": (D, 2 * D), "w_xattn_out": (D, D),
        "norm_ffn_g": (D,), "w_router_group": (D, 4), "b_router_group": (4,), "w_router_expert": (D, 32),
        "b_router_expert": (32,), "w_expert_gate": (N_EXP * D, 1024), "w_expert_up": (N_EXP * D, 1024),
        "w_expert_down": (N_EXP * 1024, D), "norm_final_g": (D,),
    }
    shared = {k: f(inputs[k]).reshape(shp) for k, shp in shapes.items() if k in names}
    in_maps = []
    for c in range(8):
        b, s = c // 4, c % 4
        xp = np.zeros((NPREV * T, D), np.float32)
        if s > 0:
            xp[(NPREV - s) * T:] = x[b, 0:s * T]
        m = dict(shared)
        m["x_own"] = np.ascontiguousarray(x[b, s * T:(s + 1) * T])
        m["x_prev"] = xp
        if "mem" in names:
            m["mem"] = f(inputs["mem"])[b]
        in_maps.append(m)
    return in_maps


def run(inputs, stage=3, final_norm=True, trace=False, cores=8):
    nc, P, names = build_nc(stage=stage, final_norm=final_norm)
    in_maps = make_in_maps(inputs, names)[:cores]
    res = run_bass_kernel_spmd(nc, in_maps, core_ids=list(range(cores)), trace=trace)
    outp = np.zeros((2, 4096, D), np.float32)
    for c in range(cores):
        b, s = c // 4, c % 4
        outp[b, s * T:(s + 1) * T] = res.results[c]["out"]
    return outp, res


def kernel(**inputs):
    outp, _ = run(inputs)
    return outp
```
